# Optimizing a Trainium2 kernel written in Bass

```python
import jax, jax.numpy as jnp
from jax import lax
import numpy as np

D_MODEL = 2048
BATCH = 4
SEQ = 4096
DEPTH = 1

CHUNK = 64
LRU_WIDTH = 1024
LRU_BLOCKS = 16
LRU_BLOCK_W = LRU_WIDTH // LRU_BLOCKS
CONV_WIDTH = 4
RG_C = 8.0
RWKV_WIDTH = D_MODEL - LRU_WIDTH
HEAD_SIZE = 64
RWKV_HEADS = RWKV_WIDTH // HEAD_SIZE
DECAY_LORA = 96
AAA_LORA = 96
GATE_LORA = 256
RWKV_SHIFT_W = 3 * RWKV_WIDTH + DECAY_LORA + AAA_LORA + GATE_LORA
IN_PROJ_W = 2 * LRU_WIDTH + RWKV_SHIFT_W
N_EXPERTS = 32
TOP_K = 4
D_EXPERT = D_MODEL
SWIGLU_LIMIT = 7.0
SWIGLU_ALPHA = 1.702
MOE_BLOCK = 128
LN_EPS = 1e-5
GN_EPS = HEAD_SIZE * 1e-5
DEEPNORM_ALPHA = (2.0 * DEPTH) ** 0.25
DEEPNORM_BETA = (8.0 * DEPTH) ** -0.25

kernel_name = 'hybrid_rglru_rwkv7_moe_deepnorm'


def _layer_norm(x, g, b):
    xf = x.astype(jnp.float32)
    mu = jnp.mean(xf, axis=-1, keepdims=True)
    var = jnp.mean(jnp.square(xf - mu), axis=-1, keepdims=True)
    return ((xf - mu) * lax.rsqrt(var + LN_EPS) * g + b).astype(x.dtype)


def _rglru_branch(u, gate_in, conv_w, conv_b, w_rgate, b_rgate, w_igate, b_igate, lru_lambda):
    bsz, t, c = u.shape
    f32 = jnp.float32
    uc = lax.conv_general_dilated(u, conv_w[:, None, :], window_strides=(1,), padding=[(CONV_WIDTH - 1, 0)], dimension_numbers=('NWC', 'WIO', 'NWC'), feature_group_count=c) + conv_b
    ub = uc.reshape(bsz, t, LRU_BLOCKS, LRU_BLOCK_W)
    r_gate = jax.nn.sigmoid(jnp.einsum('btnj,njk->btnk', ub, w_rgate).reshape(bsz, t, c) + b_rgate)
    i_gate = jax.nn.sigmoid(jnp.einsum('btnj,njk->btnk', ub, w_igate).reshape(bsz, t, c) + b_igate)
    log_a = (-RG_C * r_gate.astype(f32)) * jax.nn.softplus(-lru_lambda.astype(f32))
    a = jnp.exp(log_a)
    b = jnp.sqrt(-jnp.expm1(2.0 * log_a)) * (i_gate * uc).astype(f32)

    def combine(lhs, rhs):
        a1, b1 = lhs
        a2, b2 = rhs
        return a1 * a2, a2 * b1 + b2

    _, h = lax.associative_scan(combine, (a, b), axis=1)
    return h.astype(u.dtype) * jax.nn.gelu(gate_in)


def _rwkv7_scan(r, w, k, v, kk, a):
    bsz, t, h, n = r.shape
    n_chunks = t // CHUNK

    def to_chunks(z):
        return z.transpose(1, 0, 2, 3).reshape(n_chunks, CHUNK, bsz, h, n)

    def step(s, inp):
        r_t, w_t, k_t, v_t, kk_t, a_t = inp
        s_kk = jnp.einsum('bhvk,bhk->bhv', s, kk_t)
        s = s * w_t[:, :, None, :] - s_kk[..., None] * (kk_t * a_t)[:, :, None, :] + v_t[..., None] * k_t[:, :, None, :]
        return s, jnp.einsum('bhvk,bhk->bhv', s, r_t)

    def chunk_step(s, inp_c):
        return lax.scan(step, s, inp_c)

    s0 = jnp.zeros((bsz, h, n, n), jnp.float32)
    _, y = lax.scan(chunk_step, s0, tuple(to_chunks(z) for z in (r, w, k, v, kk, a)))
    return y.reshape(t, bsz, h, n).transpose(1, 0, 2, 3)


def _rwkv7_branch(p, shift_mu, w0, rw_decay_up, a0, rw_aaa_up, rw_gate_up, k_k, k_a, r_k, gn_g, gn_b):
    bsz, t, _ = p.shape
    f32 = jnp.float32
    p_prev = jnp.pad(p, ((0, 0), (1, 0), (0, 0)))[:, :-1]
    p = p + (p_prev - p) * shift_mu
    splits = [RWKV_WIDTH, 2 * RWKV_WIDTH, 3 * RWKV_WIDTH, 3 * RWKV_WIDTH + DECAY_LORA, 3 * RWKV_WIDTH + DECAY_LORA + AAA_LORA]
    r, k, v, wd, ad, gd = jnp.split(p, splits, axis=-1)
    w_log = -jax.nn.softplus(-(w0 + jnp.tanh(wd) @ rw_decay_up).astype(f32)) - 0.5
    decay = jnp.exp(-jnp.exp(w_log))
    a = jax.nn.sigmoid((a0 + ad @ rw_aaa_up).astype(f32))
    g = (jax.nn.sigmoid(gd) @ rw_gate_up).astype(f32)

    def heads(z):
        return z.reshape(bsz, t, RWKV_HEADS, HEAD_SIZE)

    kk = heads((k * k_k).astype(f32))
    kk = kk / jnp.maximum(jnp.linalg.norm(kk, axis=-1, keepdims=True), 1e-12)
    k = k.astype(f32) * (1.0 + (a - 1.0) * k_a.astype(f32))
    r_h, k_h, v_h = heads(r.astype(f32)), heads(k), heads(v.astype(f32))
    y = _rwkv7_scan(r_h, heads(decay), k_h, v_h, kk, heads(a))
    mu = jnp.mean(y, axis=-1, keepdims=True)
    var = jnp.mean(jnp.square(y - mu), axis=-1, keepdims=True)
    y = (y - mu) * lax.rsqrt(var + GN_EPS) * gn_g.reshape(RWKV_HEADS, HEAD_SIZE) + gn_b.reshape(RWKV_HEADS, HEAD_SIZE)
    y = y + jnp.sum(r_h * k_h * r_k, axis=-1, keepdims=True) * v_h
    return (y.reshape(bsz, t, RWKV_WIDTH) * g).astype(p.dtype)


def _clamped_swiglu(h):
    gate, up = jnp.split(h, 2, axis=-1)
    gate = jnp.minimum(gate, SWIGLU_LIMIT)
    up = jnp.clip(up, -SWIGLU_LIMIT, SWIGLU_LIMIT)
    return gate * jax.nn.sigmoid(SWIGLU_ALPHA * gate) * (up + 1.0)


def _moe(x2, w_router, b_router, w_exp1, b_exp1, w_exp2, b_exp2):
    n = x2.shape[0]
    logits = (x2 @ w_router + b_router).astype(jnp.float32)
    top_v, top_i = lax.top_k(logits, TOP_K)
    gates = jax.nn.softmax(top_v, axis=-1).astype(x2.dtype)
    flat_e = top_i.reshape(-1)
    flat_g = gates.reshape(-1)
    order = jnp.argsort(flat_e)
    sorted_e = flat_e[order]
    sorted_tok = (order // TOP_K).astype(jnp.int32)
    counts = jnp.bincount(flat_e, length=N_EXPERTS)
    padded = ((counts + MOE_BLOCK - 1) // MOE_BLOCK) * MOE_BLOCK
    pad_end = jnp.cumsum(padded)
    pad_start = pad_end - padded
    grp_start = jnp.cumsum(counts) - counts
    dest = pad_start[sorted_e] + (jnp.arange(n * TOP_K) - grp_start[sorted_e])
    n_slots = ((n * TOP_K + MOE_BLOCK - 1) // MOE_BLOCK + N_EXPERTS) * MOE_BLOCK
    n_blocks = n_slots // MOE_BLOCK
    slot_tok = jnp.zeros((n_slots,), jnp.int32).at[dest].set(sorted_tok)
    slot_gate = jnp.zeros((n_slots,), x2.dtype).at[dest].set(flat_g[order])
    blk_e = jnp.minimum(jnp.searchsorted(pad_end, jnp.arange(n_blocks) * MOE_BLOCK, side='right'), N_EXPERTS - 1)

    def expert_block(y, blk):
        tok, gate, e = blk
        h = x2[tok] @ w_exp1[e] + b_exp1[e]
        out = _clamped_swiglu(h) @ w_exp2[e] + b_exp2[e]
        return y.at[tok].add(out * gate[:, None]), None

    y, _ = lax.scan(expert_block, jnp.zeros_like(x2), (slot_tok.reshape(n_blocks, MOE_BLOCK), slot_gate.reshape(n_blocks, MOE_BLOCK), blk_e))
    return y


def setup_inputs(seed: int = 0) -> dict:
    key = jax.random.key(seed)
    ks = iter(jax.random.split(key, 40))
    f32 = jnp.float32
    L = DEPTH

    def nrm(shape, scale):
        return jax.random.normal(next(ks), shape, f32) * scale

    def unif(shape, lo, hi):
        return jax.random.uniform(next(ks), shape, f32, lo, hi)

    a_init = unif((L, LRU_WIDTH), 0.9, 0.999)
    return {
        'x': nrm((BATCH, SEQ, D_MODEL), 1.0),
        'ln_in_g': 1.0 + nrm((D_MODEL,), 0.02),
        'ln_in_b': nrm((D_MODEL,), 0.02),
        'w_in': nrm((L, D_MODEL, IN_PROJ_W), D_MODEL ** -0.5),
        'conv_w': nrm((L, CONV_WIDTH, LRU_WIDTH), CONV_WIDTH ** -0.5),
        'conv_b': nrm((L, LRU_WIDTH), 0.01),
        'w_rgate': nrm((L, LRU_BLOCKS, LRU_BLOCK_W, LRU_BLOCK_W), LRU_BLOCK_W ** -0.5),
        'b_rgate': nrm((L, LRU_WIDTH), 0.01),
        'w_igate': nrm((L, LRU_BLOCKS, LRU_BLOCK_W, LRU_BLOCK_W), LRU_BLOCK_W ** -0.5),
        'b_igate': nrm((L, LRU_WIDTH), 0.01),
        'lru_lambda': jnp.log(a_init) - jnp.log1p(-a_init),
        'shift_mu': unif((L, RWKV_SHIFT_W), 0.0, 1.0),
        'w0': unif((L, RWKV_WIDTH), -6.0, 1.0),
        'rw_decay_up': nrm((L, DECAY_LORA, RWKV_WIDTH), 0.1 * DECAY_LORA ** -0.5),
        'a0': nrm((L, RWKV_WIDTH), 0.1),
        'rw_aaa_up': nrm((L, AAA_LORA, RWKV_WIDTH), 0.1 * AAA_LORA ** -0.5),
        'rw_gate_up': nrm((L, GATE_LORA, RWKV_WIDTH), GATE_LORA ** -0.5),
        'k_k': 0.85 + nrm((L, RWKV_WIDTH), 0.02),
        'k_a': 1.0 + nrm((L, RWKV_WIDTH), 0.02),
        'r_k': nrm((L, RWKV_HEADS, HEAD_SIZE), 0.1),
        'gn_g': 1.0 + nrm((L, RWKV_WIDTH), 0.02),
        'gn_b': nrm((L, RWKV_WIDTH), 0.02),
        'w_out': nrm((L, D_MODEL, D_MODEL), DEEPNORM_BETA * D_MODEL ** -0.5),
        'ln1_g': 1.0 + nrm((L, D_MODEL), 0.02),
        'ln1_b': nrm((L, D_MODEL), 0.02),
        'w_router': nrm((L, D_MODEL, N_EXPERTS), D_MODEL ** -0.5),
        'b_router': nrm((L, N_EXPERTS), 0.01),
        'w_exp1': nrm((L, N_EXPERTS, D_MODEL, 2 * D_EXPERT), D_MODEL ** -0.5),
        'b_exp1': nrm((L, N_EXPERTS, 2 * D_EXPERT), 0.01),
        'w_exp2': nrm((L, N_EXPERTS, D_EXPERT, D_MODEL), DEEPNORM_BETA * D_EXPERT ** -0.5),
        'b_exp2': nrm((L, N_EXPERTS, D_MODEL), 0.01),
        'ln2_g': 1.0 + nrm((L, D_MODEL), 0.02),
        'ln2_b': nrm((L, D_MODEL), 0.02),
    }


def reference(x, ln_in_g, ln_in_b, w_in, conv_w, conv_b, w_rgate, b_rgate, w_igate, b_igate, lru_lambda, shift_mu, w0, rw_decay_up, a0, rw_aaa_up, rw_gate_up, k_k, k_a, r_k, gn_g, gn_b, w_out, ln1_g, ln1_b, w_router, b_router, w_exp1, b_exp1, w_exp2, b_exp2, ln2_g, ln2_b):
    bsz, t, d = x.shape
    h = _layer_norm(x, ln_in_g, ln_in_b)
    for l in range(DEPTH):
        p = h @ w_in[l]
        lru_u, lru_gate, rw = jnp.split(p, [LRU_WIDTH, 2 * LRU_WIDTH], axis=-1)
        y_lru = _rglru_branch(lru_u, lru_gate, conv_w[l], conv_b[l], w_rgate[l], b_rgate[l], w_igate[l], b_igate[l], lru_lambda[l])
        y_rw = _rwkv7_branch(rw, shift_mu[l], w0[l], rw_decay_up[l], a0[l], rw_aaa_up[l], rw_gate_up[l], k_k[l], k_a[l], r_k[l], gn_g[l], gn_b[l])
        mix = jnp.concatenate([y_lru, y_rw], axis=-1) @ w_out[l]
        h = _layer_norm(DEEPNORM_ALPHA * h + mix, ln1_g[l], ln1_b[l])
        moe = _moe(h.reshape(bsz * t, d), w_router[l], b_router[l], w_exp1[l], b_exp1[l], w_exp2[l], b_exp2[l]).reshape(bsz, t, d)
        h = _layer_norm(DEEPNORM_ALPHA * h + moe, ln2_g[l], ln2_b[l])
    return h
```

```python
from contextlib import ExitStack
import numpy as np
import concourse.bass as bass
import concourse.mybir as mybir
from concourse.bass_utils import run_bass_kernel_spmd

F32 = mybir.dt.float32
BF16 = mybir.dt.bfloat16
I32 = mybir.dt.int32
U32 = mybir.dt.uint32
AF = mybir.ActivationFunctionType
ALU = mybir.AluOpType
AX = mybir.AxisListType

D = 2048
FC = 16
LRU_W = 1024
NH = 16
HS = 64
G = 512
TOPK = 4
DE = 2048
ALPHA = 2.0 ** 0.25
LN_EPS = 1e-5
GN_EPS = 64 * 1e-5
EPOCH = 4000
NDS = 12
DEBUG = False


class Reg:
    __slots__ = ("w", "r")

    def __init__(self):
        self.w = None
        self.r = {}


class Buf:
    def __init__(self, t):
        self.t = t
        self.reg = Reg()

    def __getitem__(self, k):
        return self.t[k]


class Ring:
    def __init__(self, bufs):
        self.bufs = bufs
        self.i = 0

    def next(self):
        b = self.bufs[self.i]
        self.i = (self.i + 1) % len(self.bufs)
        return b


class KB:
    def __init__(self, nc, es):
        self.nc = nc
        self.es = es
        self.es_sem = es
        self.uid = 0
        self.eng = {"sp": nc.sync, "act": nc.scalar, "dve": nc.vector, "pool": nc.gpsimd, "pe": nc.tensor}
        self.sems = []
        self.esem = {}
        self.cnt = {}
        self.waited = {e: {} for e in self.eng}
        for e in self.eng:
            self._new_epoch(e)
        self.dsem = [self._sem("d%d" % i) for i in range(2 * NDS)]
        self.dval = [0] * (2 * NDS)
        self.drr = {"hw": 0, "sw": 0}
        self.n_ins = 0

    def _sem(self, name):
        s = self.es_sem.enter_context(self.nc.semaphore(name))
        self.sems.append(s)
        return len(self.sems) - 1

    def _new_epoch(self, e):
        self.esem[e] = self._sem("e_%s_%d" % (e, len(self.sems)))
        self.cnt[e] = 0

    def sb(self, name, shape, dt=F32):
        self.uid += 1
        return Buf(self.es.enter_context(self.nc.sbuf_tensor("%s_%d" % (name, self.uid), shape, dt)))

    def barrier(self):
        toks = [(self.esem[f], self.cnt[f]) for f in self.eng if self.cnt[f] > 0]
        toks += [(self.dsem[i], self.dval[i]) for i in range(2 * NDS) if self.dval[i] > 0]
        for e in self.eng:
            for (s_, v_) in toks:
                if self.waited[e].get(s_, 0) < v_:
                    self.eng[e].wait_ge(self.sems[s_], v_)
                    self.waited[e][s_] = v_
                    self.n_ins += 1

    def ring(self, name, shape, dt, n):
        return Ring([self.sb("%s%d" % (name, i), shape, dt) for i in range(n)])

    def _deps(self, e, reads, writes, extra=()):
        deps = list(extra)
        for r in reads:
            if r.reg.w is not None:
                deps.append(r.reg.w)
        for w in writes:
            if w.reg.w is not None:
                deps.append(w.reg.w)
            deps.extend(w.reg.r.items())
        for (s, v) in deps:
            if e == "pe" and s == self.esem["pe"]:
                continue
            if self.waited[e].get(s, 0) < v:
                self.eng[e].wait_ge(self.sems[s], v)
                self.waited[e][s] = v
                self.n_ins += 1

    def _record(self, tok, reads, writes):
        s, v = tok
        for r in reads:
            if r.reg.r.get(s, 0) < v:
                r.reg.r[s] = v
        for w in writes:
            w.reg.w = tok
            w.reg.r = {}

    def op(self, e, fn, reads=(), writes=()):
        self._deps(e, reads, writes)
        if self.cnt[e] >= EPOCH:
            self._new_epoch(e)
        ins = fn(self.eng[e])
        self.cnt[e] += 1
        s = self.esem[e]
        ins.then_inc(self.sems[s], 1)
        self.n_ins += 1
        self._record((s, self.cnt[e]), reads, writes)

    def dma(self, e, fn, reads=(), writes=()):
        kind = "sw" if e == "pool" else "hw"
        k = self.drr[kind] + (NDS if kind == "sw" else 0)
        self.drr[kind] = (self.drr[kind] + 1) % NDS
        extra = [(self.dsem[k], self.dval[k])] if self.dval[k] > 0 else []
        self._deps(e, reads, writes, extra)
        ins = fn(self.eng[e])
        self.dval[k] += 16
        ins.then_inc(self.sems[self.dsem[k]], 16)
        self.n_ins += 1
        self._record((self.dsem[k], self.dval[k]), reads, writes)

    def wait_all(self, e, bufs):
        self._deps(e, bufs, bufs)

    def v(self, fn, r=(), w=()):
        self.op("dve", fn, r, w)

    def a(self, fn, r=(), w=()):
        self.op("act", fn, r, w)

    def pe(self, fn, r=(), w=()):
        self.op("pe", fn, r, w)


def build(TH, NE, mode="full"):
    NT = 2 * TH
    NG = NT // G
    NTILE = TH // 128
    NBLK = TH * TOPK // 128 + NE
    nc = bass.Bass("TRN2", target_bir_lowering=False)

    def din(name, shape, dt=F32):
        return nc.dram_tensor(name, list(shape), dt, kind="ExternalInput").ap()

    xa = din("xa", [NT, D])
    flag_d = din("flag", [128, 1])
    lnrep = din("lnrep", [6, 128, D])
    win_lru = din("win_lru", [16, 128, FC, 128])
    win_rkv = din("win_rkv", [NH, 128, FC, 3 * HS])
    win_lora = din("win_lora", [128, FC, 448])
    lru_cp_d = din("lru_cp", [128, 8, 8])
    wrg_d = din("wrg_bd", [8, 128, 128])
    wig_d = din("wig_bd", [8, 128, 128])
    rw_cp_d = din("rw_cp", [HS, NH, 10])
    mu_lora_d = din("mu_lora", [128, 4])
    dup_d = din("dec_up", [NH, 96, HS])
    aup_d = din("aaa_up", [NH, 96, HS])
    gup_d = din("gate_up", [NH, 128, 2, HS])
    wout_l = din("wout_l", [4, 128, 8, 512])
    wout_r = din("wout_r", [4, HS, NH, 512])
    wr_d = din("w_router", [128, FC, NE])
    br_d = din("b_router", [128, NE])
    w1_d = din("w_exp1", [NE * 8 * 128 if mode == "full" else 128, FC * 512])
    b1_d = din("b_exp1", [NE, 2 * DE])
    w2_d = din("w_exp2", [NE * 4 * 128 if mode == "full" else 128, FC * 512])
    b2_d = din("b_exp2", [NE, D])
    cst_d = din("cst", [128, 128 * 3 + 64 * 4 + NE + 1])
    out_d = nc.dram_tensor("out", [TH, D], F32, kind="ExternalOutput").ap()
    if mode == "mixer" and DEBUG:
        dbg_yl = nc.dram_tensor("dbg_yl", [128, 8, G], BF16, kind="ExternalOutput").ap()
        dbg_yr = nc.dram_tensor("dbg_yr", [HS, NH, G], BF16, kind="ExternalOutput").ap()
        dbg_hT = nc.dram_tensor("dbg_hT", [128, FC, G], BF16, kind="ExternalOutput").ap()
    H1 = nc.dram_tensor("H1", [TH, D], F32, kind="Internal").ap()
    XS = nc.dram_tensor("XS", [NBLK * 128, D], BF16, kind="Internal").ap()
    OS = nc.dram_tensor("OS", [NBLK * 128, D], F32, kind="Internal").ap()
    BLKE = nc.dram_tensor("BLKE", [128, 1], I32, kind="Internal").ap()

    with ExitStack() as es:
        k = KB(nc, es)
        dbg_list = []

        def dbg(name, ap, shape, buf, dt=F32):
            if not DEBUG:
                return
            t_ = nc.dram_tensor("dbg_" + name, list(shape), dt, kind="ExternalOutput").ap()
            b_ = Buf(t_)
            dbg_list.append(b_)
            k.dma("sp", lambda q: q.dma_start(out=t_, in_=ap), [buf], [b_])

        H1b, XSb, OSb, BLKEb, OUTB = Buf(H1), Buf(XS), Buf(OS), Buf(BLKE), Buf(out_d)
        psf = Ring([Buf(es.enter_context(nc.psum_tensor("psf%d" % i, [128, 512], F32))) for i in range(6)])
        pyb = Buf(es.enter_context(nc.psum_tensor("pyb", [128, 512], F32)))
        psb = Buf(es.enter_context(nc.psum_tensor("psb", [128, 1024], BF16)))
        psb_half = [0]
        NC_C = 128 * 3 + 64 * 4 + NE + 1
        cst = k.sb("cst", [128, NC_C])
        k.dma("sp", lambda q: q.dma_start(out=cst[:, :], in_=cst_d), [], [cst])
        ident = cst[:, 0:128]
        ones = cst[:, 128:256]
        tri = cst[:, 256:384]
        o = 384
        m_sl = cst[0:64, o:o + 64]
        m_su = cst[0:64, o + 64:o + 128]
        m_ui = cst[0:64, o + 128:o + 192]
        i64 = cst[0:64, o + 192:o + 256]
        iota_e = cst[:, o + 256:o + 256 + NE]
        iota_p = cst[:, o + 256 + NE:o + 257 + NE]
        ones64 = cst[0:64, 128:192]
        identb = k.sb("identb", [128, 128], BF16)
        k.v(lambda q: q.tensor_copy(out=identb[:, :], in_=ident), [cst], [identb])
        onesb = k.sb("onesb", [1, 128], BF16)
        k.v(lambda q: q.tensor_copy(out=onesb[:, :], in_=cst[0:1, 128:256]), [cst], [onesb])
        flag = k.sb("flagt", [128, 1])
        k.dma("sp", lambda q: q.dma_start(out=flag[:, :], in_=flag_d), [], [flag])
        eidf = k.sb("eidf", [128, NTILE, 8])
        gates = k.sb("gates", [128, NTILE, TOPK])
        ohall = k.sb("ohall", [128, NTILE * TOPK, NE])
        mall = k.sb("mall", [128, NTILE, NE])
        slot_i = k.sb("slot_i", [128, NTILE * TOPK, 1], I32)
        idx1_i = k.sb("idx1_i", [128, 8, 128], I32)
        idx2_i = k.sb("idx2_i", [128, 4, 128], I32)
        eidx_i = k.sb("eidx_i", [128, 128], I32)

        def layer_norm(src, srcbuf, dst, dstbuf, gb, gbbuf, gi, scale, tmp, st, mv):
            for c in range(4):
                k.v(lambda q, c=c: q.bn_stats(out=st[:, c * 6:(c + 1) * 6], in_=src[:, c * 512:(c + 1) * 512]), [srcbuf], [st])
            k.v(lambda q: q.bn_aggr(out=mv[:, 0:2], in_=st[:, 0:24]), [st], [mv])
            k.v(lambda q: q.tensor_scalar(out=mv[:, 2:3], in0=mv[:, 1:2], scalar1=LN_EPS, scalar2=None, op0=ALU.add), [mv], [mv])
            k.a(lambda q: q.activation(out=mv[:, 2:3], in_=mv[:, 2:3], func=AF.Sqrt), [mv], [mv])
            k.v(lambda q: q.reciprocal(out=mv[:, 2:3], in_=mv[:, 2:3]), [mv], [mv])
            k.v(lambda q: q.tensor_scalar(out=tmp[:, :], in0=src, scalar1=mv[:, 0:1], scalar2=mv[:, 2:3], op0=ALU.subtract, op1=ALU.mult), [srcbuf, mv], [tmp])
            k.v(lambda q: q.tensor_tensor(out=tmp[:, :], in0=tmp[:, :], in1=gb[:, gi, :], op=ALU.mult), [tmp, gbbuf], [tmp])
            if scale == 1.0:
                k.v(lambda q: q.tensor_tensor(out=dst, in0=tmp[:, :], in1=gb[:, gi + 1, :], op=ALU.add), [tmp, gbbuf], [dstbuf])
            else:
                k.v(lambda q: q.tensor_tensor(out=tmp[:, :], in0=tmp[:, :], in1=gb[:, gi + 1, :], op=ALU.add), [tmp, gbbuf], [tmp])
                k.v(lambda q: q.tensor_scalar(out=dst, in0=tmp[:, :], scalar1=float(scale), scalar2=None, op0=ALU.mult), [tmp], [dstbuf])

        esM = ExitStack()
        k.es = esM
        hT = k.sb("hT", [128, FC, G], BF16)
        lact = k.sb("lact", [128, 4, G])
        S = k.sb("S", [HS, NH, HS])
        yl = k.sb("yl", [128, 8, G], BF16)
        yr = k.sb("yr", [HS, NH, G], BF16)
        hist = k.sb("hist", [HS, NH, 3])
        hstate = k.sb("hstate", [128, 8])
        uhist = k.sb("uhist", [128, 8, 3])
        lhist = k.sb("lhist", [128, 4])
        for t_ in (S, hist, hstate, uhist, lhist):
            k.v(lambda q, t_=t_: q.memset(t_.t[:], 0.0), [], [t_])
        lru_cp = k.sb("lru_cp", [128, 8, 8])
        k.dma("sp", lambda q: q.dma_start(out=lru_cp[:, :, :], in_=lru_cp_d), [], [lru_cp])
        lru_c = k.sb("lru_c", [128, 8, 2])
        k.a(lambda q: q.activation(out=lru_c[:, :, 0], in_=lru_cp[:, :, 7], func=AF.Exp, scale=-1.0), [lru_cp], [lru_c])
        k.a(lambda q: q.activation(out=lru_c[:, :, 1], in_=lru_c[:, :, 0], func=AF.Ln, bias=1.0), [lru_c], [lru_c])
        k.v(lambda q: q.tensor_scalar(out=lru_c[:, :, 0], in0=lru_c[:, :, 1], scalar1=-8.0, scalar2=None, op0=ALU.mult), [lru_c], [lru_c])
        k.v(lambda q: q.tensor_scalar(out=lru_c[:, :, 1], in0=lru_c[:, :, 0], scalar1=2.0, scalar2=None, op0=ALU.mult), [lru_c], [lru_c])
        wrg = k.sb("wrg", [128, 8, 128])
        wig = k.sb("wig", [128, 8, 128])
        k.dma("sp", lambda q: q.dma_start(out=wrg[:, :, :], in_=wrg_d.rearrange("n p m -> p n m")), [], [wrg])
        k.dma("sp", lambda q: q.dma_start(out=wig[:, :, :], in_=wig_d.rearrange("n p m -> p n m")), [], [wig])
        rw_cp = k.sb("rw_cp", [HS, NH, 10])
        k.dma("sp", lambda q: q.dma_start(out=rw_cp[:, :, :], in_=rw_cp_d), [], [rw_cp])
        mu_lora = k.sb("mu_lora", [128, 4])
        k.dma("sp", lambda q: q.dma_start(out=mu_lora[:, :], in_=mu_lora_d), [], [mu_lora])
        wr = k.sb("wr", [128, FC, NE])
        brt = k.sb("brt", [128, NE])
        k.dma("sp", lambda q: q.dma_start(out=wr[:, :, :], in_=wr_d), [], [wr])
        k.dma("sp", lambda q: q.dma_start(out=brt[:, :], in_=br_d), [], [brt])
        st = k.sb("st", [128, 24])
        mv = k.sb("mv", [128, 4])
        lg = k.sb("lg", [128, NE])
        m8 = k.sb("m8", [128, 8])
        i8 = k.sb("i8", [128, 8], U32)
        sm = k.sb("sm", [128, 8])

        def proj(wt, col0, M, out_ps):
            for kc in range(FC):
                k.pe(lambda q, kc=kc: q.matmul(out_ps[0:M, :], lhsT=wt[:, kc, col0:col0 + M], rhs=hT[:, kc, :], start=(kc == 0), stop=(kc == FC - 1)), [wt, hT], [out_ps])

        def early_stop():
            k.barrier()
            with ExitStack() as esX:
                k.es = esX
                tb = k.sb("tbx", [128, D])
                for tg in range(NTILE):
                    k.dma("sp", lambda q, tg=tg: q.dma_start(out=tb[:, :], in_=xa[tg * 128:(tg + 1) * 128, :]), [], [tb])
                    k.dma("sp", lambda q, tg=tg: q.dma_start(out=out_d[tg * 128:(tg + 1) * 128, :], in_=tb[:, :]), [tb], [OUTB])
                k.wait_all("sp", [OUTB])
            esM.close()
            print("instructions:", k.n_ins)
            return nc

        for g in range(NG):
            t0g = g * G
            with ExitStack() as s1:
                k.es = s1
                lnin = k.sb("lnin", [128, 2, D])
                k.dma("sp", lambda q: q.dma_start(out=lnin[:, :, :], in_=lnrep[0:2].rearrange("a p d -> p a d")), [], [lnin])
                xt = k.sb("xt", [128, D])
                lntmp = k.sb("lntmp", [128, D])
                h0b = k.sb("h0b", [128, D], BF16)
                wlr = k.ring("wlr", [128, FC, 128], BF16, 3)
                wlora = k.sb("wlora", [128, FC, 448], BF16)
                k.dma("pool", lambda q: q.dma_start(out=wlora[:, :, :], in_=win_lora), [], [wlora])
                lw = [k.sb("lw%d" % i, [128, G]) for i in range(6)]
                ub = k.sb("ub", [128, 3 + G])
                lb = k.sb("lb", [128, 1 + G])
                for ti in range(4):
                    r0 = t0g + ti * 128
                    k.dma("sp", lambda q, r0=r0: q.dma_start(out=xt[:, :], in_=xa[r0:r0 + 128, :]), [], [xt])
                    layer_norm(xt[:, :], xt, h0b[:, :], h0b, lnin, lnin, 0, 1.0, lntmp, st, mv)
                    for c4 in range(4):
                        hf = psb_half[0]
                        psb_half[0] ^= 1
                        for j in range(4):
                            kc = c4 * 4 + j
                            k.pe(lambda q, kc=kc, hf=hf, j=j: q.transpose(out=psb[:, hf * 512 + j * 128: hf * 512 + (j + 1) * 128], in_=h0b[:, kc * 128:(kc + 1) * 128], identity=identb[:, :]), [h0b, identb], [psb])
                        k.a(lambda q, c4=c4, hf=hf, ti=ti: q.activation(out=hT[:, c4 * 4:(c4 + 1) * 4, ti * 128:(ti + 1) * 128], in_=psb[:, hf * 512:(hf + 1) * 512].rearrange("p (a b) -> p a b", a=4), func=AF.Copy), [psb], [hT])
                if g == NG // 2:
                    for t_, np_ in ((hstate, 128), (uhist, 128), (lhist, 128), (hist, HS), (S, HS)):
                        k.v(lambda q, t_=t_, np_=np_: q.tensor_scalar(out=t_.t[:], in0=t_.t[:], scalar1=flag[0:np_, 0:1], scalar2=None, op0=ALU.mult), [t_, flag], [t_])
                for ti in range(8):
                    wt = wlr.next()
                    k.dma("pool", lambda q, wt=wt, ti=ti: q.dma_start(out=wt[:, :, :], in_=win_lru[ti]), [], [wt])
                    wt2 = wlr.next()
                    k.dma("pool", lambda q, wt2=wt2, ti=ti: q.dma_start(out=wt2[:, :, :], in_=win_lru[8 + ti]), [], [wt2])
                    pu = psf.next()
                    proj(wt, 0, 128, pu)
                    k.v(lambda q, ti=ti: q.tensor_copy(out=ub[:, 0:3], in_=uhist[:, ti, :]), [uhist], [ub])
                    k.a(lambda q, pu=pu: q.activation(out=ub[:, 3:3 + G], in_=pu[:, :], func=AF.Copy), [pu], [ub])
                    k.v(lambda q, ti=ti: q.tensor_copy(out=uhist[:, ti, :], in_=ub[:, G:G + 3]), [ub], [uhist])
                    pg = psf.next()
                    proj(wt2, 0, 128, pg)
                    uc, rr, ii, aa, bb, gg = lw
                    k.v(lambda q, ti=ti: q.tensor_scalar(out=uc[:, :], in0=ub[:, 0:G], scalar1=lru_cp[:, ti, 0:1], scalar2=lru_cp[:, ti, 4:5], op0=ALU.mult, op1=ALU.add), [ub, lru_cp], [uc])
                    for j in range(1, 4):
                        k.v(lambda q, ti=ti, j=j: q.scalar_tensor_tensor(out=uc[:, :], in0=ub[:, j:j + G], scalar=lru_cp[:, ti, j:j + 1], in1=uc[:, :], op0=ALU.mult, op1=ALU.add), [ub, lru_cp, uc], [uc])
                    pr = psf.next()
                    k.pe(lambda q, ti=ti, pr=pr: q.matmul(pr[:, :], lhsT=wrg[:, ti, :], rhs=uc[:, :], start=True, stop=True), [wrg, uc], [pr])
                    pi = psf.next()
                    k.pe(lambda q, ti=ti, pi=pi: q.matmul(pi[:, :], lhsT=wig[:, ti, :], rhs=uc[:, :], start=True, stop=True), [wig, uc], [pi])
                    k.a(lambda q, ti=ti, pr=pr: q.activation(out=rr[:, :], in_=pr[:, :], func=AF.Sigmoid, bias=lru_cp[:, ti, 5:6]), [pr, lru_cp], [rr])
                    k.a(lambda q, ti=ti, pi=pi: q.activation(out=ii[:, :], in_=pi[:, :], func=AF.Sigmoid, bias=lru_cp[:, ti, 6:7]), [pi, lru_cp], [ii])
                    k.a(lambda q, ti=ti: q.activation(out=aa[:, :], in_=rr[:, :], func=AF.Exp, scale=lru_c[:, ti, 0:1]), [rr, lru_c], [aa])
                    k.a(lambda q, ti=ti: q.activation(out=bb[:, :], in_=rr[:, :], func=AF.Exp, scale=lru_c[:, ti, 1:2]), [rr, lru_c], [bb])
                    k.v(lambda q: q.tensor_scalar(out=bb[:, :], in0=bb[:, :], scalar1=-1.0, scalar2=1.0, op0=ALU.mult, op1=ALU.add), [bb], [bb])
                    k.a(lambda q: q.activation(out=bb[:, :], in_=bb[:, :], func=AF.Sqrt), [bb], [bb])
                    k.v(lambda q: q.tensor_tensor(out=ii[:, :], in0=ii[:, :], in1=uc[:, :], op=ALU.mult), [ii, uc], [ii])
                    k.v(lambda q: q.tensor_tensor(out=bb[:, :], in0=bb[:, :], in1=ii[:, :], op=ALU.mult), [bb, ii], [bb])
                    k.v(lambda q, ti=ti: q.tensor_tensor_scan(out=rr[:, :], data0=aa[:, :], data1=bb[:, :], initial=hstate[:, ti:ti + 1], op0=ALU.mult, op1=ALU.add), [aa, bb, hstate], [rr])
                    k.v(lambda q, ti=ti: q.tensor_copy(out=hstate[:, ti:ti + 1], in_=rr[:, G - 1:G]), [rr], [hstate])
                    k.a(lambda q, pg=pg: q.activation(out=gg[:, :], in_=pg[:, :], func=AF.Copy), [pg], [gg])
                    k.v(lambda q: q.tensor_tensor(out=aa[:, :], in0=gg[:, :], in1=gg[:, :], op=ALU.mult), [gg], [aa])
                    k.v(lambda q: q.tensor_scalar(out=aa[:, :], in0=aa[:, :], scalar1=0.044715, scalar2=1.0, op0=ALU.mult, op1=ALU.add), [aa], [aa])
                    k.v(lambda q: q.tensor_tensor(out=aa[:, :], in0=aa[:, :], in1=gg[:, :], op=ALU.mult), [aa, gg], [aa])
                    k.a(lambda q: q.activation(out=aa[:, :], in_=aa[:, :], func=AF.Sigmoid, scale=1.5957691216), [aa], [aa])
                    k.v(lambda q: q.tensor_tensor(out=aa[:, :], in0=aa[:, :], in1=gg[:, :], op=ALU.mult), [aa, gg], [aa])
                    if g == NG // 2 and ti == 0:
                        dbg("h", rr[:, :], [128, G], rr)
                        dbg("gelu", aa[:, :], [128, G], aa)
                        dbg("b", bb[:, :], [128, G], bb)
                        dbg("uc", uc[:, :], [128, G], uc)
                        dbg("ub", ub[:, :], [128, 3 + G], ub)
                    k.v(lambda q, ti=ti: q.tensor_tensor(out=yl[:, ti, :], in0=aa[:, :], in1=rr[:, :], op=ALU.mult), [aa, rr], [yl])
                for li, (c0, M) in enumerate([(0, 96), (96, 96), (192, 128), (320, 128)]):
                    pp = psf.next()
                    proj(wlora, c0, M, pp)
                    k.v(lambda q, li=li, M=M: q.tensor_copy(out=lb[0:M, 0:1], in_=lhist[0:M, li:li + 1]), [lhist], [lb])
                    k.a(lambda q, pp=pp, M=M: q.activation(out=lb[0:M, 1:1 + G], in_=pp[0:M, :], func=AF.Copy), [pp], [lb])
                    k.v(lambda q, li=li, M=M: q.tensor_copy(out=lhist[0:M, li:li + 1], in_=lb[0:M, G:G + 1]), [lb], [lhist])
                    tt = lw[0]
                    k.v(lambda q, M=M: q.tensor_tensor(out=tt[0:M, :], in0=lb[0:M, 0:G], in1=lb[0:M, 1:1 + G], op=ALU.subtract), [lb], [tt])
                    k.v(lambda q, li=li, M=M: q.scalar_tensor_tensor(out=tt[0:M, :], in0=tt[0:M, :], scalar=mu_lora[0:M, li:li + 1], in1=lb[0:M, 1:1 + G], op0=ALU.mult, op1=ALU.add), [tt, mu_lora, lb], [tt])
                    fn = [AF.Tanh, AF.Copy, AF.Sigmoid, AF.Sigmoid][li]
                    k.a(lambda q, li=li, M=M, fn=fn: q.activation(out=lact[0:M, li, :], in_=tt[0:M, :], func=fn), [tt], [lact])
            k.barrier()
            if mode == "s1":
                return early_stop()
            with ExitStack() as sA:
                k.es = sA
                wlr = k.ring("wrk", [128, FC, 192], BF16, 2)
                dupr = k.ring("dupr", [96, HS], F32, 2)
                aupr = k.ring("aupr", [96, HS], F32, 2)
                gupr = k.ring("gupr", [128, 2, HS], F32, 2)
                pb = k.sb("pb", [HS, 3, 1 + G])
                hw = [k.sb("hw%d" % i, [HS, G]) for i in range(17)]
                (r_, k_, v_, kk_, a_, g_, lgw, L_, eL, Rp, Kpp, b_, Ke, Be, t0, t1, ysb) = hw
                vtm = k.sb("vtm", [HS, 8, HS])
                ketm = k.sb("ketm", [HS, 8, HS])
                betm = k.sb("betm", [HS, 8, HS])
                P = [k.sb("P%d" % i, [HS, 8, HS]) for i in range(2)]
                Q = [k.sb("Q%d" % i, [HS, 8, HS]) for i in range(2)]
                PI = k.sb("PI", [HS, 8, HS])
                Z = [k.sb("Z%d" % i, [HS, 8, HS]) for i in range(2)]
                MT = k.sb("MT", [HS, 8, HS])
                GT = k.sb("GT", [HS, 8, HS])
                HT = k.sb("HT", [HS, 8, HS])
                Xsb = k.sb("Xsb", [HS, HS])
                NU = k.sb("NU", [HS, HS])
                for h in range(NH):
                    wt = wlr.next()
                    k.dma("pool", lambda q, wt=wt, h=h: q.dma_start(out=wt[:, :, :], in_=win_rkv[h]), [], [wt])
                    du, au, gu = dupr.next(), aupr.next(), gupr.next()
                    k.dma("sp", lambda q, du=du, h=h: q.dma_start(out=du[:, :], in_=dup_d[h]), [], [du])
                    k.dma("sp", lambda q, au=au, h=h: q.dma_start(out=au[:, :], in_=aup_d[h]), [], [au])
                    k.dma("sp", lambda q, gu=gu, h=h: q.dma_start(out=gu[:, :, :], in_=gup_d[h]), [], [gu])
                    k.v(lambda q, h=h: q.tensor_copy(out=pb[:, :, 0], in_=hist[:, h, :]), [hist], [pb])
                    for wi in range(3):
                        pp = psf.next()
                        proj(wt, wi * HS, HS, pp)
                        k.a(lambda q, pp=pp, wi=wi: q.activation(out=pb[:, wi, 1:1 + G], in_=pp[0:HS, :], func=AF.Copy), [pp], [pb])
                    k.v(lambda q, h=h: q.tensor_copy(out=hist[:, h, :], in_=pb[:, :, G]), [pb], [hist])
                    for wi, dst in enumerate([r_, k_, v_]):
                        k.v(lambda q, wi=wi: q.tensor_tensor(out=t0[:, :], in0=pb[:, wi, 0:G], in1=pb[:, wi, 1:1 + G], op=ALU.subtract), [pb], [t0])
                        k.v(lambda q, wi=wi, dst=dst, h=h: q.scalar_tensor_tensor(out=dst[:, :], in0=t0[:, :], scalar=rw_cp[:, h, wi:wi + 1], in1=pb[:, wi, 1:1 + G], op0=ALU.mult, op1=ALU.add), [t0, rw_cp, pb], [dst])
                    if mode == "p1" and h == 0:
                        sA.close()
                        return early_stop()
                    pp = psf.next()
                    k.pe(lambda q, pp=pp, du=du: q.matmul(pp[0:HS, :], lhsT=du[:, :], rhs=lact[0:96, 0, :], start=True, stop=True), [du, lact], [pp])
                    k.a(lambda q, pp=pp, h=h: q.activation(out=lgw[:, :], in_=pp[0:HS, :], func=AF.Sigmoid, bias=rw_cp[:, h, 3:4]), [pp, rw_cp], [lgw])
                    k.v(lambda q: q.tensor_scalar(out=lgw[:, :], in0=lgw[:, :], scalar1=-0.6065306597126334, scalar2=None, op0=ALU.mult), [lgw], [lgw])
                    pp = psf.next()
                    k.pe(lambda q, pp=pp, au=au: q.matmul(pp[0:HS, :], lhsT=au[:, :], rhs=lact[0:96, 1, :], start=True, stop=True), [au, lact], [pp])
                    k.a(lambda q, pp=pp, h=h: q.activation(out=a_[:, :], in_=pp[0:HS, :], func=AF.Sigmoid, bias=rw_cp[:, h, 4:5]), [pp, rw_cp], [a_])
                    pp = psf.next()
                    for j in range(2):
                        k.pe(lambda q, pp=pp, j=j, gu=gu: q.matmul(pp[0:HS, :], lhsT=gu[:, j, :], rhs=lact[:, 2 + j, :], start=(j == 0), stop=(j == 1)), [gu, lact], [pp])
                    k.a(lambda q, pp=pp: q.activation(out=g_[:, :], in_=pp[0:HS, :], func=AF.Copy), [pp], [g_])
                    if mode == "p2" and h == 0:
                        sA.close()
                        return early_stop()
                    k.v(lambda q, h=h: q.tensor_scalar(out=kk_[:, :], in0=k_[:, :], scalar1=rw_cp[:, h, 5:6], scalar2=None, op0=ALU.mult), [k_, rw_cp], [kk_])
                    k.v(lambda q: q.tensor_tensor(out=t0[:, :], in0=kk_[:, :], in1=kk_[:, :], op=ALU.mult), [kk_], [t0])
                    pp = psf.next()
                    k.pe(lambda q, pp=pp: q.matmul(pp[0:HS, :], lhsT=ones64, rhs=t0[:, :], start=True, stop=True), [cst, t0], [pp])
                    k.v(lambda q, pp=pp: q.tensor_scalar(out=t1[:, :], in0=pp[0:HS, :], scalar1=1e-24, scalar2=None, op0=ALU.max), [pp], [t1])
                    k.a(lambda q: q.activation(out=t1[:, :], in_=t1[:, :], func=AF.Sqrt), [t1], [t1])
                    k.v(lambda q: q.reciprocal(out=t1[:, :], in_=t1[:, :]), [t1], [t1])
                    k.v(lambda q: q.tensor_tensor(out=kk_[:, :], in0=kk_[:, :], in1=t1[:, :], op=ALU.mult), [kk_, t1], [kk_])
                    k.v(lambda q, h=h: q.tensor_scalar(out=t0[:, :], in0=a_[:, :], scalar1=1.0, scalar2=rw_cp[:, h, 6:7], op0=ALU.subtract, op1=ALU.mult), [a_, rw_cp], [t0])
                    k.v(lambda q: q.scalar_tensor_tensor(out=k_[:, :], in0=t0[:, :], scalar=1.0, in1=k_[:, :], op0=ALU.add, op1=ALU.mult), [t0, k_], [k_])
                    k.v(lambda q: q.tensor_tensor(out=b_[:, :], in0=kk_[:, :], in1=a_[:, :], op=ALU.mult), [kk_, a_], [b_])
                    if mode == "p3" and h == 0:
                        sA.close()
                        return early_stop()
                    for c in range(8):
                        cs = slice(c * HS, (c + 1) * HS)
                        k.v(lambda q, cs=cs: q.tensor_tensor_scan(out=L_[:, cs], data0=ones64, data1=lgw[:, cs], initial=0.0, op0=ALU.mult, op1=ALU.add), [cst, lgw], [L_])
                    k.a(lambda q: q.activation(out=eL[:, :], in_=L_[:, :], func=AF.Exp), [L_], [eL])
                    k.v(lambda q: q.tensor_tensor(out=Rp[:, :], in0=r_[:, :], in1=eL[:, :], op=ALU.mult), [r_, eL], [Rp])
                    k.v(lambda q: q.tensor_tensor(out=t0[:, :], in0=L_[:, :], in1=lgw[:, :], op=ALU.subtract), [L_, lgw], [t0])
                    k.a(lambda q: q.activation(out=a_[:, :], in_=t0[:, :], func=AF.Exp), [t0], [a_])
                    k.v(lambda q: q.tensor_tensor(out=kk_[:, :], in0=kk_[:, :], in1=a_[:, :], op=ALU.mult), [kk_, a_], [kk_])
                    KKp = kk_
                    for c in range(8):
                        cs = slice(c * HS, (c + 1) * HS)
                        k.a(lambda q, cs=cs, c=c: q.activation(out=t1[:, cs], in_=L_[:, cs], func=AF.Exp, scale=-1.0, bias=L_[:, c * HS + HS - 1:c * HS + HS]), [L_], [t1])
                    k.v(lambda q: q.tensor_tensor(out=Ke[:, :], in0=k_[:, :], in1=t1[:, :], op=ALU.mult), [k_, t1], [Ke])
                    k.v(lambda q: q.tensor_tensor(out=Be[:, :], in0=b_[:, :], in1=t1[:, :], op=ALU.mult), [b_, t1], [Be])
                    k.a(lambda q: q.activation(out=a_[:, :], in_=L_[:, :], func=AF.Exp, scale=-1.0), [L_], [a_])
                    k.v(lambda q: q.tensor_tensor(out=Kpp[:, :], in0=k_[:, :], in1=a_[:, :], op=ALU.mult), [k_, a_], [Kpp])
                    k.v(lambda q: q.tensor_tensor(out=b_[:, :], in0=b_[:, :], in1=a_[:, :], op=ALU.mult), [b_, a_], [b_])
                    Bpp = b_
                    if mode == "p4" and h == 0:
                        sA.close()
                        return early_stop()
                    for src, dst in ((v_, vtm), (Ke, ketm), (Be, betm)):
                        pp = psf.next()
                        for c in range(8):
                            cs = slice(c * HS, (c + 1) * HS)
                            k.pe(lambda q, pp=pp, src=src, cs=cs: q.transpose(out=pp[0:HS, cs], in_=src[:, cs], identity=i64), [src, cst], [pp])
                        k.a(lambda q, pp=pp, dst=dst: q.activation(out=dst[:, :, :], in_=pp[0:HS, :].rearrange("p (a b) -> p a b", a=8), func=AF.Copy), [pp], [dst])

                    if mode == "p5" and h == 0:
                        sA.close()
                        return early_stop()
                    def cmat(lh, rh, mask, sign, dst):
                        pp = psf.next()
                        for c in range(8):
                            cs = slice(c * HS, (c + 1) * HS)
                            k.pe(lambda q, pp=pp, cs=cs: q.matmul(pp[0:HS, cs], lhsT=lh[:, cs], rhs=rh[:, cs], start=True, stop=True), [lh, rh], [pp])
                        for c in range(8):
                            cs = slice(c * HS, (c + 1) * HS)
                            k.v(lambda q, pp=pp, cs=cs, c=c: q.scalar_tensor_tensor(out=dst[:, c, :], in0=pp[0:HS, cs], scalar=float(sign), in1=mask, op0=ALU.mult, op1=ALU.mult), [pp, cst], [dst])

                    cmat(KKp, Bpp, m_sl, -1.0, P[0])
                    cmat(Bpp, KKp, m_su, -1.0, Q[0])
                    cmat(Kpp, KKp, m_su, 1.0, MT)
                    cmat(Kpp, Rp, m_ui, 1.0, GT)
                    cmat(Bpp, Rp, m_ui, 1.0, HT)
                    for c in range(8):
                        k.v(lambda q, c=c: q.tensor_tensor(out=Z[0][:, c, :], in0=Q[0][:, c, :], in1=i64, op=ALU.add), [Q[0], cst], [Z[0]])
                    if mode == "p6" and h == 0:
                        sA.close()
                        return early_stop()
                    cur = 0
                    for lvl in range(5):
                        nxt = cur ^ 1
                        ppP = psf.next()
                        for c in range(8):
                            cs = slice(c * HS, (c + 1) * HS)
                            k.pe(lambda q, ppP=ppP, cs=cs, c=c, cur=cur: q.matmul(ppP[0:HS, cs], lhsT=Q[cur][:, c, :], rhs=P[cur][:, c, :], start=True, stop=True), [Q[cur], P[cur]], [ppP])
                        if mode == "d%d0" % lvl and h == 0:
                            sA.close()
                            return early_stop()
                        if lvl < 4:
                            ppQ = psf.next()
                            for c in range(8):
                                cs = slice(c * HS, (c + 1) * HS)
                                k.pe(lambda q, ppQ=ppQ, cs=cs, c=c, cur=cur: q.matmul(ppQ[0:HS, cs], lhsT=P[cur][:, c, :], rhs=Q[cur][:, c, :], start=True, stop=True), [Q[cur], P[cur]], [ppQ])
                            k.v(lambda q, ppP=ppP, nxt=nxt: q.tensor_copy(out=P[nxt][:, :, :], in_=ppP[0:HS, :].rearrange("p (a b) -> p a b", a=8)), [ppP], [P[nxt]])
                            k.v(lambda q, ppQ=ppQ, nxt=nxt: q.tensor_copy(out=Q[nxt][:, :, :], in_=ppQ[0:HS, :].rearrange("p (a b) -> p a b", a=8)), [ppQ], [Q[nxt]])
                        if mode == "d%d1" % lvl and h == 0:
                            sA.close()
                            return early_stop()
                        for c in range(8):
                            cs = slice(c * HS, (c + 1) * HS)
                            k.v(lambda q, ppP=ppP, cs=cs, c=c: q.tensor_tensor(out=PI[:, c, :], in0=ppP[0:HS, cs], in1=i64, op=ALU.add), [ppP, cst], [PI])
                        if mode == "d%d2" % lvl and h == 0:
                            sA.close()
                            return early_stop()
                        zi, zo = lvl % 2, (lvl + 1) % 2
                        ppZ = psf.next()
                        for c in range(8):
                            cs = slice(c * HS, (c + 1) * HS)
                            k.pe(lambda q, ppZ=ppZ, cs=cs, c=c, zi=zi: q.matmul(ppZ[0:HS, cs], lhsT=PI[:, c, :], rhs=Z[zi][:, c, :], start=True, stop=True), [PI, Z[zi]], [ppZ])
                        k.v(lambda q, ppZ=ppZ, zo=zo: q.tensor_copy(out=Z[zo][:, :, :], in_=ppZ[0:HS, :].rearrange("p (a b) -> p a b", a=8)), [ppZ], [Z[zo]])
                        cur = nxt
                        if mode == "d%d3" % lvl and h == 0:
                            sA.close()
                            return early_stop()
                    if mode == "p7" and h == 0:
                        sA.close()
                        return early_stop()
                    ZF = Z[1]
                    py = pyb
                    for c in range(8):
                        cs = slice(c * HS, (c + 1) * HS)
                        px = psf.next()
                        k.pe(lambda q, px=px, cs=cs, h=h: q.matmul(px[0:HS, 0:HS], lhsT=KKp[:, cs], rhs=S[:, h, :], start=True, stop=False), [KKp, S], [px])
                        k.pe(lambda q, px=px, c=c: q.matmul(px[0:HS, 0:HS], lhsT=MT[:, c, :], rhs=vtm[:, c, :], start=False, stop=True), [MT, vtm], [px])
                        k.a(lambda q, px=px: q.activation(out=Xsb[:, :], in_=px[0:HS, 0:HS], func=AF.Copy), [px], [Xsb])
                        pu_ = psf.next()
                        k.pe(lambda q, pu_=pu_, c=c: q.matmul(pu_[0:HS, 0:HS], lhsT=ZF[:, c, :], rhs=Xsb[:, :], start=True, stop=True), [ZF, Xsb], [pu_])
                        k.a(lambda q, pu_=pu_: q.activation(out=NU[:, :], in_=pu_[0:HS, 0:HS], func=AF.Copy, scale=-1.0), [pu_], [NU])
                        k.pe(lambda q, cs=cs, h=h: q.matmul(py[0:HS, cs], lhsT=S[:, h, :], rhs=Rp[:, cs], start=True, stop=False), [S, Rp], [py])
                        k.pe(lambda q, cs=cs, c=c: q.matmul(py[0:HS, cs], lhsT=vtm[:, c, :], rhs=GT[:, c, :], start=False, stop=False), [vtm, GT], [py])
                        k.pe(lambda q, cs=cs, c=c: q.matmul(py[0:HS, cs], lhsT=NU[:, :], rhs=HT[:, c, :], start=False, stop=True), [NU, HT], [py])
                        pS = psf.next()
                        k.pe(lambda q, pS=pS, c=c: q.matmul(pS[0:HS, 0:HS], lhsT=ketm[:, c, :], rhs=vtm[:, c, :], start=True, stop=False), [ketm, vtm], [pS])
                        k.pe(lambda q, pS=pS, c=c: q.matmul(pS[0:HS, 0:HS], lhsT=betm[:, c, :], rhs=NU[:, :], start=False, stop=True), [betm, NU], [pS])
                        k.v(lambda q, pS=pS, c=c, h=h: q.scalar_tensor_tensor(out=S[:, h, :], in0=S[:, h, :], scalar=eL[:, c * HS + HS - 1:c * HS + HS], in1=pS[0:HS, 0:HS], op0=ALU.mult, op1=ALU.add), [S, eL, pS], [S])
                    k.a(lambda q: q.activation(out=ysb[:, :], in_=py[0:HS, :], func=AF.Copy), [py], [ysb])
                    if mode == "p8" and h == 0:
                        sA.close()
                        return early_stop()
                    pp = psf.next()
                    k.pe(lambda q, pp=pp: q.matmul(pp[0:HS, :], lhsT=ones64, rhs=ysb[:, :], start=True, stop=True), [cst, ysb], [pp])
                    k.v(lambda q, pp=pp: q.scalar_tensor_tensor(out=ysb[:, :], in0=pp[0:HS, :], scalar=-1.0 / HS, in1=ysb[:, :], op0=ALU.mult, op1=ALU.add), [pp, ysb], [ysb])
                    k.v(lambda q: q.tensor_tensor(out=t0[:, :], in0=ysb[:, :], in1=ysb[:, :], op=ALU.mult), [ysb], [t0])
                    pp = psf.next()
                    k.pe(lambda q, pp=pp: q.matmul(pp[0:HS, :], lhsT=ones64, rhs=t0[:, :], start=True, stop=True), [cst, t0], [pp])
                    k.v(lambda q, pp=pp: q.tensor_scalar(out=t1[:, :], in0=pp[0:HS, :], scalar1=1.0 / HS, scalar2=GN_EPS, op0=ALU.mult, op1=ALU.add), [pp], [t1])
                    k.a(lambda q: q.activation(out=t1[:, :], in_=t1[:, :], func=AF.Sqrt), [t1], [t1])
                    k.v(lambda q: q.reciprocal(out=t1[:, :], in_=t1[:, :]), [t1], [t1])
                    k.v(lambda q: q.tensor_tensor(out=ysb[:, :], in0=ysb[:, :], in1=t1[:, :], op=ALU.mult), [ysb, t1], [ysb])
                    k.v(lambda q, h=h: q.tensor_scalar(out=ysb[:, :], in0=ysb[:, :], scalar1=rw_cp[:, h, 8:9], scalar2=rw_cp[:, h, 9:10], op0=ALU.mult, op1=ALU.add), [ysb, rw_cp], [ysb])
                    k.v(lambda q, h=h: q.scalar_tensor_tensor(out=t0[:, :], in0=r_[:, :], scalar=rw_cp[:, h, 7:8], in1=k_[:, :], op0=ALU.mult, op1=ALU.mult), [r_, rw_cp, k_], [t0])
                    pp = psf.next()
                    k.pe(lambda q, pp=pp: q.matmul(pp[0:HS, :], lhsT=ones64, rhs=t0[:, :], start=True, stop=True), [cst, t0], [pp])
                    k.v(lambda q, pp=pp: q.tensor_tensor(out=t1[:, :], in0=pp[0:HS, :], in1=v_[:, :], op=ALU.mult), [pp, v_], [t1])
                    k.v(lambda q: q.tensor_tensor(out=ysb[:, :], in0=ysb[:, :], in1=t1[:, :], op=ALU.add), [ysb, t1], [ysb])
                    k.v(lambda q, h=h: q.tensor_tensor(out=yr[:, h, :], in0=ysb[:, :], in1=g_[:, :], op=ALU.mult), [ysb, g_], [yr])
            k.barrier()
            if mode == "sA":
                return early_stop()
            if g < NG // 2:
                continue
            if mode == "mixer" and DEBUG and g == NG // 2:
                dbb = Buf(dbg_yl)
                k.dma("sp", lambda q: q.dma_start(out=dbg_yl, in_=yl[:, :, :]), [yl], [dbb])
                k.dma("sp", lambda q: q.dma_start(out=dbg_yr, in_=yr[:, :, :]), [yr], [dbb])
                k.dma("sp", lambda q: q.dma_start(out=dbg_hT, in_=hT[:, :, :]), [hT], [dbb])
            with ExitStack() as sB:
                k.es = sB
                lnin = k.sb("lninB", [128, 2, D])
                k.dma("sp", lambda q: q.dma_start(out=lnin[:, :, :], in_=lnrep[0:2].rearrange("a p d -> p a d")), [], [lnin])
                ln1 = k.sb("ln1", [128, 2, D])
                k.dma("sp", lambda q: q.dma_start(out=ln1[:, :, :], in_=lnrep[2:4].rearrange("a p d -> p a d")), [], [ln1])
                lntmp = k.sb("lntmpB", [128, D])
                zb = [k.sb("zb%d" % i, [128, D]) for i in range(4)]
                wol = k.sb("wol", [128, 8, 512], BF16)
                wor = k.sb("wor", [HS, NH, 512], BF16)
                h1T = k.sb("h1T", [128, FC, 128])
                for ti in range(4):
                    r0 = t0g + ti * 128
                    k.dma("sp", lambda q, ti=ti, r0=r0: q.dma_start(out=zb[ti][:, :], in_=xa[r0:r0 + 128, :]), [], [zb[ti]])
                    layer_norm(zb[ti][:, :], zb[ti], zb[ti][:, :], zb[ti], lnin, lnin, 0, ALPHA, lntmp, st, mv)
                for n in range(4):
                    k.dma("pool", lambda q, n=n: q.dma_start(out=wol[:, :, :], in_=wout_l[n]), [], [wol])
                    k.dma("pool", lambda q, n=n: q.dma_start(out=wor[:, :, :], in_=wout_r[n]), [], [wor])
                    for ti in range(4):
                        ts_ = slice(ti * 128, (ti + 1) * 128)
                        pp = psf.next()
                        for j in range(8):
                            k.pe(lambda q, pp=pp, j=j, ts_=ts_: q.matmul(pp[:, :], lhsT=yl[:, j, ts_], rhs=wol[:, j, :], start=(j == 0), stop=False), [yl, wol], [pp])
                        for j in range(NH):
                            k.pe(lambda q, pp=pp, j=j, ts_=ts_: q.matmul(pp[:, :], lhsT=yr[:, j, ts_], rhs=wor[:, j, :], start=False, stop=(j == NH - 1)), [yr, wor], [pp])
                        k.v(lambda q, pp=pp, ti=ti, n=n: q.tensor_tensor(out=zb[ti][:, n * 512:(n + 1) * 512], in0=zb[ti][:, n * 512:(n + 1) * 512], in1=pp[:, :], op=ALU.add), [zb[ti], pp], [zb[ti]])
                for ti in range(4):
                    tg = (g - NG // 2) * 4 + ti
                    h1 = zb[ti]
                    layer_norm(h1[:, :], h1, h1[:, :], h1, ln1, ln1, 0, 1.0, lntmp, st, mv)
                    k.dma("sp", lambda q, tg=tg, h1=h1: q.dma_start(out=H1[tg * 128:(tg + 1) * 128, :], in_=h1[:, :]), [h1], [H1b])
                    for c4 in range(4):
                        pp = psf.next()
                        for j in range(4):
                            kc = c4 * 4 + j
                            k.pe(lambda q, pp=pp, kc=kc, j=j, h1=h1: q.transpose(out=pp[:, j * 128:(j + 1) * 128], in_=h1[:, kc * 128:(kc + 1) * 128], identity=ident), [h1, cst], [pp])
                        k.a(lambda q, pp=pp, c4=c4: q.activation(out=h1T[:, c4 * 4:(c4 + 1) * 4, :], in_=pp[:, :].rearrange("p (a b) -> p a b", a=4), func=AF.Copy), [pp], [h1T])
                    pp = psf.next()
                    for kc in range(FC):
                        k.pe(lambda q, pp=pp, kc=kc: q.matmul(pp[:, 0:NE], lhsT=h1T[:, kc, :], rhs=wr[:, kc, :], start=(kc == 0), stop=(kc == FC - 1)), [h1T, wr], [pp])
                    k.v(lambda q, pp=pp: q.tensor_tensor(out=lg[:, :], in0=pp[:, 0:NE], in1=brt[:, :], op=ALU.add), [pp, brt], [lg])
                    k.v(lambda q: q.max(out=m8[:, :], in_=lg[:, :]), [lg], [m8])
                    k.v(lambda q: q.max_index(out=i8[:, :], in_max=m8[:, :], in_values=lg[:, :]), [m8, lg], [i8])
                    k.v(lambda q, tg=tg: q.tensor_copy(out=eidf[:, tg, :], in_=i8[:, :]), [i8], [eidf])
                    k.v(lambda q: q.tensor_scalar(out=sm[:, 0:1], in0=m8[:, 0:1], scalar1=-1.0, scalar2=None, op0=ALU.mult), [m8], [sm])
                    k.a(lambda q: q.activation(out=sm[:, 4:8], in_=m8[:, 0:4], func=AF.Exp, bias=sm[:, 0:1]), [m8, sm], [sm])
                    k.v(lambda q: q.reduce_sum(out=sm[:, 1:2], in_=sm[:, 4:8], axis=AX.X), [sm], [sm])
                    k.v(lambda q: q.reciprocal(out=sm[:, 2:3], in_=sm[:, 1:2]), [sm], [sm])
                    k.v(lambda q, tg=tg: q.tensor_scalar(out=gates[:, tg, :], in0=sm[:, 4:8], scalar1=sm[:, 2:3], scalar2=None, op0=ALU.mult), [sm], [gates])
                    for kk in range(TOPK):
                        k.v(lambda q, tg=tg, kk=kk: q.tensor_scalar(out=ohall[:, tg * TOPK + kk, :], in0=iota_e, scalar1=eidf[:, tg, kk:kk + 1], scalar2=None, op0=ALU.is_equal), [cst, eidf], [ohall])
                    k.v(lambda q, tg=tg: q.tensor_tensor(out=mall[:, tg, :], in0=ohall[:, tg * TOPK, :], in1=ohall[:, tg * TOPK + 1, :], op=ALU.add), [ohall], [mall])
                    for kk in range(2, TOPK):
                        k.v(lambda q, tg=tg, kk=kk: q.tensor_tensor(out=mall[:, tg, :], in0=mall[:, tg, :], in1=ohall[:, tg * TOPK + kk, :], op=ALU.add), [ohall, mall], [mall])
            k.barrier()
        with ExitStack() as sC:
            k.es = sC
            cntb = k.sb("cntb", [128, 4, NE])
            pp = psf.next()
            for tg in range(NTILE):
                k.pe(lambda q, pp=pp, tg=tg: q.matmul(pp[:, 0:NE], lhsT=ones, rhs=mall[:, tg, :], start=(tg == 0), stop=(tg == NTILE - 1)), [cst, mall], [pp])
            k.v(lambda q, pp=pp: q.tensor_copy(out=cntb[:, 3, :], in_=pp[:, 0:NE]), [pp], [cntb])
            k.v(lambda q: q.tensor_scalar(out=cntb[:, 0, :], in0=cntb[:, 3, :], scalar1=0.0, scalar2=None, op0=ALU.is_gt), [cntb], [cntb])
            for j_ in range(1, NTILE):
                k.v(lambda q, j_=j_: q.scalar_tensor_tensor(out=cntb[:, 0, :], in0=cntb[:, 3, :], scalar=float(128 * j_), in1=cntb[:, 0, :], op0=ALU.is_gt, op1=ALU.add), [cntb], [cntb])
            k.v(lambda q: q.tensor_scalar(out=cntb[:, 0, :], in0=cntb[:, 0, :], scalar1=128.0, scalar2=None, op0=ALU.mult), [cntb], [cntb])
            k.v(lambda q: q.tensor_tensor_scan(out=cntb[:, 1, :], data0=ones[:, 0:NE], data1=cntb[:, 0, :], initial=0.0, op0=ALU.mult, op1=ALU.add), [cst, cntb], [cntb])
            k.v(lambda q: q.tensor_tensor(out=cntb[:, 2, :], in0=cntb[:, 1, :], in1=cntb[:, 0, :], op=ALU.subtract), [cntb], [cntb])
            blkf = k.sb("blkf", [128, 2])
            blki = k.sb("blki", [128, 1], I32)
            k.v(lambda q: q.tensor_scalar(out=cntb[:, 3, :], in0=cntb[:, 1, :], scalar1=iota_p, scalar2=None, op0=ALU.is_le), [cntb, cst], [cntb])
            k.v(lambda q: q.reduce_sum(out=blkf[:, 0:1], in_=cntb[:, 3, :], axis=AX.X), [cntb], [blkf])
            k.v(lambda q: q.tensor_scalar(out=blkf[:, 1:2], in0=blkf[:, 0:1], scalar1=float(NE - 1), scalar2=None, op0=ALU.min), [blkf], [blkf])
            dg = k.sb("dg", [128, 128])
            ef = k.sb("ef", [128, 3, 128])
            tf = k.sb("tf", [128, 8, 128])
            pcol = k.sb("pcol", [128, 1])
            k.v(lambda q: q.tensor_scalar(out=pcol[:, :], in0=iota_p, scalar1=1.0 / 128.0, scalar2=None, op0=ALU.mult), [cst], [pcol])
            k.v(lambda q: q.tensor_scalar(out=dg[:, :], in0=ident, scalar1=blkf[:, 1:2], scalar2=None, op0=ALU.mult), [cst, blkf], [dg])
            ppe = psf.next()
            k.pe(lambda q: q.matmul(ppe[:, 0:128], lhsT=ones, rhs=dg[:, :], start=True, stop=True), [cst, dg], [ppe])
            k.v(lambda q: q.tensor_copy(out=ef[:, 0, :], in_=ppe[:, 0:128]), [ppe], [ef])
            k.v(lambda q: q.tensor_scalar(out=ef[:, 1, :], in0=ef[:, 0, :], scalar1=1024.0, scalar2=pcol[:, 0:1], op0=ALU.mult, op1=ALU.add), [ef, pcol], [ef])
            k.v(lambda q: q.tensor_scalar(out=ef[:, 2, :], in0=ef[:, 0, :], scalar1=512.0, scalar2=pcol[:, 0:1], op0=ALU.mult, op1=ALU.add), [ef, pcol], [ef])
            k.v(lambda q: q.tensor_copy(out=eidx_i[:, :], in_=ef[:, 0, :]), [ef], [eidx_i])
            for n_ in range(8):
                k.v(lambda q, n_=n_: q.tensor_scalar(out=tf[:, n_, :], in0=ef[:, 1, :], scalar1=float(n_ * 128), scalar2=None, op0=ALU.add), [ef], [tf])
            k.v(lambda q: q.tensor_copy(out=idx1_i[:, :, :], in_=tf[:, :, :]), [tf], [idx1_i])
            for n_ in range(4):
                k.v(lambda q, n_=n_: q.tensor_scalar(out=tf[:, n_, :], in0=ef[:, 2, :], scalar1=float(n_ * 128), scalar2=None, op0=ALU.add), [ef], [tf])
            k.v(lambda q: q.tensor_copy(out=idx2_i[:, :, :], in_=tf[:, 0:4, :]), [tf], [idx2_i])
            slotf = k.sb("slotf", [128, NTILE * TOPK])
            basef = k.sb("basef", [128, NE])
            prodf = k.sb("prodf", [128, NE])
            for tg in range(NTILE):
                pp = psf.next()
                for t2 in range(tg):
                    k.pe(lambda q, pp=pp, t2=t2: q.matmul(pp[:, 0:NE], lhsT=ones, rhs=mall[:, t2, :], start=(t2 == 0), stop=False), [cst, mall], [pp])
                k.pe(lambda q, pp=pp, tg=tg: q.matmul(pp[:, 0:NE], lhsT=tri, rhs=mall[:, tg, :], start=(tg == 0), stop=True), [cst, mall], [pp])
                k.v(lambda q, pp=pp: q.tensor_tensor(out=basef[:, :], in0=pp[:, 0:NE], in1=cntb[:, 2, :], op=ALU.add), [pp, cntb], [basef])
                for kk in range(TOPK):
                    j = tg * TOPK + kk
                    k.v(lambda q, j=j: q.tensor_tensor(out=prodf[:, :], in0=basef[:, :], in1=ohall[:, j, :], op=ALU.mult), [basef, ohall], [prodf])
                    k.v(lambda q, j=j: q.reduce_sum(out=slotf[:, j:j + 1], in_=prodf[:, :], axis=AX.X), [prodf], [slotf])
            k.v(lambda q: q.tensor_copy(out=slot_i[:, :, 0], in_=slotf[:, :]), [slotf], [slot_i])
            dbg("slotf", slotf[:, :], [128, NTILE * TOPK], slotf)
            dbg("cntb", cntb[:, :, :], [128, 4, NE], cntb)
            dbg("eidf", eidf[:, :, :], [128, NTILE, 8], eidf)
            dbg("gates", gates[:, :, :], [128, NTILE, TOPK], gates)
            dbg("sloti", slot_i[:, :, :], [128, NTILE * TOPK, 1], slot_i, I32)
            dbg("ef", ef[:, :, :], [128, 3, 128], ef)
            dbg("mall", mall[:, :, :], [128, NTILE, NE], mall)
        k.barrier()
        esM.close()
        if mode == "mixer":
            with ExitStack() as esX:
                k.es = esX
                tb = k.sb("tb", [128, D])
                for tg in range(NTILE):
                    k.dma("sp", lambda q, tg=tg: q.dma_start(out=tb[:, :], in_=H1[tg * 128:(tg + 1) * 128, :]), [H1b], [tb])
                    k.dma("sp", lambda q, tg=tg: q.dma_start(out=out_d[tg * 128:(tg + 1) * 128, :], in_=tb[:, :]), [tb], [OUTB])
                k.wait_all("sp", [OUTB])
            k.es = es
            print("instructions:", k.n_ins)
            return nc
        with ExitStack() as es3:
            k.es = es3
            hb = k.ring("hb", [128, D], BF16, 2)
            zt_ = hb.next()
            k.v(lambda q: q.memset(zt_[:, :], 0.0), [], [zt_])
            for s in range(NBLK):
                k.dma("sp", lambda q, s=s: q.dma_start(out=XS[s * 128:(s + 1) * 128, :], in_=zt_[:, :]), [zt_], [XSb])
            for tg in range(NTILE):
                t = hb.next()
                k.dma("pool", lambda q, t=t, tg=tg: q.dma_start(out=t[:, :], in_=H1[tg * 128:(tg + 1) * 128, :]), [H1b], [t])
                for kk in range(TOPK):
                    j = tg * TOPK + kk
                    k.dma("pool", lambda q, t=t, j=j: q.indirect_dma_start(out=XS, out_offset=bass.IndirectOffsetOnAxis(ap=slot_i[:, j, :], axis=0), in_=t[:, :], in_offset=None), [t, slot_i], [XSb])
            wbuf = k.ring("wbuf", [128, FC * 512], BF16, 3)
            b1f = k.sb("b1f", [128, 2 * DE])
            b2f = k.sb("b2f", [128, D])
            xb = k.ring("xb", [128, D], BF16, 2)
            xT = k.sb("xT", [128, FC, 128], BF16)
            act = k.sb("act", [128, DE], BF16)
            actT = k.sb("actT", [128, FC, 128], BF16)
            ob = k.ring("ob", [128, D], F32, 2)
            b1b = k.ring("b1b", [1, 2 * DE], BF16, 1)
            b2b = k.ring("b2b", [1, D], BF16, 1)
            gsb = k.ring("gsb", [128, 4, 512], F32, 2)
            s_g, s_u, s_s, s_p = 0, 1, 2, 3
            for s in range(NBLK):
                x_ = xb.next()
                k.dma("sp", lambda q, x_=x_, s=s: q.dma_start(out=x_[:, :], in_=XS[s * 128:(s + 1) * 128, :]), [XSb], [x_])
                b1 = b1b.next()
                b2 = b2b.next()
                k.dma("pool", lambda q, s=s: q.indirect_dma_start(out=b1f[:, :], out_offset=None, in_=b1_d, in_offset=bass.IndirectOffsetOnAxis(ap=eidx_i[:, s:s + 1], axis=0)), [eidx_i], [b1f])
                k.dma("pool", lambda q, s=s: q.indirect_dma_start(out=b2f[:, :], out_offset=None, in_=b2_d, in_offset=bass.IndirectOffsetOnAxis(ap=eidx_i[:, s:s + 1], axis=0)), [eidx_i], [b2f])
                k.a(lambda q, b1=b1: q.activation(out=b1[:, :], in_=b1f[0:1, :], func=AF.Copy), [b1f], [b1])
                k.a(lambda q, b2=b2: q.activation(out=b2[:, :], in_=b2f[0:1, :], func=AF.Copy), [b2f], [b2])
                for c4 in range(4):
                    hf = psb_half[0]
                    psb_half[0] ^= 1
                    for j in range(4):
                        kc = c4 * 4 + j
                        k.pe(lambda q, kc=kc, hf=hf, j=j, x_=x_: q.transpose(out=psb[:, hf * 512 + j * 128: hf * 512 + (j + 1) * 128], in_=x_[:, kc * 128:(kc + 1) * 128], identity=identb[:, :]), [x_, identb], [psb])
                    k.a(lambda q, c4=c4, hf=hf: q.activation(out=xT[:, c4 * 4:(c4 + 1) * 4, :], in_=psb[:, hf * 512:(hf + 1) * 512].rearrange("p (a b) -> p a b", a=4), func=AF.Copy), [psb], [xT])
                for n in range(4):
                    gs = gsb.next()
                    pgu = []
                    for half in range(2):
                        col0 = half * DE + n * 512
                        w = wbuf.next()
                        n8 = half * 4 + n
                        k.dma("pool", lambda q, w=w, n8=n8, s=s: q.indirect_dma_start(out=w[:, :], out_offset=None, in_=w1_d, in_offset=bass.IndirectOffsetOnAxis(ap=idx1_i[:, n8, s:s + 1], axis=0)), [idx1_i], [w])
                        pp = psf.next()
                        for kc in range(FC):
                            k.pe(lambda q, pp=pp, kc=kc, w=w: q.matmul(pp[:, :], lhsT=xT[:, kc, :], rhs=w[:, kc * 512:(kc + 1) * 512], start=(kc == 0), stop=False), [xT, w], [pp])
                        k.pe(lambda q, pp=pp, col0=col0, b1=b1: q.matmul(pp[:, :], lhsT=onesb[:, :], rhs=b1[:, col0:col0 + 512], start=False, stop=True), [onesb, b1], [pp])
                        pgu.append(pp)
                    k.v(lambda q, gs=gs, pp=pgu[0]: q.tensor_scalar(out=gs[:, s_g, :], in0=pp[:, :], scalar1=7.0, scalar2=None, op0=ALU.min), [pgu[0]], [gs])
                    k.a(lambda q, gs=gs: q.activation(out=gs[:, s_s, :], in_=gs[:, s_g, :], func=AF.Sigmoid, scale=1.702), [gs], [gs])
                    k.v(lambda q, gs=gs, pp=pgu[1]: q.tensor_scalar(out=gs[:, s_u, :], in0=pp[:, :], scalar1=7.0, scalar2=-7.0, op0=ALU.min, op1=ALU.max), [pgu[1]], [gs])
                    k.v(lambda q, gs=gs: q.tensor_tensor(out=gs[:, s_p, :], in0=gs[:, s_g, :], in1=gs[:, s_s, :], op=ALU.mult), [gs], [gs])
                    k.v(lambda q, gs=gs, n=n: q.scalar_tensor_tensor(out=act[:, n * 512:(n + 1) * 512], in0=gs[:, s_u, :], scalar=1.0, in1=gs[:, s_p, :], op0=ALU.add, op1=ALU.mult), [gs], [act])
                for c4 in range(4):
                    hf = psb_half[0]
                    psb_half[0] ^= 1
                    for j in range(4):
                        kc = c4 * 4 + j
                        k.pe(lambda q, kc=kc, hf=hf, j=j: q.transpose(out=psb[:, hf * 512 + j * 128: hf * 512 + (j + 1) * 128], in_=act[:, kc * 128:(kc + 1) * 128], identity=identb[:, :]), [act, identb], [psb])
                    k.a(lambda q, c4=c4, hf=hf: q.activation(out=actT[:, c4 * 4:(c4 + 1) * 4, :], in_=psb[:, hf * 512:(hf + 1) * 512].rearrange("p (a b) -> p a b", a=4), func=AF.Copy), [psb], [actT])
                o_ = ob.next()
                for n in range(4):
                    w = wbuf.next()
                    k.dma("pool", lambda q, w=w, n=n, s=s: q.indirect_dma_start(out=w[:, :], out_offset=None, in_=w2_d, in_offset=bass.IndirectOffsetOnAxis(ap=idx2_i[:, n, s:s + 1], axis=0)), [idx2_i], [w])
                    pp = psf.next()
                    for kc in range(FC):
                        k.pe(lambda q, pp=pp, kc=kc, w=w: q.matmul(pp[:, :], lhsT=actT[:, kc, :], rhs=w[:, kc * 512:(kc + 1) * 512], start=(kc == 0), stop=False), [actT, w], [pp])
                    k.pe(lambda q, pp=pp, n=n, b2=b2: q.matmul(pp[:, :], lhsT=onesb[:, :], rhs=b2[:, n * 512:(n + 1) * 512], start=False, stop=True), [onesb, b2], [pp])
                    k.a(lambda q, pp=pp, n=n, o_=o_: q.activation(out=o_[:, n * 512:(n + 1) * 512], in_=pp[:, :], func=AF.Copy), [pp], [o_])
                k.dma("sp", lambda q, o_=o_, s=s: q.dma_start(out=OS[s * 128:(s + 1) * 128, :], in_=o_[:, :]), [o_], [OSb])
        k.barrier()
        with ExitStack() as es4:
            k.es = es4
            lnp = k.sb("ln2", [128, 2, D])
            k.dma("sp", lambda q: q.dma_start(out=lnp[:, :, :], in_=lnrep[4:6].rearrange("a p d -> p a d")), [], [lnp])
            gth = k.ring("gth", [128, D], F32, 3)
            h1r = k.ring("h1r", [128, D], F32, 2)
            zt = k.sb("zt", [128, D])
            lntmp = k.sb("lntmp2", [128, D])
            st = k.sb("st2", [128, 24])
            mv = k.sb("mv2", [128, 4])
            outr = k.ring("outr", [128, D], F32, 2)
            for tg in range(NTILE):
                hh = h1r.next()
                k.dma("sp", lambda q, hh=hh, tg=tg: q.dma_start(out=hh[:, :], in_=H1[tg * 128:(tg + 1) * 128, :]), [H1b], [hh])
                k.v(lambda q, hh=hh: q.tensor_scalar(out=zt[:, :], in0=hh[:, :], scalar1=float(ALPHA), scalar2=None, op0=ALU.mult), [hh], [zt])
                for kk in range(TOPK):
                    j = tg * TOPK + kk
                    gt = gth.next()
                    k.dma("pool", lambda q, gt=gt, j=j: q.indirect_dma_start(out=gt[:, :], out_offset=None, in_=OS, in_offset=bass.IndirectOffsetOnAxis(ap=slot_i[:, j, :], axis=0)), [OSb, slot_i], [gt])
                    k.v(lambda q, gt=gt, tg=tg, kk=kk: q.scalar_tensor_tensor(out=zt[:, :], in0=gt[:, :], scalar=gates[:, tg, kk:kk + 1], in1=zt[:, :], op0=ALU.mult, op1=ALU.add), [gt, gates, zt], [zt])
                ot = outr.next()
                layer_norm(zt[:, :], zt, ot[:, :], ot, lnp, lnp, 0, 1.0, lntmp, st, mv)
                k.dma("sp", lambda q, ot=ot, tg=tg: q.dma_start(out=out_d[tg * 128:(tg + 1) * 128, :], in_=ot[:, :]), [ot], [OUTB])
            k.wait_all("sp", [OUTB])
        k.es = es
        print("instructions:", k.n_ins)
    return nc


def _consts(NE):
    c = np.zeros((128, 128 * 3 + 64 * 4 + NE + 1), np.float32)
    c[:, 0:128] = np.eye(128)
    c[:, 128:256] = 1.0
    c[:, 256:384] = np.triu(np.ones((128, 128)), 1)
    o = 384
    t = np.arange(64)
    c[0:64, o:o + 64] = (t[:, None] > t[None, :])
    c[0:64, o + 64:o + 128] = (t[None, :] > t[:, None])
    c[0:64, o + 128:o + 192] = (t[None, :] >= t[:, None])
    c[0:64, o + 192:o + 256] = np.eye(64)
    c[:, o + 256:o + 256 + NE] = np.arange(NE)[None, :]
    c[:, o + 256 + NE] = np.arange(128) * 128.0
    return c


def prepare_shared(p, NE):
    f = np.float32
    sh = {}
    ln = np.stack([p["ln_in_g"], p["ln_in_b"], p["ln1_g"][0], p["ln1_b"][0], p["ln2_g"][0], p["ln2_b"][0]])
    sh["lnrep"] = np.ascontiguousarray(np.broadcast_to(ln[:, None, :], (6, 128, D))).astype(f)
    w_in = p["w_in"][0]
    wl = w_in[:, 0:2048].reshape(FC, 128, 16, 128)
    sh["win_lru"] = np.ascontiguousarray(wl.transpose(2, 1, 0, 3))
    rkv = w_in[:, 2048:2048 + 3072].reshape(FC, 128, 3, NH, HS)
    sh["win_rkv"] = np.ascontiguousarray(rkv.transpose(3, 1, 0, 2, 4).reshape(NH, 128, FC, 3 * HS))
    lo = w_in[:, 2048 + 3072:].reshape(FC, 128, 448)
    sh["win_lora"] = np.ascontiguousarray(lo.transpose(1, 0, 2))
    cp = np.stack([p["conv_w"][0][0], p["conv_w"][0][1], p["conv_w"][0][2], p["conv_w"][0][3], p["conv_b"][0], p["b_rgate"][0], p["b_igate"][0], p["lru_lambda"][0]], -1)
    sh["lru_cp"] = np.ascontiguousarray(cp.reshape(8, 128, 8).transpose(1, 0, 2))

    def bd(w):
        o = np.zeros((8, 128, 128), f)
        for n in range(16):
            t, q = n // 2, (n % 2) * 64
            o[t, q:q + 64, q:q + 64] = w[n]
        return o
    sh["wrg_bd"] = bd(p["w_rgate"][0])
    sh["wig_bd"] = bd(p["w_igate"][0])
    mu = p["shift_mu"][0]
    hv = lambda v: v.reshape(NH, HS).T
    cols = [hv(mu[0:1024]), hv(mu[1024:2048]), hv(mu[2048:3072]), hv(p["w0"][0]), hv(p["a0"][0]), hv(p["k_k"][0]), hv(p["k_a"][0]), p["r_k"][0].T, hv(p["gn_g"][0]), hv(p["gn_b"][0])]
    sh["rw_cp"] = np.ascontiguousarray(np.stack(cols, -1)).astype(f)
    ml = np.zeros((128, 4), f)
    ml[0:96, 0] = mu[3072:3168]
    ml[0:96, 1] = mu[3168:3264]
    ml[:, 2] = mu[3264:3392]
    ml[:, 3] = mu[3392:3520]
    sh["mu_lora"] = ml
    sh["dec_up"] = np.ascontiguousarray(p["rw_decay_up"][0].reshape(96, NH, HS).transpose(1, 0, 2))
    sh["aaa_up"] = np.ascontiguousarray(p["rw_aaa_up"][0].reshape(96, NH, HS).transpose(1, 0, 2))
    sh["gate_up"] = np.ascontiguousarray(p["rw_gate_up"][0].reshape(2, 128, NH, HS).transpose(2, 1, 0, 3))
    wo = p["w_out"][0]
    sh["wout_l"] = np.ascontiguousarray(wo[0:1024].reshape(8, 128, 4, 512).transpose(2, 1, 0, 3))
    sh["wout_r"] = np.ascontiguousarray(wo[1024:2048].reshape(NH, HS, 4, 512).transpose(2, 1, 0, 3))
    sh["w_router"] = np.ascontiguousarray(p["w_router"][0].reshape(FC, 128, NE).transpose(1, 0, 2))
    sh["b_router"] = np.ascontiguousarray(np.broadcast_to(p["b_router"][0][None, :], (128, NE))).astype(f)
    sh["w_exp1"] = np.ascontiguousarray(p["w_exp1"][0].reshape(NE, FC, 128, 8, 512).transpose(0, 3, 2, 1, 4)).reshape(NE * 8 * 128, FC * 512)
    sh["b_exp1"] = np.ascontiguousarray(p["b_exp1"][0])
    sh["w_exp2"] = np.ascontiguousarray(p["w_exp2"][0].reshape(NE, FC, 128, 4, 512).transpose(0, 3, 2, 1, 4)).reshape(NE * 4 * 128, FC * 512)
    sh["b_exp2"] = np.ascontiguousarray(p["b_exp2"][0])
    sh["cst"] = _consts(NE)
    return sh


def run(inputs, TH, NE, n_cores, mode="full"):
    x = np.asarray(inputs["x"], np.float32)
    B, T, _ = x.shape
    assert T == 2 * TH and B * 2 == n_cores
    p = {kk: np.asarray(v, np.float32) for kk, v in inputs.items() if kk != "x"}
    sh = prepare_shared(p, NE)
    in_maps = []
    for c in range(n_cores):
        b, j = c // 2, c % 2
        first = x[b, 0:TH]
        second = x[b, j * TH:(j + 1) * TH]
        m = dict(sh)
        m["xa"] = np.ascontiguousarray(np.concatenate([first, second], 0))
        m["flag"] = np.full((128, 1), float(j), np.float32)
        in_maps.append(m)
    if mode != "full":
        for m in in_maps:
            m["w_exp1"] = np.zeros((128, FC * 512), np.float32)
            m["w_exp2"] = np.zeros((128, FC * 512), np.float32)
    nc = build(TH, NE, mode)
    res = run_bass_kernel_spmd(nc, in_maps, core_ids=list(range(n_cores)))
    out = np.zeros((B, T, D), np.float32)
    for c in range(n_cores):
        b, j = c // 2, c % 2
        out[b, j * TH:(j + 1) * TH] = res.results[c]["out"]
    return out


def kernel(**inputs):
    return run(inputs, 2048, 32, 8)
```

```python
from contextlib import ExitStack
import numpy as np
import concourse.bass as bass
import concourse.mybir as mybir
from concourse.bass_utils import run_bass_kernel_spmd

F32 = mybir.dt.float32
BF16 = mybir.dt.bfloat16
I32 = mybir.dt.int32
U32 = mybir.dt.uint32
AF = mybir.ActivationFunctionType
ALU = mybir.AluOpType
AX = mybir.AxisListType

D = 2048
FC = 16
LRU_W = 1024
NH = 16
HS = 64
G = 512
TOPK = 4
DE = 2048
ALPHA = 2.0 ** 0.25
LN_EPS = 1e-5
GN_EPS = 64 * 1e-5
EPOCH = 4000
NDS = 12
DEBUG = False


class Reg:
    __slots__ = ("w", "r")

    def __init__(self):
        self.w = None
        self.r = {}


class Buf:
    def __init__(self, t):
        self.t = t
        self.reg = Reg()

    def __getitem__(self, k):
        return self.t[k]


class Ring:
    def __init__(self, bufs):
        self.bufs = bufs
        self.i = 0

    def next(self):
        b = self.bufs[self.i]
        self.i = (self.i + 1) % len(self.bufs)
        return b


class KB:
    def __init__(self, nc, es):
        self.nc = nc
        self.es = es
        self.es_sem = es
        self.uid = 0
        self.eng = {"sp": nc.sync, "act": nc.scalar, "dve": nc.vector, "pool": nc.gpsimd, "pe": nc.tensor}
        self.sems = []
        self.esem = {}
        self.cnt = {}
        self.waited = {e: {} for e in self.eng}
        for e in self.eng:
            self._new_epoch(e)
        self.dsem = [self._sem("d%d" % i) for i in range(2 * NDS)]
        self.dval = [0] * (2 * NDS)
        self.drr = {"hw": 0, "sw": 0}
        self.n_ins = 0

    def _sem(self, name):
        s = self.es_sem.enter_context(self.nc.semaphore(name))
        self.sems.append(s)
        return len(self.sems) - 1

    def _new_epoch(self, e):
        self.esem[e] = self._sem("e_%s_%d" % (e, len(self.sems)))
        self.cnt[e] = 0

    def sb(self, name, shape, dt=F32):
        self.uid += 1
        return Buf(self.es.enter_context(self.nc.sbuf_tensor("%s_%d" % (name, self.uid), shape, dt)))

    def barrier(self):
        toks = [(self.esem[f], self.cnt[f]) for f in self.eng if self.cnt[f] > 0]
        toks += [(self.dsem[i], self.dval[i]) for i in range(2 * NDS) if self.dval[i] > 0]
        for e in self.eng:
            for (s_, v_) in toks:
                if self.waited[e].get(s_, 0) < v_:
                    self.eng[e].wait_ge(self.sems[s_], v_)
                    self.waited[e][s_] = v_
                    self.n_ins += 1

    def ring(self, name, shape, dt, n):
        return Ring([self.sb("%s%d" % (name, i), shape, dt) for i in range(n)])

    def _deps(self, e, reads, writes, extra=()):
        deps = list(extra)
        for r in reads:
            if r.reg.w is not None:
                deps.append(r.reg.w)
        for w in writes:
            if w.reg.w is not None:
                deps.append(w.reg.w)
            deps.extend(w.reg.r.items())
        for (s, v) in deps:
            if e == "pe" and s == self.esem["pe"]:
                continue
            if self.waited[e].get(s, 0) < v:
                self.eng[e].wait_ge(self.sems[s], v)
                self.waited[e][s] = v
                self.n_ins += 1

    def _record(self, tok, reads, writes):
        s, v = tok
        for r in reads:
            if r.reg.r.get(s, 0) < v:
                r.reg.r[s] = v
        for w in writes:
            w.reg.w = tok
            w.reg.r = {}

    def op(self, e, fn, reads=(), writes=()):
        self._deps(e, reads, writes)
        if self.cnt[e] >= EPOCH:
            self._new_epoch(e)
        ins = fn(self.eng[e])
        self.cnt[e] += 1
        s = self.esem[e]
        ins.then_inc(self.sems[s], 1)
        self.n_ins += 1
        self._record((s, self.cnt[e]), reads, writes)

    def dma(self, e, fn, reads=(), writes=()):
        kind = "sw" if e == "pool" else "hw"
        k = self.drr[kind] + (NDS if kind == "sw" else 0)
        self.drr[kind] = (self.drr[kind] + 1) % NDS
        extra = [(self.dsem[k], self.dval[k])] if self.dval[k] > 0 else []
        self._deps(e, reads, writes, extra)
        ins = fn(self.eng[e])
        self.dval[k] += 16
        ins.then_inc(self.sems[self.dsem[k]], 16)
        self.n_ins += 1
        self._record((self.dsem[k], self.dval[k]), reads, writes)

    def wait_all(self, e, bufs):
        self._deps(e, bufs, bufs)

    def v(self, fn, r=(), w=()):
        self.op("dve", fn, r, w)

    def a(self, fn, r=(), w=()):
        self.op("act", fn, r, w)

    def pe(self, fn, r=(), w=()):
        self.op("pe", fn, r, w)


def build(TH, NE, mode="full"):
    NT = 2 * TH
    NG = NT // G
    NTILE = TH // 128
    NBLK = TH * TOPK // 128 + NE
    nc = bass.Bass("TRN2", target_bir_lowering=False)

    def din(name, shape, dt=F32):
        return nc.dram_tensor(name, list(shape), dt, kind="ExternalInput").ap()

    xa = din("xa", [NT, D])
    flag_d = din("flag", [128, 1])
    lnrep = din("lnrep", [6, 128, D])
    win_lru = din("win_lru", [16, 128, FC, 128])
    win_rkv = din("win_rkv", [NH, 128, FC, 3 * HS])
    win_lora = din("win_lora", [128, FC, 448])
    lru_cp_d = din("lru_cp", [128, 8, 8])
    wrg_d = din("wrg_bd", [8, 128, 128])
    wig_d = din("wig_bd", [8, 128, 128])
    rw_cp_d = din("rw_cp", [HS, NH, 10])
    mu_lora_d = din("mu_lora", [128, 4])
    dup_d = din("dec_up", [NH, 96, HS])
    aup_d = din("aaa_up", [NH, 96, HS])
    gup_d = din("gate_up", [NH, 128, 2, HS])
    wout_l = din("wout_l", [4, 128, 8, 512])
    wout_r = din("wout_r", [4, HS, NH, 512])
    wr_d = din("w_router", [128, FC, NE])
    br_d = din("b_router", [128, NE])
    w1_d = din("w_exp1", [NE * 8 * 128 if mode == "full" else 128, FC * 512])
    b1_d = din("b_exp1", [NE, 2 * DE])
    w2_d = din("w_exp2", [NE * 4 * 128 if mode == "full" else 128, FC * 512])
    b2_d = din("b_exp2", [NE, D])
    cst_d = din("cst", [128, 128 * 3 + 64 * 4 + NE + 1])
    out_d = nc.dram_tensor("out", [TH, D], F32, kind="ExternalOutput").ap()
    if mode == "mixer" and DEBUG:
        dbg_yl = nc.dram_tensor("dbg_yl", [128, 8, G], BF16, kind="ExternalOutput").ap()
        dbg_yr = nc.dram_tensor("dbg_yr", [HS, NH, G], BF16, kind="ExternalOutput").ap()
        dbg_hT = nc.dram_tensor("dbg_hT", [128, FC, G], BF16, kind="ExternalOutput").ap()
    H1 = nc.dram_tensor("H1", [TH, D], F32, kind="Internal").ap()
    XS = nc.dram_tensor("XS", [NBLK * 128, D], BF16, kind="Internal").ap()
    OS = nc.dram_tensor("OS", [NBLK * 128, D], F32, kind="Internal").ap()
    BLKE = nc.dram_tensor("BLKE", [128, 1], I32, kind="Internal").ap()

    with ExitStack() as es:
        k = KB(nc, es)
        dbg_list = []

        def dbg(name, ap, shape, buf, dt=F32):
            if not DEBUG:
                return
            t_ = nc.dram_tensor("dbg_" + name, list(shape), dt, kind="ExternalOutput").ap()
            b_ = Buf(t_)
            dbg_list.append(b_)
            k.dma("sp", lambda q: q.dma_start(out=t_, in_=ap), [buf], [b_])

        H1b, XSb, OSb, BLKEb, OUTB = Buf(H1), Buf(XS), Buf(OS), Buf(BLKE), Buf(out_d)
        psf = Ring([Buf(es.enter_context(nc.psum_tensor("psf%d" % i, [128, 512], F32))) for i in range(6)])
        pyb = Buf(es.enter_context(nc.psum_tensor("pyb", [128, 512], F32)))
        psb = Buf(es.enter_context(nc.psum_tensor("psb", [128, 1024], BF16)))
        psb_half = [0]
        NC_C = 128 * 3 + 64 * 4 + NE + 1
        cst = k.sb("cst", [128, NC_C])
        k.dma("sp", lambda q: q.dma_start(out=cst[:, :], in_=cst_d), [], [cst])
        ident = cst[:, 0:128]
        ones = cst[:, 128:256]
        tri = cst[:, 256:384]
        o = 384
        m_sl = cst[0:64, o:o + 64]
        m_su = cst[0:64, o + 64:o + 128]
        m_ui = cst[0:64, o + 128:o + 192]
        i64 = cst[0:64, o + 192:o + 256]
        iota_e = cst[:, o + 256:o + 256 + NE]
        iota_p = cst[:, o + 256 + NE:o + 257 + NE]
        ones64 = cst[0:64, 128:192]
        identb = k.sb("identb", [128, 128], BF16)
        k.v(lambda q: q.tensor_copy(out=identb[:, :], in_=ident), [cst], [identb])
        onesb = k.sb("onesb", [1, 128], BF16)
        k.v(lambda q: q.tensor_copy(out=onesb[:, :], in_=cst[0:1, 128:256]), [cst], [onesb])
        flag = k.sb("flagt", [128, 1])
        k.dma("sp", lambda q: q.dma_start(out=flag[:, :], in_=flag_d), [], [flag])
        eidf = k.sb("eidf", [128, NTILE, 8])
        gates = k.sb("gates", [128, NTILE, TOPK])
        ohall = k.sb("ohall", [128, NTILE * TOPK, NE])
        mall = k.sb("mall", [128, NTILE, NE])
        slot_i = k.sb("slot_i", [128, NTILE * TOPK, 1], I32)
        idx1_i = k.sb("idx1_i", [128, 8, 128], I32)
        idx2_i = k.sb("idx2_i", [128, 4, 128], I32)
        eidx_i = k.sb("eidx_i", [128, 128], I32)

        def layer_norm(src, srcbuf, dst, dstbuf, gb, gbbuf, gi, scale, tmp, st, mv):
            for c in range(4):
                k.v(lambda q, c=c: q.bn_stats(out=st[:, c * 6:(c + 1) * 6], in_=src[:, c * 512:(c + 1) * 512]), [srcbuf], [st])
            k.v(lambda q: q.bn_aggr(out=mv[:, 0:2], in_=st[:, 0:24]), [st], [mv])
            k.v(lambda q: q.tensor_scalar(out=mv[:, 2:3], in0=mv[:, 1:2], scalar1=LN_EPS, scalar2=None, op0=ALU.add), [mv], [mv])
            k.a(lambda q: q.activation(out=mv[:, 2:3], in_=mv[:, 2:3], func=AF.Sqrt), [mv], [mv])
            k.v(lambda q: q.reciprocal(out=mv[:, 2:3], in_=mv[:, 2:3]), [mv], [mv])
            k.v(lambda q: q.tensor_scalar(out=tmp[:, :], in0=src, scalar1=mv[:, 0:1], scalar2=mv[:, 2:3], op0=ALU.subtract, op1=ALU.mult), [srcbuf, mv], [tmp])
            k.v(lambda q: q.tensor_tensor(out=tmp[:, :], in0=tmp[:, :], in1=gb[:, gi, :], op=ALU.mult), [tmp, gbbuf], [tmp])
            if scale == 1.0:
                k.v(lambda q: q.tensor_tensor(out=dst, in0=tmp[:, :], in1=gb[:, gi + 1, :], op=ALU.add), [tmp, gbbuf], [dstbuf])
            else:
                k.v(lambda q: q.tensor_tensor(out=tmp[:, :], in0=tmp[:, :], in1=gb[:, gi + 1, :], op=ALU.add), [tmp, gbbuf], [tmp])
                k.v(lambda q: q.tensor_scalar(out=dst, in0=tmp[:, :], scalar1=float(scale), scalar2=None, op0=ALU.mult), [tmp], [dstbuf])

        esM = ExitStack()
        k.es = esM
        hT = k.sb("hT", [128, FC, G], BF16)
        lact = k.sb("lact", [128, 4, G])
        S = k.sb("S", [HS, NH, HS])
        yl = k.sb("yl", [128, 8, G], BF16)
        yr = k.sb("yr", [HS, NH, G], BF16)
        hist = k.sb("hist", [HS, NH, 3])
        hstate = k.sb("hstate", [128, 8])
        uhist = k.sb("uhist", [128, 8, 3])
        lhist = k.sb("lhist", [128, 4])
        for t_ in (S, hist, hstate, uhist, lhist):
            k.v(lambda q, t_=t_: q.memset(t_.t[:], 0.0), [], [t_])
        lru_cp = k.sb("lru_cp", [128, 8, 8])
        k.dma("sp", lambda q: q.dma_start(out=lru_cp[:, :, :], in_=lru_cp_d), [], [lru_cp])
        lru_c = k.sb("lru_c", [128, 8, 2])
        k.a(lambda q: q.activation(out=lru_c[:, :, 0], in_=lru_cp[:, :, 7], func=AF.Exp, scale=-1.0), [lru_cp], [lru_c])
        k.a(lambda q: q.activation(out=lru_c[:, :, 1], in_=lru_c[:, :, 0], func=AF.Ln, bias=1.0), [lru_c], [lru_c])
        k.v(lambda q: q.tensor_scalar(out=lru_c[:, :, 0], in0=lru_c[:, :, 1], scalar1=-8.0, scalar2=None, op0=ALU.mult), [lru_c], [lru_c])
        k.v(lambda q: q.tensor_scalar(out=lru_c[:, :, 1], in0=lru_c[:, :, 0], scalar1=2.0, scalar2=None, op0=ALU.mult), [lru_c], [lru_c])
        wrg = k.sb("wrg", [128, 8, 128])
        wig = k.sb("wig", [128, 8, 128])
        k.dma("sp", lambda q: q.dma_start(out=wrg[:, :, :], in_=wrg_d.rearrange("n p m -> p n m")), [], [wrg])
        k.dma("sp", lambda q: q.dma_start(out=wig[:, :, :], in_=wig_d.rearrange("n p m -> p n m")), [], [wig])
        rw_cp = k.sb("rw_cp", [HS, NH, 10])
        k.dma("sp", lambda q: q.dma_start(out=rw_cp[:, :, :], in_=rw_cp_d), [], [rw_cp])
        mu_lora = k.sb("mu_lora", [128, 4])
        k.dma("sp", lambda q: q.dma_start(out=mu_lora[:, :], in_=mu_lora_d), [], [mu_lora])
        wr = k.sb("wr", [128, FC, NE])
        brt = k.sb("brt", [128, NE])
        k.dma("sp", lambda q: q.dma_start(out=wr[:, :, :], in_=wr_d), [], [wr])
        k.dma("sp", lambda q: q.dma_start(out=brt[:, :], in_=br_d), [], [brt])
        st = k.sb("st", [128, 24])
        mv = k.sb("mv", [128, 4])
        lg = k.sb("lg", [128, NE])
        m8 = k.sb("m8", [128, 8])
        i8 = k.sb("i8", [128, 8], U32)
        sm = k.sb("sm", [128, 8])

        def proj(wt, col0, M, out_ps):
            for kc in range(FC):
                k.pe(lambda q, kc=kc: q.matmul(out_ps[0:M, :], lhsT=wt[:, kc, col0:col0 + M], rhs=hT[:, kc, :], start=(kc == 0), stop=(kc == FC - 1)), [wt, hT], [out_ps])

        def early_stop():
            k.barrier()
            with ExitStack() as esX:
                k.es = esX
                tb = k.sb("tbx", [128, D])
                for tg in range(NTILE):
                    k.dma("sp", lambda q, tg=tg: q.dma_start(out=tb[:, :], in_=xa[tg * 128:(tg + 1) * 128, :]), [], [tb])
                    k.dma("sp", lambda q, tg=tg: q.dma_start(out=out_d[tg * 128:(tg + 1) * 128, :], in_=tb[:, :]), [tb], [OUTB])
                k.wait_all("sp", [OUTB])
            esM.close()
            print("instructions:", k.n_ins)
            return nc

        for g in range(NG):
            t0g = g * G
            out_grp = g >= NG // 2
            with ExitStack() as s1:
                k.es = s1
                lnin = k.sb("lnin", [128, 2, D])
                k.dma("sp", lambda q: q.dma_start(out=lnin[:, :, :], in_=lnrep[0:2].rearrange("a p d -> p a d")), [], [lnin])
                xt = k.sb("xt", [128, D])
                lntmp = k.sb("lntmp", [128, D])
                h0b = k.sb("h0b", [128, D], BF16)
                wlr = k.ring("wlr", [128, FC, 128], BF16, 3)
                wlora = k.sb("wlora", [128, FC, 448], BF16)
                k.dma("pool", lambda q: q.dma_start(out=wlora[:, :, :], in_=win_lora), [], [wlora])
                lw = [k.sb("lw%d" % i, [128, G]) for i in range(6)]
                ub = k.sb("ub", [128, 3 + G])
                lb = k.sb("lb", [128, 1 + G])
                for ti in range(4):
                    r0 = t0g + ti * 128
                    k.dma("sp", lambda q, r0=r0: q.dma_start(out=xt[:, :], in_=xa[r0:r0 + 128, :]), [], [xt])
                    layer_norm(xt[:, :], xt, h0b[:, :], h0b, lnin, lnin, 0, 1.0, lntmp, st, mv)
                    for c4 in range(4):
                        hf = psb_half[0]
                        psb_half[0] ^= 1
                        for j in range(4):
                            kc = c4 * 4 + j
                            k.pe(lambda q, kc=kc, hf=hf, j=j: q.transpose(out=psb[:, hf * 512 + j * 128: hf * 512 + (j + 1) * 128], in_=h0b[:, kc * 128:(kc + 1) * 128], identity=identb[:, :]), [h0b, identb], [psb])
                        k.a(lambda q, c4=c4, hf=hf, ti=ti: q.activation(out=hT[:, c4 * 4:(c4 + 1) * 4, ti * 128:(ti + 1) * 128], in_=psb[:, hf * 512:(hf + 1) * 512].rearrange("p (a b) -> p a b", a=4), func=AF.Copy), [psb], [hT])
                if g == NG // 2:
                    for t_, np_ in ((hstate, 128), (uhist, 128), (lhist, 128), (hist, HS), (S, HS)):
                        k.v(lambda q, t_=t_, np_=np_: q.tensor_scalar(out=t_.t[:], in0=t_.t[:], scalar1=flag[0:np_, 0:1], scalar2=None, op0=ALU.mult), [t_, flag], [t_])
                for ti in range(8):
                    wt = wlr.next()
                    k.dma("pool", lambda q, wt=wt, ti=ti: q.dma_start(out=wt[:, :, :], in_=win_lru[ti]), [], [wt])
                    if out_grp:
                        wt2 = wlr.next()
                        k.dma("pool", lambda q, wt2=wt2, ti=ti: q.dma_start(out=wt2[:, :, :], in_=win_lru[8 + ti]), [], [wt2])
                    pu = psf.next()
                    proj(wt, 0, 128, pu)
                    k.v(lambda q, ti=ti: q.tensor_copy(out=ub[:, 0:3], in_=uhist[:, ti, :]), [uhist], [ub])
                    k.a(lambda q, pu=pu: q.activation(out=ub[:, 3:3 + G], in_=pu[:, :], func=AF.Copy), [pu], [ub])
                    k.v(lambda q, ti=ti: q.tensor_copy(out=uhist[:, ti, :], in_=ub[:, G:G + 3]), [ub], [uhist])
                    if out_grp:
                        pg = psf.next()
                        proj(wt2, 0, 128, pg)
                    uc, rr, ii, aa, bb, gg = lw
                    k.v(lambda q, ti=ti: q.tensor_scalar(out=uc[:, :], in0=ub[:, 0:G], scalar1=lru_cp[:, ti, 0:1], scalar2=lru_cp[:, ti, 4:5], op0=ALU.mult, op1=ALU.add), [ub, lru_cp], [uc])
                    for j in range(1, 4):
                        k.v(lambda q, ti=ti, j=j: q.scalar_tensor_tensor(out=uc[:, :], in0=ub[:, j:j + G], scalar=lru_cp[:, ti, j:j + 1], in1=uc[:, :], op0=ALU.mult, op1=ALU.add), [ub, lru_cp, uc], [uc])
                    pr = psf.next()
                    k.pe(lambda q, ti=ti, pr=pr: q.matmul(pr[:, :], lhsT=wrg[:, ti, :], rhs=uc[:, :], start=True, stop=True), [wrg, uc], [pr])
                    pi = psf.next()
                    k.pe(lambda q, ti=ti, pi=pi: q.matmul(pi[:, :], lhsT=wig[:, ti, :], rhs=uc[:, :], start=True, stop=True), [wig, uc], [pi])
                    k.a(lambda q, ti=ti, pr=pr: q.activation(out=rr[:, :], in_=pr[:, :], func=AF.Sigmoid, bias=lru_cp[:, ti, 5:6]), [pr, lru_cp], [rr])
                    k.a(lambda q, ti=ti, pi=pi: q.activation(out=ii[:, :], in_=pi[:, :], func=AF.Sigmoid, bias=lru_cp[:, ti, 6:7]), [pi, lru_cp], [ii])
                    k.a(lambda q, ti=ti: q.activation(out=aa[:, :], in_=rr[:, :], func=AF.Exp, scale=lru_c[:, ti, 0:1]), [rr, lru_c], [aa])
                    k.a(lambda q, ti=ti: q.activation(out=bb[:, :], in_=rr[:, :], func=AF.Exp, scale=lru_c[:, ti, 1:2]), [rr, lru_c], [bb])
                    k.v(lambda q: q.tensor_scalar(out=bb[:, :], in0=bb[:, :], scalar1=-1.0, scalar2=1.0, op0=ALU.mult, op1=ALU.add), [bb], [bb])
                    k.a(lambda q: q.activation(out=bb[:, :], in_=bb[:, :], func=AF.Sqrt), [bb], [bb])
                    k.v(lambda q: q.tensor_tensor(out=ii[:, :], in0=ii[:, :], in1=uc[:, :], op=ALU.mult), [ii, uc], [ii])
                    k.v(lambda q: q.tensor_tensor(out=bb[:, :], in0=bb[:, :], in1=ii[:, :], op=ALU.mult), [bb, ii], [bb])
                    k.v(lambda q, ti=ti: q.tensor_tensor_scan(out=rr[:, :], data0=aa[:, :], data1=bb[:, :], initial=hstate[:, ti:ti + 1], op0=ALU.mult, op1=ALU.add), [aa, bb, hstate], [rr])
                    k.v(lambda q, ti=ti: q.tensor_copy(out=hstate[:, ti:ti + 1], in_=rr[:, G - 1:G]), [rr], [hstate])
                    if not out_grp:
                        continue
                    k.a(lambda q, pg=pg: q.activation(out=gg[:, :], in_=pg[:, :], func=AF.Copy), [pg], [gg])
                    k.v(lambda q: q.tensor_tensor(out=aa[:, :], in0=gg[:, :], in1=gg[:, :], op=ALU.mult), [gg], [aa])
                    k.v(lambda q: q.tensor_scalar(out=aa[:, :], in0=aa[:, :], scalar1=0.044715, scalar2=1.0, op0=ALU.mult, op1=ALU.add), [aa], [aa])
                    k.v(lambda q: q.tensor_tensor(out=aa[:, :], in0=aa[:, :], in1=gg[:, :], op=ALU.mult), [aa, gg], [aa])
                    k.a(lambda q: q.activation(out=aa[:, :], in_=aa[:, :], func=AF.Sigmoid, scale=1.5957691216), [aa], [aa])
                    k.v(lambda q: q.tensor_tensor(out=aa[:, :], in0=aa[:, :], in1=gg[:, :], op=ALU.mult), [aa, gg], [aa])
                    if g == NG // 2 and ti == 0:
                        dbg("h", rr[:, :], [128, G], rr)
                        dbg("gelu", aa[:, :], [128, G], aa)
                        dbg("b", bb[:, :], [128, G], bb)
                        dbg("uc", uc[:, :], [128, G], uc)
                        dbg("ub", ub[:, :], [128, 3 + G], ub)
                    k.v(lambda q, ti=ti: q.tensor_tensor(out=yl[:, ti, :], in0=aa[:, :], in1=rr[:, :], op=ALU.mult), [aa, rr], [yl])
                for li, (c0, M) in enumerate([(0, 96), (96, 96), (192, 128), (320, 128)]):
                    pp = psf.next()
                    proj(wlora, c0, M, pp)
                    k.v(lambda q, li=li, M=M: q.tensor_copy(out=lb[0:M, 0:1], in_=lhist[0:M, li:li + 1]), [lhist], [lb])
                    k.a(lambda q, pp=pp, M=M: q.activation(out=lb[0:M, 1:1 + G], in_=pp[0:M, :], func=AF.Copy), [pp], [lb])
                    k.v(lambda q, li=li, M=M: q.tensor_copy(out=lhist[0:M, li:li + 1], in_=lb[0:M, G:G + 1]), [lb], [lhist])
                    tt = lw[0]
                    k.v(lambda q, M=M: q.tensor_tensor(out=tt[0:M, :], in0=lb[0:M, 0:G], in1=lb[0:M, 1:1 + G], op=ALU.subtract), [lb], [tt])
                    k.v(lambda q, li=li, M=M: q.scalar_tensor_tensor(out=tt[0:M, :], in0=tt[0:M, :], scalar=mu_lora[0:M, li:li + 1], in1=lb[0:M, 1:1 + G], op0=ALU.mult, op1=ALU.add), [tt, mu_lora, lb], [tt])
                    fn = [AF.Tanh, AF.Copy, AF.Sigmoid, AF.Sigmoid][li]
                    k.a(lambda q, li=li, M=M, fn=fn: q.activation(out=lact[0:M, li, :], in_=tt[0:M, :], func=fn), [tt], [lact])
            k.barrier()
            if mode == "s1":
                return early_stop()
            with ExitStack() as sA:
                k.es = sA
                wlr = k.ring("wrk", [128, FC, 192], BF16, 2)
                dupr = k.ring("dupr", [96, HS], F32, 2)
                aupr = k.ring("aupr", [96, HS], F32, 2)
                gupr = k.ring("gupr", [128, 2, HS], F32, 2)
                pb = k.sb("pb", [HS, 3, 1 + G])
                hw = [k.sb("hw%d" % i, [HS, G]) for i in range(17)]
                (r_, k_, v_, kk_, a_, g_, lgw, L_, eL, Rp, Kpp, b_, Ke, Be, t0, t1, ysb) = hw
                vtm = k.sb("vtm", [HS, 8, HS])
                ketm = k.sb("ketm", [HS, 8, HS])
                betm = k.sb("betm", [HS, 8, HS])
                kktm = k.sb("kktm", [HS, 8, HS])
                P = [k.sb("P%d" % i, [HS, 8, HS]) for i in range(2)]
                Q = [k.sb("Q%d" % i, [HS, 8, HS]) for i in range(2)]
                PI = k.sb("PI", [HS, 8, HS])
                Z = [k.sb("Z%d" % i, [HS, 8, HS]) for i in range(2)]
                MT = k.sb("MT", [HS, 8, HS])
                GT = k.sb("GT", [HS, 8, HS])
                HT = k.sb("HT", [HS, 8, HS])
                Xsb = k.sb("Xsb", [HS, HS])
                NU = k.sb("NU", [HS, HS])
                for h in range(NH):
                    wt = wlr.next()
                    k.dma("pool", lambda q, wt=wt, h=h: q.dma_start(out=wt[:, :, :], in_=win_rkv[h]), [], [wt])
                    du, au, gu = dupr.next(), aupr.next(), gupr.next()
                    k.dma("sp", lambda q, du=du, h=h: q.dma_start(out=du[:, :], in_=dup_d[h]), [], [du])
                    k.dma("sp", lambda q, au=au, h=h: q.dma_start(out=au[:, :], in_=aup_d[h]), [], [au])
                    k.dma("sp", lambda q, gu=gu, h=h: q.dma_start(out=gu[:, :, :], in_=gup_d[h]), [], [gu])
                    k.v(lambda q, h=h: q.tensor_copy(out=pb[:, :, 0], in_=hist[:, h, :]), [hist], [pb])
                    for wi in range(3):
                        pp = psf.next()
                        proj(wt, wi * HS, HS, pp)
                        k.a(lambda q, pp=pp, wi=wi: q.activation(out=pb[:, wi, 1:1 + G], in_=pp[0:HS, :], func=AF.Copy), [pp], [pb])
                    k.v(lambda q, h=h: q.tensor_copy(out=hist[:, h, :], in_=pb[:, :, G]), [pb], [hist])
                    for wi, dst in enumerate([r_, k_, v_]):
                        k.v(lambda q, wi=wi: q.tensor_tensor(out=t0[:, :], in0=pb[:, wi, 0:G], in1=pb[:, wi, 1:1 + G], op=ALU.subtract), [pb], [t0])
                        k.v(lambda q, wi=wi, dst=dst, h=h: q.scalar_tensor_tensor(out=dst[:, :], in0=t0[:, :], scalar=rw_cp[:, h, wi:wi + 1], in1=pb[:, wi, 1:1 + G], op0=ALU.mult, op1=ALU.add), [t0, rw_cp, pb], [dst])
                    if mode == "p1" and h == 0:
                        sA.close()
                        return early_stop()
                    pp = psf.next()
                    k.pe(lambda q, pp=pp, du=du: q.matmul(pp[0:HS, :], lhsT=du[:, :], rhs=lact[0:96, 0, :], start=True, stop=True), [du, lact], [pp])
                    k.a(lambda q, pp=pp, h=h: q.activation(out=lgw[:, :], in_=pp[0:HS, :], func=AF.Sigmoid, bias=rw_cp[:, h, 3:4]), [pp, rw_cp], [lgw])
                    k.v(lambda q: q.tensor_scalar(out=lgw[:, :], in0=lgw[:, :], scalar1=-0.6065306597126334, scalar2=None, op0=ALU.mult), [lgw], [lgw])
                    pp = psf.next()
                    k.pe(lambda q, pp=pp, au=au: q.matmul(pp[0:HS, :], lhsT=au[:, :], rhs=lact[0:96, 1, :], start=True, stop=True), [au, lact], [pp])
                    k.a(lambda q, pp=pp, h=h: q.activation(out=a_[:, :], in_=pp[0:HS, :], func=AF.Sigmoid, bias=rw_cp[:, h, 4:5]), [pp, rw_cp], [a_])
                    pp = psf.next()
                    for j in range(2):
                        k.pe(lambda q, pp=pp, j=j, gu=gu: q.matmul(pp[0:HS, :], lhsT=gu[:, j, :], rhs=lact[:, 2 + j, :], start=(j == 0), stop=(j == 1)), [gu, lact], [pp])
                    k.a(lambda q, pp=pp: q.activation(out=g_[:, :], in_=pp[0:HS, :], func=AF.Copy), [pp], [g_])
                    if mode == "p2" and h == 0:
                        sA.close()
                        return early_stop()
                    k.v(lambda q, h=h: q.tensor_scalar(out=kk_[:, :], in0=k_[:, :], scalar1=rw_cp[:, h, 5:6], scalar2=None, op0=ALU.mult), [k_, rw_cp], [kk_])
                    k.v(lambda q: q.tensor_tensor(out=t0[:, :], in0=kk_[:, :], in1=kk_[:, :], op=ALU.mult), [kk_], [t0])
                    pp = psf.next()
                    k.pe(lambda q, pp=pp: q.matmul(pp[0:HS, :], lhsT=ones64, rhs=t0[:, :], start=True, stop=True), [cst, t0], [pp])
                    k.v(lambda q, pp=pp: q.tensor_scalar(out=t1[:, :], in0=pp[0:HS, :], scalar1=1e-24, scalar2=None, op0=ALU.max), [pp], [t1])
                    k.a(lambda q: q.activation(out=t1[:, :], in_=t1[:, :], func=AF.Sqrt), [t1], [t1])
                    k.v(lambda q: q.reciprocal(out=t1[:, :], in_=t1[:, :]), [t1], [t1])
                    k.v(lambda q: q.tensor_tensor(out=kk_[:, :], in0=kk_[:, :], in1=t1[:, :], op=ALU.mult), [kk_, t1], [kk_])
                    k.v(lambda q, h=h: q.tensor_scalar(out=t0[:, :], in0=a_[:, :], scalar1=1.0, scalar2=rw_cp[:, h, 6:7], op0=ALU.subtract, op1=ALU.mult), [a_, rw_cp], [t0])
                    k.v(lambda q: q.scalar_tensor_tensor(out=k_[:, :], in0=t0[:, :], scalar=1.0, in1=k_[:, :], op0=ALU.add, op1=ALU.mult), [t0, k_], [k_])
                    k.v(lambda q: q.tensor_tensor(out=b_[:, :], in0=kk_[:, :], in1=a_[:, :], op=ALU.mult), [kk_, a_], [b_])
                    if mode == "p3" and h == 0:
                        sA.close()
                        return early_stop()
                    for c in range(8):
                        cs = slice(c * HS, (c + 1) * HS)
                        k.v(lambda q, cs=cs: q.tensor_tensor_scan(out=L_[:, cs], data0=ones64, data1=lgw[:, cs], initial=0.0, op0=ALU.mult, op1=ALU.add), [cst, lgw], [L_])
                    k.a(lambda q: q.activation(out=eL[:, :], in_=L_[:, :], func=AF.Exp), [L_], [eL])
                    k.v(lambda q: q.tensor_tensor(out=Rp[:, :], in0=r_[:, :], in1=eL[:, :], op=ALU.mult), [r_, eL], [Rp])
                    k.v(lambda q: q.tensor_tensor(out=t0[:, :], in0=L_[:, :], in1=lgw[:, :], op=ALU.subtract), [L_, lgw], [t0])
                    k.a(lambda q: q.activation(out=a_[:, :], in_=t0[:, :], func=AF.Exp), [t0], [a_])
                    k.v(lambda q: q.tensor_tensor(out=kk_[:, :], in0=kk_[:, :], in1=a_[:, :], op=ALU.mult), [kk_, a_], [kk_])
                    KKp = kk_
                    for c in range(8):
                        cs = slice(c * HS, (c + 1) * HS)
                        k.a(lambda q, cs=cs, c=c: q.activation(out=t1[:, cs], in_=L_[:, cs], func=AF.Exp, scale=-1.0, bias=L_[:, c * HS + HS - 1:c * HS + HS]), [L_], [t1])
                    k.v(lambda q: q.tensor_tensor(out=Ke[:, :], in0=k_[:, :], in1=t1[:, :], op=ALU.mult), [k_, t1], [Ke])
                    k.v(lambda q: q.tensor_tensor(out=Be[:, :], in0=b_[:, :], in1=t1[:, :], op=ALU.mult), [b_, t1], [Be])
                    k.a(lambda q: q.activation(out=a_[:, :], in_=L_[:, :], func=AF.Exp, scale=-1.0), [L_], [a_])
                    k.v(lambda q: q.tensor_tensor(out=Kpp[:, :], in0=k_[:, :], in1=a_[:, :], op=ALU.mult), [k_, a_], [Kpp])
                    k.v(lambda q: q.tensor_tensor(out=b_[:, :], in0=b_[:, :], in1=a_[:, :], op=ALU.mult), [b_, a_], [b_])
                    Bpp = b_
                    if mode == "p4" and h == 0:
                        sA.close()
                        return early_stop()
                    for src, dst in ((v_, vtm), (Ke, ketm), (Be, betm), (KKp, kktm)):
                        pp = psf.next()
                        for c in range(8):
                            cs = slice(c * HS, (c + 1) * HS)
                            k.pe(lambda q, pp=pp, src=src, cs=cs: q.transpose(out=pp[0:HS, cs], in_=src[:, cs], identity=i64), [src, cst], [pp])
                        k.a(lambda q, pp=pp, dst=dst: q.activation(out=dst[:, :, :], in_=pp[0:HS, :].rearrange("p (a b) -> p a b", a=8), func=AF.Copy), [pp], [dst])

                    if mode == "p5" and h == 0:
                        sA.close()
                        return early_stop()
                    def cmat(lh, rh, mask, sign, dst):
                        pp = psf.next()
                        for c in range(8):
                            cs = slice(c * HS, (c + 1) * HS)
                            k.pe(lambda q, pp=pp, cs=cs: q.matmul(pp[0:HS, cs], lhsT=lh[:, cs], rhs=rh[:, cs], start=True, stop=True), [lh, rh], [pp])
                        for c in range(8):
                            cs = slice(c * HS, (c + 1) * HS)
                            k.v(lambda q, pp=pp, cs=cs, c=c: q.scalar_tensor_tensor(out=dst[:, c, :], in0=pp[0:HS, cs], scalar=float(sign), in1=mask, op0=ALU.mult, op1=ALU.mult), [pp, cst], [dst])

                    cmat(KKp, Bpp, m_sl, -1.0, P[0])
                    cmat(Bpp, KKp, m_su, -1.0, Q[0])
                    cmat(Kpp, KKp, m_su, 1.0, MT)
                    if out_grp:
                        cmat(Kpp, Rp, m_ui, 1.0, GT)
                        cmat(Bpp, Rp, m_ui, 1.0, HT)
                    for c in range(8):
                        k.v(lambda q, c=c: q.tensor_tensor(out=Z[0][:, c, :], in0=Q[0][:, c, :], in1=i64, op=ALU.add), [Q[0], cst], [Z[0]])
                    if mode == "p6" and h == 0:
                        sA.close()
                        return early_stop()
                    cur = 0
                    for lvl in range(5):
                        nxt = cur ^ 1
                        ppP = psf.next()
                        for c in range(8):
                            cs = slice(c * HS, (c + 1) * HS)
                            k.pe(lambda q, ppP=ppP, cs=cs, c=c, cur=cur: q.matmul(ppP[0:HS, cs], lhsT=Q[cur][:, c, :], rhs=P[cur][:, c, :], start=True, stop=True), [Q[cur], P[cur]], [ppP])
                        if mode == "d%d0" % lvl and h == 0:
                            sA.close()
                            return early_stop()
                        if lvl < 4:
                            ppQ = psf.next()
                            for c in range(8):
                                cs = slice(c * HS, (c + 1) * HS)
                                k.pe(lambda q, ppQ=ppQ, cs=cs, c=c, cur=cur: q.matmul(ppQ[0:HS, cs], lhsT=P[cur][:, c, :], rhs=Q[cur][:, c, :], start=True, stop=True), [Q[cur], P[cur]], [ppQ])
                            k.v(lambda q, ppP=ppP, nxt=nxt: q.tensor_copy(out=P[nxt][:, :, :], in_=ppP[0:HS, :].rearrange("p (a b) -> p a b", a=8)), [ppP], [P[nxt]])
                            k.v(lambda q, ppQ=ppQ, nxt=nxt: q.tensor_copy(out=Q[nxt][:, :, :], in_=ppQ[0:HS, :].rearrange("p (a b) -> p a b", a=8)), [ppQ], [Q[nxt]])
                        if mode == "d%d1" % lvl and h == 0:
                            sA.close()
                            return early_stop()
                        for c in range(8):
                            cs = slice(c * HS, (c + 1) * HS)
                            k.v(lambda q, ppP=ppP, cs=cs, c=c: q.tensor_tensor(out=PI[:, c, :], in0=ppP[0:HS, cs], in1=i64, op=ALU.add), [ppP, cst], [PI])
                        if mode == "d%d2" % lvl and h == 0:
                            sA.close()
                            return early_stop()
                        zi, zo = lvl % 2, (lvl + 1) % 2
                        ppZ = psf.next()
                        for c in range(8):
                            cs = slice(c * HS, (c + 1) * HS)
                            k.pe(lambda q, ppZ=ppZ, cs=cs, c=c, zi=zi: q.matmul(ppZ[0:HS, cs], lhsT=PI[:, c, :], rhs=Z[zi][:, c, :], start=True, stop=True), [PI, Z[zi]], [ppZ])
                        k.v(lambda q, ppZ=ppZ, zo=zo: q.tensor_copy(out=Z[zo][:, :, :], in_=ppZ[0:HS, :].rearrange("p (a b) -> p a b", a=8)), [ppZ], [Z[zo]])
                        cur = nxt
                        if mode == "d%d3" % lvl and h == 0:
                            sA.close()
                            return early_stop()
                    if mode == "p7" and h == 0:
                        sA.close()
                        return early_stop()
                    ZF = Z[1]
                    TKKT, MV = P[0], Q[0]
                    pp = psf.next()
                    for c in range(8):
                        cs = slice(c * HS, (c + 1) * HS)
                        k.pe(lambda q, pp=pp, cs=cs, c=c: q.matmul(pp[0:HS, cs], lhsT=kktm[:, c, :], rhs=ZF[:, c, :], start=True, stop=True), [kktm, ZF], [pp])
                    k.v(lambda q, pp=pp: q.tensor_copy(out=TKKT[:, :, :], in_=pp[0:HS, :].rearrange("p (a b) -> p a b", a=8)), [pp], [TKKT])
                    pp = psf.next()
                    for c in range(8):
                        cs = slice(c * HS, (c + 1) * HS)
                        k.pe(lambda q, pp=pp, cs=cs, c=c: q.matmul(pp[0:HS, cs], lhsT=MT[:, c, :], rhs=vtm[:, c, :], start=True, stop=True), [MT, vtm], [pp])
                    k.v(lambda q, pp=pp: q.tensor_copy(out=MV[:, :, :], in_=pp[0:HS, :].rearrange("p (a b) -> p a b", a=8)), [pp], [MV])
                    py = pyb
                    for c in range(8):
                        cs = slice(c * HS, (c + 1) * HS)
                        pu_ = psf.next()
                        k.pe(lambda q, pu_=pu_, c=c, h=h: q.matmul(pu_[0:HS, 0:HS], lhsT=TKKT[:, c, :], rhs=S[:, h, :], start=True, stop=False), [TKKT, S], [pu_])
                        k.pe(lambda q, pu_=pu_, c=c: q.matmul(pu_[0:HS, 0:HS], lhsT=ZF[:, c, :], rhs=MV[:, c, :], start=False, stop=True), [ZF, MV], [pu_])
                        k.a(lambda q, pu_=pu_: q.activation(out=NU[:, :], in_=pu_[0:HS, 0:HS], func=AF.Copy, scale=-1.0), [pu_], [NU])
                        if out_grp:
                            k.pe(lambda q, cs=cs, h=h: q.matmul(py[0:HS, cs], lhsT=S[:, h, :], rhs=Rp[:, cs], start=True, stop=False), [S, Rp], [py])
                            k.pe(lambda q, cs=cs, c=c: q.matmul(py[0:HS, cs], lhsT=vtm[:, c, :], rhs=GT[:, c, :], start=False, stop=False), [vtm, GT], [py])
                            k.pe(lambda q, cs=cs, c=c: q.matmul(py[0:HS, cs], lhsT=NU[:, :], rhs=HT[:, c, :], start=False, stop=True), [NU, HT], [py])
                        pS = psf.next()
                        k.pe(lambda q, pS=pS, c=c: q.matmul(pS[0:HS, 0:HS], lhsT=ketm[:, c, :], rhs=vtm[:, c, :], start=True, stop=False), [ketm, vtm], [pS])
                        k.pe(lambda q, pS=pS, c=c: q.matmul(pS[0:HS, 0:HS], lhsT=betm[:, c, :], rhs=NU[:, :], start=False, stop=True), [betm, NU], [pS])
                        k.v(lambda q, pS=pS, c=c, h=h: q.scalar_tensor_tensor(out=S[:, h, :], in0=S[:, h, :], scalar=eL[:, c * HS + HS - 1:c * HS + HS], in1=pS[0:HS, 0:HS], op0=ALU.mult, op1=ALU.add), [S, eL, pS], [S])
                    if not out_grp:
                        continue
                    k.a(lambda q: q.activation(out=ysb[:, :], in_=py[0:HS, :], func=AF.Copy), [py], [ysb])
                    if mode == "p8" and h == 0:
                        sA.close()
                        return early_stop()
                    pp = psf.next()
                    k.pe(lambda q, pp=pp: q.matmul(pp[0:HS, :], lhsT=ones64, rhs=ysb[:, :], start=True, stop=True), [cst, ysb], [pp])
                    k.v(lambda q, pp=pp: q.scalar_tensor_tensor(out=ysb[:, :], in0=pp[0:HS, :], scalar=-1.0 / HS, in1=ysb[:, :], op0=ALU.mult, op1=ALU.add), [pp, ysb], [ysb])
                    k.v(lambda q: q.tensor_tensor(out=t0[:, :], in0=ysb[:, :], in1=ysb[:, :], op=ALU.mult), [ysb], [t0])
                    pp = psf.next()
                    k.pe(lambda q, pp=pp: q.matmul(pp[0:HS, :], lhsT=ones64, rhs=t0[:, :], start=True, stop=True), [cst, t0], [pp])
                    k.v(lambda q, pp=pp: q.tensor_scalar(out=t1[:, :], in0=pp[0:HS, :], scalar1=1.0 / HS, scalar2=GN_EPS, op0=ALU.mult, op1=ALU.add), [pp], [t1])
                    k.a(lambda q: q.activation(out=t1[:, :], in_=t1[:, :], func=AF.Sqrt), [t1], [t1])
                    k.v(lambda q: q.reciprocal(out=t1[:, :], in_=t1[:, :]), [t1], [t1])
                    k.v(lambda q: q.tensor_tensor(out=ysb[:, :], in0=ysb[:, :], in1=t1[:, :], op=ALU.mult), [ysb, t1], [ysb])
                    k.v(lambda q, h=h: q.tensor_scalar(out=ysb[:, :], in0=ysb[:, :], scalar1=rw_cp[:, h, 8:9], scalar2=rw_cp[:, h, 9:10], op0=ALU.mult, op1=ALU.add), [ysb, rw_cp], [ysb])
                    k.v(lambda q, h=h: q.scalar_tensor_tensor(out=t0[:, :], in0=r_[:, :], scalar=rw_cp[:, h, 7:8], in1=k_[:, :], op0=ALU.mult, op1=ALU.mult), [r_, rw_cp, k_], [t0])
                    pp = psf.next()
                    k.pe(lambda q, pp=pp: q.matmul(pp[0:HS, :], lhsT=ones64, rhs=t0[:, :], start=True, stop=True), [cst, t0], [pp])
                    k.v(lambda q, pp=pp: q.tensor_tensor(out=t1[:, :], in0=pp[0:HS, :], in1=v_[:, :], op=ALU.mult), [pp, v_], [t1])
                    k.v(lambda q: q.tensor_tensor(out=ysb[:, :], in0=ysb[:, :], in1=t1[:, :], op=ALU.add), [ysb, t1], [ysb])
                    k.v(lambda q, h=h: q.tensor_tensor(out=yr[:, h, :], in0=ysb[:, :], in1=g_[:, :], op=ALU.mult), [ysb, g_], [yr])
            k.barrier()
            if mode == "sA":
                return early_stop()
            if g < NG // 2:
                continue
            if mode == "mixer" and DEBUG and g == NG // 2:
                dbb = Buf(dbg_yl)
                k.dma("sp", lambda q: q.dma_start(out=dbg_yl, in_=yl[:, :, :]), [yl], [dbb])
                k.dma("sp", lambda q: q.dma_start(out=dbg_yr, in_=yr[:, :, :]), [yr], [dbb])
                k.dma("sp", lambda q: q.dma_start(out=dbg_hT, in_=hT[:, :, :]), [hT], [dbb])
            with ExitStack() as sB:
                k.es = sB
                lnin = k.sb("lninB", [128, 2, D])
                k.dma("sp", lambda q: q.dma_start(out=lnin[:, :, :], in_=lnrep[0:2].rearrange("a p d -> p a d")), [], [lnin])
                ln1 = k.sb("ln1", [128, 2, D])
                k.dma("sp", lambda q: q.dma_start(out=ln1[:, :, :], in_=lnrep[2:4].rearrange("a p d -> p a d")), [], [ln1])
                lntmp = k.sb("lntmpB", [128, D])
                zb = [k.sb("zb%d" % i, [128, D]) for i in range(4)]
                wol = k.sb("wol", [128, 8, 512], BF16)
                wor = k.sb("wor", [HS, NH, 512], BF16)
                h1T = k.sb("h1T", [128, FC, 128])
                for ti in range(4):
                    r0 = t0g + ti * 128
                    k.dma("sp", lambda q, ti=ti, r0=r0: q.dma_start(out=zb[ti][:, :], in_=xa[r0:r0 + 128, :]), [], [zb[ti]])
                    layer_norm(zb[ti][:, :], zb[ti], zb[ti][:, :], zb[ti], lnin, lnin, 0, ALPHA, lntmp, st, mv)
                for n in range(4):
                    k.dma("pool", lambda q, n=n: q.dma_start(out=wol[:, :, :], in_=wout_l[n]), [], [wol])
                    k.dma("pool", lambda q, n=n: q.dma_start(out=wor[:, :, :], in_=wout_r[n]), [], [wor])
                    for ti in range(4):
                        ts_ = slice(ti * 128, (ti + 1) * 128)
                        pp = psf.next()
                        for j in range(8):
                            k.pe(lambda q, pp=pp, j=j, ts_=ts_: q.matmul(pp[:, :], lhsT=yl[:, j, ts_], rhs=wol[:, j, :], start=(j == 0), stop=False), [yl, wol], [pp])
                        for j in range(NH):
                            k.pe(lambda q, pp=pp, j=j, ts_=ts_: q.matmul(pp[:, :], lhsT=yr[:, j, ts_], rhs=wor[:, j, :], start=False, stop=(j == NH - 1)), [yr, wor], [pp])
                        k.v(lambda q, pp=pp, ti=ti, n=n: q.tensor_tensor(out=zb[ti][:, n * 512:(n + 1) * 512], in0=zb[ti][:, n * 512:(n + 1) * 512], in1=pp[:, :], op=ALU.add), [zb[ti], pp], [zb[ti]])
                for ti in range(4):
                    tg = (g - NG // 2) * 4 + ti
                    h1 = zb[ti]
                    layer_norm(h1[:, :], h1, h1[:, :], h1, ln1, ln1, 0, 1.0, lntmp, st, mv)
                    k.dma("sp", lambda q, tg=tg, h1=h1: q.dma_start(out=H1[tg * 128:(tg + 1) * 128, :], in_=h1[:, :]), [h1], [H1b])
                    for c4 in range(4):
                        pp = psf.next()
                        for j in range(4):
                            kc = c4 * 4 + j
                            k.pe(lambda q, pp=pp, kc=kc, j=j, h1=h1: q.transpose(out=pp[:, j * 128:(j + 1) * 128], in_=h1[:, kc * 128:(kc + 1) * 128], identity=ident), [h1, cst], [pp])
                        k.a(lambda q, pp=pp, c4=c4: q.activation(out=h1T[:, c4 * 4:(c4 + 1) * 4, :], in_=pp[:, :].rearrange("p (a b) -> p a b", a=4), func=AF.Copy), [pp], [h1T])
                    pp = psf.next()
                    for kc in range(FC):
                        k.pe(lambda q, pp=pp, kc=kc: q.matmul(pp[:, 0:NE], lhsT=h1T[:, kc, :], rhs=wr[:, kc, :], start=(kc == 0), stop=(kc == FC - 1)), [h1T, wr], [pp])
                    k.v(lambda q, pp=pp: q.tensor_tensor(out=lg[:, :], in0=pp[:, 0:NE], in1=brt[:, :], op=ALU.add), [pp, brt], [lg])
                    k.v(lambda q: q.max(out=m8[:, :], in_=lg[:, :]), [lg], [m8])
                    k.v(lambda q: q.max_index(out=i8[:, :], in_max=m8[:, :], in_values=lg[:, :]), [m8, lg], [i8])
                    k.v(lambda q, tg=tg: q.tensor_copy(out=eidf[:, tg, :], in_=i8[:, :]), [i8], [eidf])
                    k.v(lambda q: q.tensor_scalar(out=sm[:, 0:1], in0=m8[:, 0:1], scalar1=-1.0, scalar2=None, op0=ALU.mult), [m8], [sm])
                    k.a(lambda q: q.activation(out=sm[:, 4:8], in_=m8[:, 0:4], func=AF.Exp, bias=sm[:, 0:1]), [m8, sm], [sm])
                    k.v(lambda q: q.reduce_sum(out=sm[:, 1:2], in_=sm[:, 4:8], axis=AX.X), [sm], [sm])
                    k.v(lambda q: q.reciprocal(out=sm[:, 2:3], in_=sm[:, 1:2]), [sm], [sm])
                    k.v(lambda q, tg=tg: q.tensor_scalar(out=gates[:, tg, :], in0=sm[:, 4:8], scalar1=sm[:, 2:3], scalar2=None, op0=ALU.mult), [sm], [gates])
                    for kk in range(TOPK):
                        k.v(lambda q, tg=tg, kk=kk: q.tensor_scalar(out=ohall[:, tg * TOPK + kk, :], in0=iota_e, scalar1=eidf[:, tg, kk:kk + 1], scalar2=None, op0=ALU.is_equal), [cst, eidf], [ohall])
                    k.v(lambda q, tg=tg: q.tensor_tensor(out=mall[:, tg, :], in0=ohall[:, tg * TOPK, :], in1=ohall[:, tg * TOPK + 1, :], op=ALU.add), [ohall], [mall])
                    for kk in range(2, TOPK):
                        k.v(lambda q, tg=tg, kk=kk: q.tensor_tensor(out=mall[:, tg, :], in0=mall[:, tg, :], in1=ohall[:, tg * TOPK + kk, :], op=ALU.add), [ohall, mall], [mall])
            k.barrier()
        with ExitStack() as sC:
            k.es = sC
            cntb = k.sb("cntb", [128, 4, NE])
            pp = psf.next()
            for tg in range(NTILE):
                k.pe(lambda q, pp=pp, tg=tg: q.matmul(pp[:, 0:NE], lhsT=ones, rhs=mall[:, tg, :], start=(tg == 0), stop=(tg == NTILE - 1)), [cst, mall], [pp])
            k.v(lambda q, pp=pp: q.tensor_copy(out=cntb[:, 3, :], in_=pp[:, 0:NE]), [pp], [cntb])
            k.v(lambda q: q.tensor_scalar(out=cntb[:, 0, :], in0=cntb[:, 3, :], scalar1=0.0, scalar2=None, op0=ALU.is_gt), [cntb], [cntb])
            for j_ in range(1, NTILE):
                k.v(lambda q, j_=j_: q.scalar_tensor_tensor(out=cntb[:, 0, :], in0=cntb[:, 3, :], scalar=float(128 * j_), in1=cntb[:, 0, :], op0=ALU.is_gt, op1=ALU.add), [cntb], [cntb])
            k.v(lambda q: q.tensor_scalar(out=cntb[:, 0, :], in0=cntb[:, 0, :], scalar1=128.0, scalar2=None, op0=ALU.mult), [cntb], [cntb])
            k.v(lambda q: q.tensor_tensor_scan(out=cntb[:, 1, :], data0=ones[:, 0:NE], data1=cntb[:, 0, :], initial=0.0, op0=ALU.mult, op1=ALU.add), [cst, cntb], [cntb])
            k.v(lambda q: q.tensor_tensor(out=cntb[:, 2, :], in0=cntb[:, 1, :], in1=cntb[:, 0, :], op=ALU.subtract), [cntb], [cntb])
            blkf = k.sb("blkf", [128, 2])
            blki = k.sb("blki", [128, 1], I32)
            k.v(lambda q: q.tensor_scalar(out=cntb[:, 3, :], in0=cntb[:, 1, :], scalar1=iota_p, scalar2=None, op0=ALU.is_le), [cntb, cst], [cntb])
            k.v(lambda q: q.reduce_sum(out=blkf[:, 0:1], in_=cntb[:, 3, :], axis=AX.X), [cntb], [blkf])
            k.v(lambda q: q.tensor_scalar(out=blkf[:, 1:2], in0=blkf[:, 0:1], scalar1=float(NE - 1), scalar2=None, op0=ALU.min), [blkf], [blkf])
            dg = k.sb("dg", [128, 128])
            ef = k.sb("ef", [128, 3, 128])
            tf = k.sb("tf", [128, 8, 128])
            pcol = k.sb("pcol", [128, 1])
            k.v(lambda q: q.tensor_scalar(out=pcol[:, :], in0=iota_p, scalar1=1.0 / 128.0, scalar2=None, op0=ALU.mult), [cst], [pcol])
            k.v(lambda q: q.tensor_scalar(out=dg[:, :], in0=ident, scalar1=blkf[:, 1:2], scalar2=None, op0=ALU.mult), [cst, blkf], [dg])
            ppe = psf.next()
            k.pe(lambda q: q.matmul(ppe[:, 0:128], lhsT=ones, rhs=dg[:, :], start=True, stop=True), [cst, dg], [ppe])
            k.v(lambda q: q.tensor_copy(out=ef[:, 0, :], in_=ppe[:, 0:128]), [ppe], [ef])
            k.v(lambda q: q.tensor_scalar(out=ef[:, 1, :], in0=ef[:, 0, :], scalar1=1024.0, scalar2=pcol[:, 0:1], op0=ALU.mult, op1=ALU.add), [ef, pcol], [ef])
            k.v(lambda q: q.tensor_scalar(out=ef[:, 2, :], in0=ef[:, 0, :], scalar1=512.0, scalar2=pcol[:, 0:1], op0=ALU.mult, op1=ALU.add), [ef, pcol], [ef])
            k.v(lambda q: q.tensor_copy(out=eidx_i[:, :], in_=ef[:, 0, :]), [ef], [eidx_i])
            for n_ in range(8):
                k.v(lambda q, n_=n_: q.tensor_scalar(out=tf[:, n_, :], in0=ef[:, 1, :], scalar1=float(n_ * 128), scalar2=None, op0=ALU.add), [ef], [tf])
            k.v(lambda q: q.tensor_copy(out=idx1_i[:, :, :], in_=tf[:, :, :]), [tf], [idx1_i])
            for n_ in range(4):
                k.v(lambda q, n_=n_: q.tensor_scalar(out=tf[:, n_, :], in0=ef[:, 2, :], scalar1=float(n_ * 128), scalar2=None, op0=ALU.add), [ef], [tf])
            k.v(lambda q: q.tensor_copy(out=idx2_i[:, :, :], in_=tf[:, 0:4, :]), [tf], [idx2_i])
            slotf = k.sb("slotf", [128, NTILE * TOPK])
            basef = k.sb("basef", [128, NE])
            prodf = k.sb("prodf", [128, NE])
            for tg in range(NTILE):
                pp = psf.next()
                for t2 in range(tg):
                    k.pe(lambda q, pp=pp, t2=t2: q.matmul(pp[:, 0:NE], lhsT=ones, rhs=mall[:, t2, :], start=(t2 == 0), stop=False), [cst, mall], [pp])
                k.pe(lambda q, pp=pp, tg=tg: q.matmul(pp[:, 0:NE], lhsT=tri, rhs=mall[:, tg, :], start=(tg == 0), stop=True), [cst, mall], [pp])
                k.v(lambda q, pp=pp: q.tensor_tensor(out=basef[:, :], in0=pp[:, 0:NE], in1=cntb[:, 2, :], op=ALU.add), [pp, cntb], [basef])
                for kk in range(TOPK):
                    j = tg * TOPK + kk
                    k.v(lambda q, j=j: q.tensor_tensor(out=prodf[:, :], in0=basef[:, :], in1=ohall[:, j, :], op=ALU.mult), [basef, ohall], [prodf])
                    k.v(lambda q, j=j: q.reduce_sum(out=slotf[:, j:j + 1], in_=prodf[:, :], axis=AX.X), [prodf], [slotf])
            k.v(lambda q: q.tensor_copy(out=slot_i[:, :, 0], in_=slotf[:, :]), [slotf], [slot_i])
            dbg("slotf", slotf[:, :], [128, NTILE * TOPK], slotf)
            dbg("cntb", cntb[:, :, :], [128, 4, NE], cntb)
            dbg("eidf", eidf[:, :, :], [128, NTILE, 8], eidf)
            dbg("gates", gates[:, :, :], [128, NTILE, TOPK], gates)
            dbg("sloti", slot_i[:, :, :], [128, NTILE * TOPK, 1], slot_i, I32)
            dbg("ef", ef[:, :, :], [128, 3, 128], ef)
            dbg("mall", mall[:, :, :], [128, NTILE, NE], mall)
        k.barrier()
        esM.close()
        if mode == "mixer":
            with ExitStack() as esX:
                k.es = esX
                tb = k.sb("tb", [128, D])
                for tg in range(NTILE):
                    k.dma("sp", lambda q, tg=tg: q.dma_start(out=tb[:, :], in_=H1[tg * 128:(tg + 1) * 128, :]), [H1b], [tb])
                    k.dma("sp", lambda q, tg=tg: q.dma_start(out=out_d[tg * 128:(tg + 1) * 128, :], in_=tb[:, :]), [tb], [OUTB])
                k.wait_all("sp", [OUTB])
            k.es = es
            print("instructions:", k.n_ins)
            return nc
        with ExitStack() as es3:
            k.es = es3
            hb = k.ring("hb", [128, D], BF16, 2)
            zt_ = hb.next()
            k.v(lambda q: q.memset(zt_[:, :], 0.0), [], [zt_])
            for s in range(NBLK):
                k.dma("sp", lambda q, s=s: q.dma_start(out=XS[s * 128:(s + 1) * 128, :], in_=zt_[:, :]), [zt_], [XSb])
            for tg in range(NTILE):
                t = hb.next()
                k.dma("pool", lambda q, t=t, tg=tg: q.dma_start(out=t[:, :], in_=H1[tg * 128:(tg + 1) * 128, :]), [H1b], [t])
                for kk in range(TOPK):
                    j = tg * TOPK + kk
                    k.dma("pool", lambda q, t=t, j=j: q.indirect_dma_start(out=XS, out_offset=bass.IndirectOffsetOnAxis(ap=slot_i[:, j, :], axis=0), in_=t[:, :], in_offset=None), [t, slot_i], [XSb])
            wbuf = k.ring("wbuf", [128, FC * 512], BF16, 3)
            b1f = k.sb("b1f", [128, 2 * DE])
            b2f = k.sb("b2f", [128, D])
            xb = k.ring("xb", [128, D], BF16, 2)
            xT = k.sb("xT", [128, FC, 128], BF16)
            act = k.sb("act", [128, DE], BF16)
            actT = k.sb("actT", [128, FC, 128], BF16)
            ob = k.ring("ob", [128, D], F32, 2)
            b1b = k.ring("b1b", [1, 2 * DE], BF16, 1)
            b2b = k.ring("b2b", [1, D], BF16, 1)
            gsb = k.ring("gsb", [128, 4, 512], F32, 2)
            s_g, s_u, s_s, s_p = 0, 1, 2, 3
            for s in range(NBLK):
                x_ = xb.next()
                k.dma("sp", lambda q, x_=x_, s=s: q.dma_start(out=x_[:, :], in_=XS[s * 128:(s + 1) * 128, :]), [XSb], [x_])
                b1 = b1b.next()
                b2 = b2b.next()
                k.dma("pool", lambda q, s=s: q.indirect_dma_start(out=b1f[:, :], out_offset=None, in_=b1_d, in_offset=bass.IndirectOffsetOnAxis(ap=eidx_i[:, s:s + 1], axis=0)), [eidx_i], [b1f])
                k.dma("pool", lambda q, s=s: q.indirect_dma_start(out=b2f[:, :], out_offset=None, in_=b2_d, in_offset=bass.IndirectOffsetOnAxis(ap=eidx_i[:, s:s + 1], axis=0)), [eidx_i], [b2f])
                k.a(lambda q, b1=b1: q.activation(out=b1[:, :], in_=b1f[0:1, :], func=AF.Copy), [b1f], [b1])
                k.a(lambda q, b2=b2: q.activation(out=b2[:, :], in_=b2f[0:1, :], func=AF.Copy), [b2f], [b2])
                for c4 in range(4):
                    hf = psb_half[0]
                    psb_half[0] ^= 1
                    for j in range(4):
                        kc = c4 * 4 + j
                        k.pe(lambda q, kc=kc, hf=hf, j=j, x_=x_: q.transpose(out=psb[:, hf * 512 + j * 128: hf * 512 + (j + 1) * 128], in_=x_[:, kc * 128:(kc + 1) * 128], identity=identb[:, :]), [x_, identb], [psb])
                    k.a(lambda q, c4=c4, hf=hf: q.activation(out=xT[:, c4 * 4:(c4 + 1) * 4, :], in_=psb[:, hf * 512:(hf + 1) * 512].rearrange("p (a b) -> p a b", a=4), func=AF.Copy), [psb], [xT])
                for n in range(4):
                    gs = gsb.next()
                    pgu = []
                    for half in range(2):
                        col0 = half * DE + n * 512
                        w = wbuf.next()
                        n8 = half * 4 + n
                        k.dma("pool", lambda q, w=w, n8=n8, s=s: q.indirect_dma_start(out=w[:, :], out_offset=None, in_=w1_d, in_offset=bass.IndirectOffsetOnAxis(ap=idx1_i[:, n8, s:s + 1], axis=0)), [idx1_i], [w])
                        pp = psf.next()
                        for kc in range(FC):
                            k.pe(lambda q, pp=pp, kc=kc, w=w: q.matmul(pp[:, :], lhsT=xT[:, kc, :], rhs=w[:, kc * 512:(kc + 1) * 512], start=(kc == 0), stop=False), [xT, w], [pp])
                        k.pe(lambda q, pp=pp, col0=col0, b1=b1: q.matmul(pp[:, :], lhsT=onesb[:, :], rhs=b1[:, col0:col0 + 512], start=False, stop=True), [onesb, b1], [pp])
                        pgu.append(pp)
                    k.v(lambda q, gs=gs, pp=pgu[0]: q.tensor_scalar(out=gs[:, s_g, :], in0=pp[:, :], scalar1=7.0, scalar2=None, op0=ALU.min), [pgu[0]], [gs])
                    k.a(lambda q, gs=gs: q.activation(out=gs[:, s_s, :], in_=gs[:, s_g, :], func=AF.Sigmoid, scale=1.702), [gs], [gs])
                    k.v(lambda q, gs=gs, pp=pgu[1]: q.tensor_scalar(out=gs[:, s_u, :], in0=pp[:, :], scalar1=7.0, scalar2=-7.0, op0=ALU.min, op1=ALU.max), [pgu[1]], [gs])
                    k.v(lambda q, gs=gs: q.tensor_tensor(out=gs[:, s_p, :], in0=gs[:, s_g, :], in1=gs[:, s_s, :], op=ALU.mult), [gs], [gs])
                    k.v(lambda q, gs=gs, n=n: q.scalar_tensor_tensor(out=act[:, n * 512:(n + 1) * 512], in0=gs[:, s_u, :], scalar=1.0, in1=gs[:, s_p, :], op0=ALU.add, op1=ALU.mult), [gs], [act])
                for c4 in range(4):
                    hf = psb_half[0]
                    psb_half[0] ^= 1
                    for j in range(4):
                        kc = c4 * 4 + j
                        k.pe(lambda q, kc=kc, hf=hf, j=j: q.transpose(out=psb[:, hf * 512 + j * 128: hf * 512 + (j + 1) * 128], in_=act[:, kc * 128:(kc + 1) * 128], identity=identb[:, :]), [act, identb], [psb])
                    k.a(lambda q, c4=c4, hf=hf: q.activation(out=actT[:, c4 * 4:(c4 + 1) * 4, :], in_=psb[:, hf * 512:(hf + 1) * 512].rearrange("p (a b) -> p a b", a=4), func=AF.Copy), [psb], [actT])
                o_ = ob.next()
                for n in range(4):
                    w = wbuf.next()
                    k.dma("pool", lambda q, w=w, n=n, s=s: q.indirect_dma_start(out=w[:, :], out_offset=None, in_=w2_d, in_offset=bass.IndirectOffsetOnAxis(ap=idx2_i[:, n, s:s + 1], axis=0)), [idx2_i], [w])
                    pp = psf.next()
                    for kc in range(FC):
                        k.pe(lambda q, pp=pp, kc=kc, w=w: q.matmul(pp[:, :], lhsT=actT[:, kc, :], rhs=w[:, kc * 512:(kc + 1) * 512], start=(kc == 0), stop=False), [actT, w], [pp])
                    k.pe(lambda q, pp=pp, n=n, b2=b2: q.matmul(pp[:, :], lhsT=onesb[:, :], rhs=b2[:, n * 512:(n + 1) * 512], start=False, stop=True), [onesb, b2], [pp])
                    k.a(lambda q, pp=pp, n=n, o_=o_: q.activation(out=o_[:, n * 512:(n + 1) * 512], in_=pp[:, :], func=AF.Copy), [pp], [o_])
                k.dma("sp", lambda q, o_=o_, s=s: q.dma_start(out=OS[s * 128:(s + 1) * 128, :], in_=o_[:, :]), [o_], [OSb])
        k.barrier()
        with ExitStack() as es4:
            k.es = es4
            lnp = k.sb("ln2", [128, 2, D])
            k.dma("sp", lambda q: q.dma_start(out=lnp[:, :, :], in_=lnrep[4:6].rearrange("a p d -> p a d")), [], [lnp])
            gth = k.ring("gth", [128, D], F32, 3)
            h1r = k.ring("h1r", [128, D], F32, 2)
            zt = k.sb("zt", [128, D])
            lntmp = k.sb("lntmp2", [128, D])
            st = k.sb("st2", [128, 24])
            mv = k.sb("mv2", [128, 4])
            outr = k.ring("outr", [128, D], F32, 2)
            for tg in range(NTILE):
                hh = h1r.next()
                k.dma("sp", lambda q, hh=hh, tg=tg: q.dma_start(out=hh[:, :], in_=H1[tg * 128:(tg + 1) * 128, :]), [H1b], [hh])
                k.v(lambda q, hh=hh: q.tensor_scalar(out=zt[:, :], in0=hh[:, :], scalar1=float(ALPHA), scalar2=None, op0=ALU.mult), [hh], [zt])
                for kk in range(TOPK):
                    j = tg * TOPK + kk
                    gt = gth.next()
                    k.dma("pool", lambda q, gt=gt, j=j: q.indirect_dma_start(out=gt[:, :], out_offset=None, in_=OS, in_offset=bass.IndirectOffsetOnAxis(ap=slot_i[:, j, :], axis=0)), [OSb, slot_i], [gt])
                    k.v(lambda q, gt=gt, tg=tg, kk=kk: q.scalar_tensor_tensor(out=zt[:, :], in0=gt[:, :], scalar=gates[:, tg, kk:kk + 1], in1=zt[:, :], op0=ALU.mult, op1=ALU.add), [gt, gates, zt], [zt])
                ot = outr.next()
                layer_norm(zt[:, :], zt, ot[:, :], ot, lnp, lnp, 0, 1.0, lntmp, st, mv)
                k.dma("sp", lambda q, ot=ot, tg=tg: q.dma_start(out=out_d[tg * 128:(tg + 1) * 128, :], in_=ot[:, :]), [ot], [OUTB])
            k.wait_all("sp", [OUTB])
        k.es = es
        print("instructions:", k.n_ins)
    return nc


def _consts(NE):
    c = np.zeros((128, 128 * 3 + 64 * 4 + NE + 1), np.float32)
    c[:, 0:128] = np.eye(128)
    c[:, 128:256] = 1.0
    c[:, 256:384] = np.triu(np.ones((128, 128)), 1)
    o = 384
    t = np.arange(64)
    c[0:64, o:o + 64] = (t[:, None] > t[None, :])
    c[0:64, o + 64:o + 128] = (t[None, :] > t[:, None])
    c[0:64, o + 128:o + 192] = (t[None, :] >= t[:, None])
    c[0:64, o + 192:o + 256] = np.eye(64)
    c[:, o + 256:o + 256 + NE] = np.arange(NE)[None, :]
    c[:, o + 256 + NE] = np.arange(128) * 128.0
    return c


def prepare_shared(p, NE):
    f = np.float32
    sh = {}
    ln = np.stack([p["ln_in_g"], p["ln_in_b"], p["ln1_g"][0], p["ln1_b"][0], p["ln2_g"][0], p["ln2_b"][0]])
    sh["lnrep"] = np.ascontiguousarray(np.broadcast_to(ln[:, None, :], (6, 128, D))).astype(f)
    w_in = p["w_in"][0]
    wl = w_in[:, 0:2048].reshape(FC, 128, 16, 128)
    sh["win_lru"] = np.ascontiguousarray(wl.transpose(2, 1, 0, 3))
    rkv = w_in[:, 2048:2048 + 3072].reshape(FC, 128, 3, NH, HS)
    sh["win_rkv"] = np.ascontiguousarray(rkv.transpose(3, 1, 0, 2, 4).reshape(NH, 128, FC, 3 * HS))
    lo = w_in[:, 2048 + 3072:].reshape(FC, 128, 448)
    sh["win_lora"] = np.ascontiguousarray(lo.transpose(1, 0, 2))
    cp = np.stack([p["conv_w"][0][0], p["conv_w"][0][1], p["conv_w"][0][2], p["conv_w"][0][3], p["conv_b"][0], p["b_rgate"][0], p["b_igate"][0], p["lru_lambda"][0]], -1)
    sh["lru_cp"] = np.ascontiguousarray(cp.reshape(8, 128, 8).transpose(1, 0, 2))

    def bd(w):
        o = np.zeros((8, 128, 128), f)
        for n in range(16):
            t, q = n // 2, (n % 2) * 64
            o[t, q:q + 64, q:q + 64] = w[n]
        return o
    sh["wrg_bd"] = bd(p["w_rgate"][0])
    sh["wig_bd"] = bd(p["w_igate"][0])
    mu = p["shift_mu"][0]
    hv = lambda v: v.reshape(NH, HS).T
    cols = [hv(mu[0:1024]), hv(mu[1024:2048]), hv(mu[2048:3072]), hv(p["w0"][0]), hv(p["a0"][0]), hv(p["k_k"][0]), hv(p["k_a"][0]), p["r_k"][0].T, hv(p["gn_g"][0]), hv(p["gn_b"][0])]
    sh["rw_cp"] = np.ascontiguousarray(np.stack(cols, -1)).astype(f)
    ml = np.zeros((128, 4), f)
    ml[0:96, 0] = mu[3072:3168]
    ml[0:96, 1] = mu[3168:3264]
    ml[:, 2] = mu[3264:3392]
    ml[:, 3] = mu[3392:3520]
    sh["mu_lora"] = ml
    sh["dec_up"] = np.ascontiguousarray(p["rw_decay_up"][0].reshape(96, NH, HS).transpose(1, 0, 2))
    sh["aaa_up"] = np.ascontiguousarray(p["rw_aaa_up"][0].reshape(96, NH, HS).transpose(1, 0, 2))
    sh["gate_up"] = np.ascontiguousarray(p["rw_gate_up"][0].reshape(2, 128, NH, HS).transpose(2, 1, 0, 3))
    wo = p["w_out"][0]
    sh["wout_l"] = np.ascontiguousarray(wo[0:1024].reshape(8, 128, 4, 512).transpose(2, 1, 0, 3))
    sh["wout_r"] = np.ascontiguousarray(wo[1024:2048].reshape(NH, HS, 4, 512).transpose(2, 1, 0, 3))
    sh["w_router"] = np.ascontiguousarray(p["w_router"][0].reshape(FC, 128, NE).transpose(1, 0, 2))
    sh["b_router"] = np.ascontiguousarray(np.broadcast_to(p["b_router"][0][None, :], (128, NE))).astype(f)
    sh["w_exp1"] = np.ascontiguousarray(p["w_exp1"][0].reshape(NE, FC, 128, 8, 512).transpose(0, 3, 2, 1, 4)).reshape(NE * 8 * 128, FC * 512)
    sh["b_exp1"] = np.ascontiguousarray(p["b_exp1"][0])
    sh["w_exp2"] = np.ascontiguousarray(p["w_exp2"][0].reshape(NE, FC, 128, 4, 512).transpose(0, 3, 2, 1, 4)).reshape(NE * 4 * 128, FC * 512)
    sh["b_exp2"] = np.ascontiguousarray(p["b_exp2"][0])
    sh["cst"] = _consts(NE)
    return sh


def run(inputs, TH, NE, n_cores, mode="full"):
    x = np.asarray(inputs["x"], np.float32)
    B, T, _ = x.shape
    assert T == 2 * TH and B * 2 == n_cores
    p = {kk: np.asarray(v, np.float32) for kk, v in inputs.items() if kk != "x"}
    sh = prepare_shared(p, NE)
    in_maps = []
    for c in range(n_cores):
        b, j = c // 2, c % 2
        first = x[b, 0:TH]
        second = x[b, j * TH:(j + 1) * TH]
        m = dict(sh)
        m["xa"] = np.ascontiguousarray(np.concatenate([first, second], 0))
        m["flag"] = np.full((128, 1), float(j), np.float32)
        in_maps.append(m)
    if mode != "full":
        for m in in_maps:
            m["w_exp1"] = np.zeros((128, FC * 512), np.float32)
            m["w_exp2"] = np.zeros((128, FC * 512), np.float32)
    nc = build(TH, NE, mode)
    res = run_bass_kernel_spmd(nc, in_maps, core_ids=list(range(n_cores)))
    out = np.zeros((B, T, D), np.float32)
    for c in range(n_cores):
        b, j = c // 2, c % 2
        out[b, j * TH:(j + 1) * TH] = res.results[c]["out"]
    return out


def kernel(**inputs):
    return run(inputs, 2048, 32, 8)
```

```python
from contextlib import ExitStack
import numpy as np
import concourse.bass as bass
import concourse.mybir as mybir
from concourse.bass_utils import run_bass_kernel_spmd

F32 = mybir.dt.float32
BF16 = mybir.dt.bfloat16
I32 = mybir.dt.int32
U32 = mybir.dt.uint32
AF = mybir.ActivationFunctionType
ALU = mybir.AluOpType
AX = mybir.AxisListType

D = 2048
FC = 16
LRU_W = 1024
NH = 16
HS = 64
G = 512
TOPK = 4
DE = 2048
ALPHA = 2.0 ** 0.25
LN_EPS = 1e-5
GN_EPS = 64 * 1e-5
EPOCH = 4000
NDS = 12
DEBUG = False


class Reg:
    __slots__ = ("w", "r")

    def __init__(self):
        self.w = None
        self.r = {}


class Buf:
    def __init__(self, t):
        self.t = t
        self.reg = Reg()

    def __getitem__(self, k):
        return self.t[k]


class Ring:
    def __init__(self, bufs):
        self.bufs = bufs
        self.i = 0

    def next(self):
        b = self.bufs[self.i]
        self.i = (self.i + 1) % len(self.bufs)
        return b


class KB:
    def __init__(self, nc, es):
        self.nc = nc
        self.es = es
        self.es_sem = es
        self.uid = 0
        self.eng = {"sp": nc.sync, "act": nc.scalar, "dve": nc.vector, "pool": nc.gpsimd, "pe": nc.tensor}
        self.sems = []
        self.esem = {}
        self.cnt = {}
        self.waited = {e: {} for e in self.eng}
        for e in self.eng:
            self._new_epoch(e)
        self.dsem = [self._sem("d%d" % i) for i in range(2 * NDS)]
        self.dval = [0] * (2 * NDS)
        self.drr = {"hw": 0, "sw": 0}
        self.n_ins = 0

    def _sem(self, name):
        s = self.es_sem.enter_context(self.nc.semaphore(name))
        self.sems.append(s)
        return len(self.sems) - 1

    def _new_epoch(self, e):
        self.esem[e] = self._sem("e_%s_%d" % (e, len(self.sems)))
        self.cnt[e] = 0

    def sb(self, name, shape, dt=F32):
        self.uid += 1
        return Buf(self.es.enter_context(self.nc.sbuf_tensor("%s_%d" % (name, self.uid), shape, dt)))

    def barrier(self):
        toks = [(self.esem[f], self.cnt[f]) for f in self.eng if self.cnt[f] > 0]
        toks += [(self.dsem[i], self.dval[i]) for i in range(2 * NDS) if self.dval[i] > 0]
        for e in self.eng:
            for (s_, v_) in toks:
                if self.waited[e].get(s_, 0) < v_:
                    self.eng[e].wait_ge(self.sems[s_], v_)
                    self.waited[e][s_] = v_
                    self.n_ins += 1

    def ring(self, name, shape, dt, n):
        return Ring([self.sb("%s%d" % (name, i), shape, dt) for i in range(n)])

    def _deps(self, e, reads, writes, extra=()):
        deps = list(extra)
        for r in reads:
            if r.reg.w is not None:
                deps.append(r.reg.w)
        for w in writes:
            if w.reg.w is not None:
                deps.append(w.reg.w)
            deps.extend(w.reg.r.items())
        for (s, v) in deps:
            if e == "pe" and s == self.esem["pe"]:
                continue
            if self.waited[e].get(s, 0) < v:
                self.eng[e].wait_ge(self.sems[s], v)
                self.waited[e][s] = v
                self.n_ins += 1

    def _record(self, tok, reads, writes):
        s, v = tok
        for r in reads:
            if r.reg.r.get(s, 0) < v:
                r.reg.r[s] = v
        for w in writes:
            w.reg.w = tok
            w.reg.r = {}

    def op(self, e, fn, reads=(), writes=()):
        self._deps(e, reads, writes)
        if self.cnt[e] >= EPOCH:
            self._new_epoch(e)
        ins = fn(self.eng[e])
        self.cnt[e] += 1
        s = self.esem[e]
        ins.then_inc(self.sems[s], 1)
        self.n_ins += 1
        self._record((s, self.cnt[e]), reads, writes)

    def dma(self, e, fn, reads=(), writes=()):
        kind = "sw" if e == "pool" else "hw"
        k = self.drr[kind] + (NDS if kind == "sw" else 0)
        self.drr[kind] = (self.drr[kind] + 1) % NDS
        extra = [(self.dsem[k], self.dval[k])] if self.dval[k] > 0 else []
        self._deps(e, reads, writes, extra)
        ins = fn(self.eng[e])
        self.dval[k] += 16
        ins.then_inc(self.sems[self.dsem[k]], 16)
        self.n_ins += 1
        self._record((self.dsem[k], self.dval[k]), reads, writes)

    def wait_all(self, e, bufs):
        self._deps(e, bufs, bufs)

    def v(self, fn, r=(), w=()):
        self.op("dve", fn, r, w)

    def a(self, fn, r=(), w=()):
        self.op("act", fn, r, w)

    def pe(self, fn, r=(), w=()):
        self.op("pe", fn, r, w)


def build(TH, NE, mode="full"):
    NT = 2 * TH
    NG = NT // G
    NTILE = TH // 128
    NBLK = TH * TOPK // 128 + NE
    nc = bass.Bass("TRN2", target_bir_lowering=False)

    def din(name, shape, dt=F32):
        return nc.dram_tensor(name, list(shape), dt, kind="ExternalInput").ap()

    xa = din("xa", [NT, D])
    flag_d = din("flag", [128, 1])
    lnrep = din("lnrep", [6, 128, D])
    win_lru = din("win_lru", [16, 128, FC, 128])
    win_rkv = din("win_rkv", [NH, 128, FC, 3 * HS])
    win_lora = din("win_lora", [128, FC, 448])
    lru_cp_d = din("lru_cp", [128, 8, 8])
    wrg_d = din("wrg_bd", [8, 128, 128])
    wig_d = din("wig_bd", [8, 128, 128])
    rw_cp_d = din("rw_cp", [HS, NH, 10])
    mu_lora_d = din("mu_lora", [128, 4])
    dup_d = din("dec_up", [NH, 96, HS])
    aup_d = din("aaa_up", [NH, 96, HS])
    gup_d = din("gate_up", [NH, 128, 2, HS])
    wout_l = din("wout_l", [4, 128, 8, 512])
    wout_r = din("wout_r", [4, HS, NH, 512])
    wr_d = din("w_router", [128, FC, NE])
    br_d = din("b_router", [128, NE])
    w1_d = din("w_exp1", [NE * 8 * 128 if mode == "full" else 128, FC * 512])
    b1_d = din("b_exp1", [NE, 2 * DE])
    w2_d = din("w_exp2", [NE * 4 * 128 if mode == "full" else 128, FC * 512])
    b2_d = din("b_exp2", [NE, D])
    cst_d = din("cst", [128, 128 * 3 + 64 * 4 + NE + 1 + 4 * 512])
    out_d = nc.dram_tensor("out", [TH, D], F32, kind="ExternalOutput").ap()
    if mode == "mixer" and DEBUG:
        dbg_yl = nc.dram_tensor("dbg_yl", [128, 8, G], BF16, kind="ExternalOutput").ap()
        dbg_yr = nc.dram_tensor("dbg_yr", [HS, NH, G], BF16, kind="ExternalOutput").ap()
        dbg_hT = nc.dram_tensor("dbg_hT", [128, FC, G], BF16, kind="ExternalOutput").ap()
    H1 = nc.dram_tensor("H1", [TH, D], F32, kind="Internal").ap()
    XS = nc.dram_tensor("XS", [NBLK * 128, D], BF16, kind="Internal").ap()
    OS = nc.dram_tensor("OS", [NBLK * 128, D], F32, kind="Internal").ap()
    BLKE = nc.dram_tensor("BLKE", [128, 1], I32, kind="Internal").ap()

    with ExitStack() as es:
        k = KB(nc, es)
        dbg_list = []

        def dbg(name, ap, shape, buf, dt=F32):
            if not DEBUG:
                return
            t_ = nc.dram_tensor("dbg_" + name, list(shape), dt, kind="ExternalOutput").ap()
            b_ = Buf(t_)
            dbg_list.append(b_)
            k.dma("sp", lambda q: q.dma_start(out=t_, in_=ap), [buf], [b_])

        H1b, XSb, OSb, BLKEb, OUTB = Buf(H1), Buf(XS), Buf(OS), Buf(BLKE), Buf(out_d)
        psf = Ring([Buf(es.enter_context(nc.psum_tensor("psf%d" % i, [128, 512], F32))) for i in range(6)])
        pyb = Buf(es.enter_context(nc.psum_tensor("pyb", [128, 512], F32)))
        psb = Buf(es.enter_context(nc.psum_tensor("psb", [128, 1024], BF16)))
        psb_half = [0]
        NC_C = 128 * 3 + 64 * 4 + NE + 1 + 4 * 512
        cst = k.sb("cst", [128, NC_C])
        k.dma("sp", lambda q: q.dma_start(out=cst[:, :], in_=cst_d), [], [cst])
        ident = cst[:, 0:128]
        ones = cst[:, 128:256]
        tri = cst[:, 256:384]
        o = 384
        m_sl = cst[0:64, o:o + 64]
        m_su = cst[0:64, o + 64:o + 128]
        m_ui = cst[0:64, o + 128:o + 192]
        i64 = cst[0:64, o + 192:o + 256]
        iota_e = cst[:, o + 256:o + 256 + NE]
        iota_p = cst[:, o + 256 + NE:o + 257 + NE]
        ones64 = cst[0:64, 128:192]
        o8 = o + 257 + NE
        r3 = lambda ap: ap.rearrange("p (a b) -> p a b", a=8)
        m_sl8 = r3(cst[0:64, o8:o8 + 512])
        m_su8 = r3(cst[0:64, o8 + 512:o8 + 1024])
        m_ui8 = r3(cst[0:64, o8 + 1024:o8 + 1536])
        i64_8 = r3(cst[0:64, o8 + 1536:o8 + 2048])
        mask8 = {id(m_sl): m_sl8, id(m_su): m_su8, id(m_ui): m_ui8}
        identb = k.sb("identb", [128, 128], BF16)
        k.v(lambda q: q.tensor_copy(out=identb[:, :], in_=ident), [cst], [identb])
        onesb = k.sb("onesb", [1, 128], BF16)
        k.v(lambda q: q.tensor_copy(out=onesb[:, :], in_=cst[0:1, 128:256]), [cst], [onesb])
        flag = k.sb("flagt", [128, 1])
        k.dma("sp", lambda q: q.dma_start(out=flag[:, :], in_=flag_d), [], [flag])
        eidf = k.sb("eidf", [128, NTILE, 8])
        gates = k.sb("gates", [128, NTILE, TOPK])
        ohall = k.sb("ohall", [128, NTILE * TOPK, NE])
        mall = k.sb("mall", [128, NTILE, NE])
        slot_i = k.sb("slot_i", [128, NTILE * TOPK, 1], I32)
        idx1_i = k.sb("idx1_i", [128, 8, 128], I32)
        idx2_i = k.sb("idx2_i", [128, 4, 128], I32)
        eidx_i = k.sb("eidx_i", [128, 128], I32)

        def layer_norm(src, srcbuf, dst, dstbuf, gb, gbbuf, gi, scale, tmp, st, mv):
            for c in range(4):
                k.v(lambda q, c=c: q.bn_stats(out=st[:, c * 6:(c + 1) * 6], in_=src[:, c * 512:(c + 1) * 512]), [srcbuf], [st])
            k.v(lambda q: q.bn_aggr(out=mv[:, 0:2], in_=st[:, 0:24]), [st], [mv])
            k.v(lambda q: q.tensor_scalar(out=mv[:, 2:3], in0=mv[:, 1:2], scalar1=LN_EPS, scalar2=None, op0=ALU.add), [mv], [mv])
            k.a(lambda q: q.activation(out=mv[:, 2:3], in_=mv[:, 2:3], func=AF.Sqrt), [mv], [mv])
            k.v(lambda q: q.reciprocal(out=mv[:, 2:3], in_=mv[:, 2:3]), [mv], [mv])
            k.v(lambda q: q.tensor_scalar(out=tmp[:, :], in0=src, scalar1=mv[:, 0:1], scalar2=mv[:, 2:3], op0=ALU.subtract, op1=ALU.mult), [srcbuf, mv], [tmp])
            k.v(lambda q: q.tensor_tensor(out=tmp[:, :], in0=tmp[:, :], in1=gb[:, gi, :], op=ALU.mult), [tmp, gbbuf], [tmp])
            if scale == 1.0:
                k.v(lambda q: q.tensor_tensor(out=dst, in0=tmp[:, :], in1=gb[:, gi + 1, :], op=ALU.add), [tmp, gbbuf], [dstbuf])
            else:
                k.v(lambda q: q.tensor_tensor(out=tmp[:, :], in0=tmp[:, :], in1=gb[:, gi + 1, :], op=ALU.add), [tmp, gbbuf], [tmp])
                k.v(lambda q: q.tensor_scalar(out=dst, in0=tmp[:, :], scalar1=float(scale), scalar2=None, op0=ALU.mult), [tmp], [dstbuf])

        esM = ExitStack()
        k.es = esM
        hT = k.sb("hT", [128, FC, G], BF16)
        lact = k.sb("lact", [128, 4, G])
        S = k.sb("S", [HS, NH, HS])
        yl = k.sb("yl", [128, 8, G], BF16)
        yr = k.sb("yr", [HS, NH, G], BF16)
        hist = k.sb("hist", [HS, NH, 3])
        hstate = k.sb("hstate", [128, 8])
        uhist = k.sb("uhist", [128, 8, 3])
        lhist = k.sb("lhist", [128, 4])
        for t_ in (S, hist, hstate, uhist, lhist):
            k.v(lambda q, t_=t_: q.memset(t_.t[:], 0.0), [], [t_])
        lru_cp = k.sb("lru_cp", [128, 8, 8])
        k.dma("sp", lambda q: q.dma_start(out=lru_cp[:, :, :], in_=lru_cp_d), [], [lru_cp])
        lru_c = k.sb("lru_c", [128, 8, 2])
        k.a(lambda q: q.activation(out=lru_c[:, :, 0], in_=lru_cp[:, :, 7], func=AF.Exp, scale=-1.0), [lru_cp], [lru_c])
        k.a(lambda q: q.activation(out=lru_c[:, :, 1], in_=lru_c[:, :, 0], func=AF.Ln, bias=1.0), [lru_c], [lru_c])
        k.v(lambda q: q.tensor_scalar(out=lru_c[:, :, 0], in0=lru_c[:, :, 1], scalar1=-8.0, scalar2=None, op0=ALU.mult), [lru_c], [lru_c])
        k.v(lambda q: q.tensor_scalar(out=lru_c[:, :, 1], in0=lru_c[:, :, 0], scalar1=2.0, scalar2=None, op0=ALU.mult), [lru_c], [lru_c])
        wrg = k.sb("wrg", [128, 8, 128])
        wig = k.sb("wig", [128, 8, 128])
        k.dma("sp", lambda q: q.dma_start(out=wrg[:, :, :], in_=wrg_d.rearrange("n p m -> p n m")), [], [wrg])
        k.dma("sp", lambda q: q.dma_start(out=wig[:, :, :], in_=wig_d.rearrange("n p m -> p n m")), [], [wig])
        rw_cp = k.sb("rw_cp", [HS, NH, 10])
        k.dma("sp", lambda q: q.dma_start(out=rw_cp[:, :, :], in_=rw_cp_d), [], [rw_cp])
        mu_lora = k.sb("mu_lora", [128, 4])
        k.dma("sp", lambda q: q.dma_start(out=mu_lora[:, :], in_=mu_lora_d), [], [mu_lora])
        wr = k.sb("wr", [128, FC, NE])
        brt = k.sb("brt", [128, NE])
        k.dma("sp", lambda q: q.dma_start(out=wr[:, :, :], in_=wr_d), [], [wr])
        k.dma("sp", lambda q: q.dma_start(out=brt[:, :], in_=br_d), [], [brt])
        st = k.sb("st", [128, 24])
        mv = k.sb("mv", [128, 4])
        lg = k.sb("lg", [128, NE])
        m8 = k.sb("m8", [128, 8])
        i8 = k.sb("i8", [128, 8], U32)
        sm = k.sb("sm", [128, 8])

        def proj(wt, col0, M, out_ps):
            for kc in range(FC):
                k.pe(lambda q, kc=kc: q.matmul(out_ps[0:M, :], lhsT=wt[:, kc, col0:col0 + M], rhs=hT[:, kc, :], start=(kc == 0), stop=(kc == FC - 1)), [wt, hT], [out_ps])

        def early_stop():
            k.barrier()
            with ExitStack() as esX:
                k.es = esX
                tb = k.sb("tbx", [128, D])
                for tg in range(NTILE):
                    k.dma("sp", lambda q, tg=tg: q.dma_start(out=tb[:, :], in_=xa[tg * 128:(tg + 1) * 128, :]), [], [tb])
                    k.dma("sp", lambda q, tg=tg: q.dma_start(out=out_d[tg * 128:(tg + 1) * 128, :], in_=tb[:, :]), [tb], [OUTB])
                k.wait_all("sp", [OUTB])
            esM.close()
            print("instructions:", k.n_ins)
            return nc

        for g in range(NG):
            t0g = g * G
            out_grp = g >= NG // 2
            with ExitStack() as s1:
                k.es = s1
                lnin = k.sb("lnin", [128, 2, D])
                k.dma("sp", lambda q: q.dma_start(out=lnin[:, :, :], in_=lnrep[0:2].rearrange("a p d -> p a d")), [], [lnin])
                xt = k.sb("xt", [128, D])
                lntmp = k.sb("lntmp", [128, D])
                h0b = k.sb("h0b", [128, D], BF16)
                wlr = k.ring("wlr", [128, FC, 128], BF16, 3)
                wlora = k.sb("wlora", [128, FC, 448], BF16)
                k.dma("pool", lambda q: q.dma_start(out=wlora[:, :, :], in_=win_lora), [], [wlora])
                lw = [k.sb("lw%d" % i, [128, G]) for i in range(6)]
                ub = k.sb("ub", [128, 3 + G])
                lb = k.sb("lb", [128, 1 + G])
                for ti in range(4):
                    r0 = t0g + ti * 128
                    k.dma("sp", lambda q, r0=r0: q.dma_start(out=xt[:, :], in_=xa[r0:r0 + 128, :]), [], [xt])
                    layer_norm(xt[:, :], xt, h0b[:, :], h0b, lnin, lnin, 0, 1.0, lntmp, st, mv)
                    for c4 in range(4):
                        hf = psb_half[0]
                        psb_half[0] ^= 1
                        for j in range(4):
                            kc = c4 * 4 + j
                            k.pe(lambda q, kc=kc, hf=hf, j=j: q.transpose(out=psb[:, hf * 512 + j * 128: hf * 512 + (j + 1) * 128], in_=h0b[:, kc * 128:(kc + 1) * 128], identity=identb[:, :]), [h0b, identb], [psb])
                        k.a(lambda q, c4=c4, hf=hf, ti=ti: q.activation(out=hT[:, c4 * 4:(c4 + 1) * 4, ti * 128:(ti + 1) * 128], in_=psb[:, hf * 512:(hf + 1) * 512].rearrange("p (a b) -> p a b", a=4), func=AF.Copy), [psb], [hT])
                if g == NG // 2:
                    for t_, np_ in ((hstate, 128), (uhist, 128), (lhist, 128), (hist, HS), (S, HS)):
                        k.v(lambda q, t_=t_, np_=np_: q.tensor_scalar(out=t_.t[:], in0=t_.t[:], scalar1=flag[0:np_, 0:1], scalar2=None, op0=ALU.mult), [t_, flag], [t_])
                for ti in range(8):
                    wt = wlr.next()
                    k.dma("pool", lambda q, wt=wt, ti=ti: q.dma_start(out=wt[:, :, :], in_=win_lru[ti]), [], [wt])
                    if out_grp:
                        wt2 = wlr.next()
                        k.dma("pool", lambda q, wt2=wt2, ti=ti: q.dma_start(out=wt2[:, :, :], in_=win_lru[8 + ti]), [], [wt2])
                    pu = psf.next()
                    proj(wt, 0, 128, pu)
                    k.v(lambda q, ti=ti: q.tensor_copy(out=ub[:, 0:3], in_=uhist[:, ti, :]), [uhist], [ub])
                    k.a(lambda q, pu=pu: q.activation(out=ub[:, 3:3 + G], in_=pu[:, :], func=AF.Copy), [pu], [ub])
                    k.v(lambda q, ti=ti: q.tensor_copy(out=uhist[:, ti, :], in_=ub[:, G:G + 3]), [ub], [uhist])
                    if out_grp:
                        pg = psf.next()
                        proj(wt2, 0, 128, pg)
                    uc, rr, ii, aa, bb, gg = lw
                    k.v(lambda q, ti=ti: q.tensor_scalar(out=uc[:, :], in0=ub[:, 0:G], scalar1=lru_cp[:, ti, 0:1], scalar2=lru_cp[:, ti, 4:5], op0=ALU.mult, op1=ALU.add), [ub, lru_cp], [uc])
                    for j in range(1, 4):
                        k.v(lambda q, ti=ti, j=j: q.scalar_tensor_tensor(out=uc[:, :], in0=ub[:, j:j + G], scalar=lru_cp[:, ti, j:j + 1], in1=uc[:, :], op0=ALU.mult, op1=ALU.add), [ub, lru_cp, uc], [uc])
                    pr = psf.next()
                    k.pe(lambda q, ti=ti, pr=pr: q.matmul(pr[:, :], lhsT=wrg[:, ti, :], rhs=uc[:, :], start=True, stop=True), [wrg, uc], [pr])
                    pi = psf.next()
                    k.pe(lambda q, ti=ti, pi=pi: q.matmul(pi[:, :], lhsT=wig[:, ti, :], rhs=uc[:, :], start=True, stop=True), [wig, uc], [pi])
                    k.a(lambda q, ti=ti, pr=pr: q.activation(out=rr[:, :], in_=pr[:, :], func=AF.Sigmoid, bias=lru_cp[:, ti, 5:6]), [pr, lru_cp], [rr])
                    k.a(lambda q, ti=ti, pi=pi: q.activation(out=ii[:, :], in_=pi[:, :], func=AF.Sigmoid, bias=lru_cp[:, ti, 6:7]), [pi, lru_cp], [ii])
                    k.a(lambda q, ti=ti: q.activation(out=aa[:, :], in_=rr[:, :], func=AF.Exp, scale=lru_c[:, ti, 0:1]), [rr, lru_c], [aa])
                    k.a(lambda q, ti=ti: q.activation(out=bb[:, :], in_=rr[:, :], func=AF.Exp, scale=lru_c[:, ti, 1:2]), [rr, lru_c], [bb])
                    k.v(lambda q: q.tensor_scalar(out=bb[:, :], in0=bb[:, :], scalar1=-1.0, scalar2=1.0, op0=ALU.mult, op1=ALU.add), [bb], [bb])
                    k.a(lambda q: q.activation(out=bb[:, :], in_=bb[:, :], func=AF.Sqrt), [bb], [bb])
                    k.v(lambda q: q.tensor_tensor(out=ii[:, :], in0=ii[:, :], in1=uc[:, :], op=ALU.mult), [ii, uc], [ii])
                    k.v(lambda q: q.tensor_tensor(out=bb[:, :], in0=bb[:, :], in1=ii[:, :], op=ALU.mult), [bb, ii], [bb])
                    k.v(lambda q, ti=ti: q.tensor_tensor_scan(out=rr[:, :], data0=aa[:, :], data1=bb[:, :], initial=hstate[:, ti:ti + 1], op0=ALU.mult, op1=ALU.add), [aa, bb, hstate], [rr])
                    k.v(lambda q, ti=ti: q.tensor_copy(out=hstate[:, ti:ti + 1], in_=rr[:, G - 1:G]), [rr], [hstate])
                    if not out_grp:
                        continue
                    k.a(lambda q, pg=pg: q.activation(out=gg[:, :], in_=pg[:, :], func=AF.Copy), [pg], [gg])
                    k.v(lambda q: q.tensor_tensor(out=aa[:, :], in0=gg[:, :], in1=gg[:, :], op=ALU.mult), [gg], [aa])
                    k.v(lambda q: q.tensor_scalar(out=aa[:, :], in0=aa[:, :], scalar1=0.044715, scalar2=1.0, op0=ALU.mult, op1=ALU.add), [aa], [aa])
                    k.v(lambda q: q.tensor_tensor(out=aa[:, :], in0=aa[:, :], in1=gg[:, :], op=ALU.mult), [aa, gg], [aa])
                    k.a(lambda q: q.activation(out=aa[:, :], in_=aa[:, :], func=AF.Sigmoid, scale=1.5957691216), [aa], [aa])
                    k.v(lambda q: q.tensor_tensor(out=aa[:, :], in0=aa[:, :], in1=gg[:, :], op=ALU.mult), [aa, gg], [aa])
                    if g == NG // 2 and ti == 0:
                        dbg("h", rr[:, :], [128, G], rr)
                        dbg("gelu", aa[:, :], [128, G], aa)
                        dbg("b", bb[:, :], [128, G], bb)
                        dbg("uc", uc[:, :], [128, G], uc)
                        dbg("ub", ub[:, :], [128, 3 + G], ub)
                    k.v(lambda q, ti=ti: q.tensor_tensor(out=yl[:, ti, :], in0=aa[:, :], in1=rr[:, :], op=ALU.mult), [aa, rr], [yl])
                for li, (c0, M) in enumerate([(0, 96), (96, 96), (192, 128), (320, 128)]):
                    pp = psf.next()
                    proj(wlora, c0, M, pp)
                    k.v(lambda q, li=li, M=M: q.tensor_copy(out=lb[0:M, 0:1], in_=lhist[0:M, li:li + 1]), [lhist], [lb])
                    k.a(lambda q, pp=pp, M=M: q.activation(out=lb[0:M, 1:1 + G], in_=pp[0:M, :], func=AF.Copy), [pp], [lb])
                    k.v(lambda q, li=li, M=M: q.tensor_copy(out=lhist[0:M, li:li + 1], in_=lb[0:M, G:G + 1]), [lb], [lhist])
                    tt = lw[0]
                    k.v(lambda q, M=M: q.tensor_tensor(out=tt[0:M, :], in0=lb[0:M, 0:G], in1=lb[0:M, 1:1 + G], op=ALU.subtract), [lb], [tt])
                    k.v(lambda q, li=li, M=M: q.scalar_tensor_tensor(out=tt[0:M, :], in0=tt[0:M, :], scalar=mu_lora[0:M, li:li + 1], in1=lb[0:M, 1:1 + G], op0=ALU.mult, op1=ALU.add), [tt, mu_lora, lb], [tt])
                    fn = [AF.Tanh, AF.Copy, AF.Sigmoid, AF.Sigmoid][li]
                    k.a(lambda q, li=li, M=M, fn=fn: q.activation(out=lact[0:M, li, :], in_=tt[0:M, :], func=fn), [tt], [lact])
            k.barrier()
            if mode == "s1":
                return early_stop()
            with ExitStack() as sA:
                k.es = sA
                wlr = k.ring("wrk", [128, FC, 192], BF16, 2)
                dupr = k.ring("dupr", [96, HS], F32, 2)
                aupr = k.ring("aupr", [96, HS], F32, 2)
                gupr = k.ring("gupr", [128, 2, HS], F32, 2)
                pb = k.sb("pb", [HS, 3, 1 + G])
                hw = [k.sb("hw%d" % i, [HS, G]) for i in range(17)]
                (r_, k_, v_, kk_, a_, g_, lgw, L_, eL, Rp, Kpp, b_, Ke, Be, t0, t1, ysb) = hw
                vtm = k.sb("vtm", [HS, 8, HS])
                ketm = k.sb("ketm", [HS, 8, HS])
                betm = k.sb("betm", [HS, 8, HS])
                kktm = k.sb("kktm", [HS, 8, HS])
                P = [k.sb("P%d" % i, [HS, 8, HS]) for i in range(2)]
                Q = [k.sb("Q%d" % i, [HS, 8, HS]) for i in range(2)]
                PI = k.sb("PI", [HS, 8, HS])
                Z = [k.sb("Z%d" % i, [HS, 8, HS]) for i in range(2)]
                MT = k.sb("MT", [HS, 8, HS])
                GT = k.sb("GT", [HS, 8, HS])
                HT = k.sb("HT", [HS, 8, HS])
                Xsb = k.sb("Xsb", [HS, HS])
                NU = k.sb("NU", [HS, HS])
                for h in range(NH):
                    wt = wlr.next()
                    k.dma("pool", lambda q, wt=wt, h=h: q.dma_start(out=wt[:, :, :], in_=win_rkv[h]), [], [wt])
                    du, au, gu = dupr.next(), aupr.next(), gupr.next()
                    k.dma("sp", lambda q, du=du, h=h: q.dma_start(out=du[:, :], in_=dup_d[h]), [], [du])
                    k.dma("sp", lambda q, au=au, h=h: q.dma_start(out=au[:, :], in_=aup_d[h]), [], [au])
                    k.dma("sp", lambda q, gu=gu, h=h: q.dma_start(out=gu[:, :, :], in_=gup_d[h]), [], [gu])
                    k.v(lambda q, h=h: q.tensor_copy(out=pb[:, :, 0], in_=hist[:, h, :]), [hist], [pb])
                    for wi in range(3):
                        pp = psf.next()
                        proj(wt, wi * HS, HS, pp)
                        k.a(lambda q, pp=pp, wi=wi: q.activation(out=pb[:, wi, 1:1 + G], in_=pp[0:HS, :], func=AF.Copy), [pp], [pb])
                    k.v(lambda q, h=h: q.tensor_copy(out=hist[:, h, :], in_=pb[:, :, G]), [pb], [hist])
                    for wi, dst in enumerate([r_, k_, v_]):
                        k.v(lambda q, wi=wi: q.tensor_tensor(out=t0[:, :], in0=pb[:, wi, 0:G], in1=pb[:, wi, 1:1 + G], op=ALU.subtract), [pb], [t0])
                        k.v(lambda q, wi=wi, dst=dst, h=h: q.scalar_tensor_tensor(out=dst[:, :], in0=t0[:, :], scalar=rw_cp[:, h, wi:wi + 1], in1=pb[:, wi, 1:1 + G], op0=ALU.mult, op1=ALU.add), [t0, rw_cp, pb], [dst])
                    if mode == "p1" and h == 0:
                        sA.close()
                        return early_stop()
                    pp = psf.next()
                    k.pe(lambda q, pp=pp, du=du: q.matmul(pp[0:HS, :], lhsT=du[:, :], rhs=lact[0:96, 0, :], start=True, stop=True), [du, lact], [pp])
                    k.a(lambda q, pp=pp, h=h: q.activation(out=lgw[:, :], in_=pp[0:HS, :], func=AF.Sigmoid, bias=rw_cp[:, h, 3:4]), [pp, rw_cp], [lgw])
                    k.v(lambda q: q.tensor_scalar(out=lgw[:, :], in0=lgw[:, :], scalar1=-0.6065306597126334, scalar2=None, op0=ALU.mult), [lgw], [lgw])
                    pp = psf.next()
                    k.pe(lambda q, pp=pp, au=au: q.matmul(pp[0:HS, :], lhsT=au[:, :], rhs=lact[0:96, 1, :], start=True, stop=True), [au, lact], [pp])
                    k.a(lambda q, pp=pp, h=h: q.activation(out=a_[:, :], in_=pp[0:HS, :], func=AF.Sigmoid, bias=rw_cp[:, h, 4:5]), [pp, rw_cp], [a_])
                    pp = psf.next()
                    for j in range(2):
                        k.pe(lambda q, pp=pp, j=j, gu=gu: q.matmul(pp[0:HS, :], lhsT=gu[:, j, :], rhs=lact[:, 2 + j, :], start=(j == 0), stop=(j == 1)), [gu, lact], [pp])
                    k.a(lambda q, pp=pp: q.activation(out=g_[:, :], in_=pp[0:HS, :], func=AF.Copy), [pp], [g_])
                    if mode == "p2" and h == 0:
                        sA.close()
                        return early_stop()
                    k.v(lambda q, h=h: q.tensor_scalar(out=kk_[:, :], in0=k_[:, :], scalar1=rw_cp[:, h, 5:6], scalar2=None, op0=ALU.mult), [k_, rw_cp], [kk_])
                    k.v(lambda q: q.tensor_tensor(out=t0[:, :], in0=kk_[:, :], in1=kk_[:, :], op=ALU.mult), [kk_], [t0])
                    pp = psf.next()
                    k.pe(lambda q, pp=pp: q.matmul(pp[0:HS, :], lhsT=ones64, rhs=t0[:, :], start=True, stop=True), [cst, t0], [pp])
                    k.v(lambda q, pp=pp: q.tensor_scalar(out=t1[:, :], in0=pp[0:HS, :], scalar1=1e-24, scalar2=None, op0=ALU.max), [pp], [t1])
                    k.a(lambda q: q.activation(out=t1[:, :], in_=t1[:, :], func=AF.Sqrt), [t1], [t1])
                    k.v(lambda q: q.reciprocal(out=t1[:, :], in_=t1[:, :]), [t1], [t1])
                    k.v(lambda q: q.tensor_tensor(out=kk_[:, :], in0=kk_[:, :], in1=t1[:, :], op=ALU.mult), [kk_, t1], [kk_])
                    k.v(lambda q, h=h: q.tensor_scalar(out=t0[:, :], in0=a_[:, :], scalar1=1.0, scalar2=rw_cp[:, h, 6:7], op0=ALU.subtract, op1=ALU.mult), [a_, rw_cp], [t0])
                    k.v(lambda q: q.scalar_tensor_tensor(out=k_[:, :], in0=t0[:, :], scalar=1.0, in1=k_[:, :], op0=ALU.add, op1=ALU.mult), [t0, k_], [k_])
                    k.v(lambda q: q.tensor_tensor(out=b_[:, :], in0=kk_[:, :], in1=a_[:, :], op=ALU.mult), [kk_, a_], [b_])
                    if mode == "p3" and h == 0:
                        sA.close()
                        return early_stop()
                    for c in range(8):
                        cs = slice(c * HS, (c + 1) * HS)
                        k.v(lambda q, cs=cs: q.tensor_tensor_scan(out=L_[:, cs], data0=ones64, data1=lgw[:, cs], initial=0.0, op0=ALU.mult, op1=ALU.add), [cst, lgw], [L_])
                    k.a(lambda q: q.activation(out=eL[:, :], in_=L_[:, :], func=AF.Exp), [L_], [eL])
                    k.v(lambda q: q.tensor_tensor(out=Rp[:, :], in0=r_[:, :], in1=eL[:, :], op=ALU.mult), [r_, eL], [Rp])
                    k.v(lambda q: q.tensor_tensor(out=t0[:, :], in0=L_[:, :], in1=lgw[:, :], op=ALU.subtract), [L_, lgw], [t0])
                    k.a(lambda q: q.activation(out=a_[:, :], in_=t0[:, :], func=AF.Exp), [t0], [a_])
                    k.v(lambda q: q.tensor_tensor(out=kk_[:, :], in0=kk_[:, :], in1=a_[:, :], op=ALU.mult), [kk_, a_], [kk_])
                    KKp = kk_
                    for c in range(8):
                        cs = slice(c * HS, (c + 1) * HS)
                        k.a(lambda q, cs=cs, c=c: q.activation(out=t1[:, cs], in_=L_[:, cs], func=AF.Exp, scale=-1.0, bias=L_[:, c * HS + HS - 1:c * HS + HS]), [L_], [t1])
                    k.v(lambda q: q.tensor_tensor(out=Ke[:, :], in0=k_[:, :], in1=t1[:, :], op=ALU.mult), [k_, t1], [Ke])
                    k.v(lambda q: q.tensor_tensor(out=Be[:, :], in0=b_[:, :], in1=t1[:, :], op=ALU.mult), [b_, t1], [Be])
                    k.a(lambda q: q.activation(out=a_[:, :], in_=L_[:, :], func=AF.Exp, scale=-1.0), [L_], [a_])
                    k.v(lambda q: q.tensor_tensor(out=Kpp[:, :], in0=k_[:, :], in1=a_[:, :], op=ALU.mult), [k_, a_], [Kpp])
                    k.v(lambda q: q.tensor_tensor(out=b_[:, :], in0=b_[:, :], in1=a_[:, :], op=ALU.mult), [b_, a_], [b_])
                    Bpp = b_
                    if mode == "p4" and h == 0:
                        sA.close()
                        return early_stop()
                    for src, dst in ((v_, vtm), (Ke, ketm), (Be, betm), (KKp, kktm)):
                        pp = psf.next()
                        for c in range(8):
                            cs = slice(c * HS, (c + 1) * HS)
                            k.pe(lambda q, pp=pp, src=src, cs=cs: q.transpose(out=pp[0:HS, cs], in_=src[:, cs], identity=i64), [src, cst], [pp])
                        k.a(lambda q, pp=pp, dst=dst: q.activation(out=dst[:, :, :], in_=pp[0:HS, :].rearrange("p (a b) -> p a b", a=8), func=AF.Copy), [pp], [dst])

                    if mode == "p5" and h == 0:
                        sA.close()
                        return early_stop()
                    def cmat(lh, rh, mask, sign, dst):
                        pp = psf.next()
                        for c in range(8):
                            cs = slice(c * HS, (c + 1) * HS)
                            k.pe(lambda q, pp=pp, cs=cs: q.matmul(pp[0:HS, cs], lhsT=lh[:, cs], rhs=rh[:, cs], start=True, stop=True), [lh, rh], [pp])
                        k.v(lambda q, pp=pp: q.scalar_tensor_tensor(out=dst[:, :, :], in0=r3(pp[0:HS, :]), scalar=float(sign), in1=mask8[id(mask)], op0=ALU.mult, op1=ALU.mult), [pp, cst], [dst])

                    cmat(KKp, Bpp, m_sl, -1.0, P[0])
                    cmat(Bpp, KKp, m_su, -1.0, Q[0])
                    cmat(Kpp, KKp, m_su, 1.0, MT)
                    if out_grp:
                        cmat(Kpp, Rp, m_ui, 1.0, GT)
                        cmat(Bpp, Rp, m_ui, 1.0, HT)
                    k.v(lambda q: q.tensor_tensor(out=Z[0][:, :, :], in0=Q[0][:, :, :], in1=i64_8, op=ALU.add), [Q[0], cst], [Z[0]])
                    if mode == "p6" and h == 0:
                        sA.close()
                        return early_stop()
                    cur = 0
                    for lvl in range(5):
                        nxt = cur ^ 1
                        ppP = psf.next()
                        for c in range(8):
                            cs = slice(c * HS, (c + 1) * HS)
                            k.pe(lambda q, ppP=ppP, cs=cs, c=c, cur=cur: q.matmul(ppP[0:HS, cs], lhsT=Q[cur][:, c, :], rhs=P[cur][:, c, :], start=True, stop=True), [Q[cur], P[cur]], [ppP])
                        if mode == "d%d0" % lvl and h == 0:
                            sA.close()
                            return early_stop()
                        if lvl < 4:
                            ppQ = psf.next()
                            for c in range(8):
                                cs = slice(c * HS, (c + 1) * HS)
                                k.pe(lambda q, ppQ=ppQ, cs=cs, c=c, cur=cur: q.matmul(ppQ[0:HS, cs], lhsT=P[cur][:, c, :], rhs=Q[cur][:, c, :], start=True, stop=True), [Q[cur], P[cur]], [ppQ])
                            k.v(lambda q, ppP=ppP, nxt=nxt: q.tensor_copy(out=P[nxt][:, :, :], in_=ppP[0:HS, :].rearrange("p (a b) -> p a b", a=8)), [ppP], [P[nxt]])
                            k.v(lambda q, ppQ=ppQ, nxt=nxt: q.tensor_copy(out=Q[nxt][:, :, :], in_=ppQ[0:HS, :].rearrange("p (a b) -> p a b", a=8)), [ppQ], [Q[nxt]])
                        if mode == "d%d1" % lvl and h == 0:
                            sA.close()
                            return early_stop()
                        k.v(lambda q, ppP=ppP: q.tensor_tensor(out=PI[:, :, :], in0=r3(ppP[0:HS, :]), in1=i64_8, op=ALU.add), [ppP, cst], [PI])
                        if mode == "d%d2" % lvl and h == 0:
                            sA.close()
                            return early_stop()
                        zi, zo = lvl % 2, (lvl + 1) % 2
                        ppZ = psf.next()
                        for c in range(8):
                            cs = slice(c * HS, (c + 1) * HS)
                            k.pe(lambda q, ppZ=ppZ, cs=cs, c=c, zi=zi: q.matmul(ppZ[0:HS, cs], lhsT=PI[:, c, :], rhs=Z[zi][:, c, :], start=True, stop=True), [PI, Z[zi]], [ppZ])
                        k.v(lambda q, ppZ=ppZ, zo=zo: q.tensor_copy(out=Z[zo][:, :, :], in_=ppZ[0:HS, :].rearrange("p (a b) -> p a b", a=8)), [ppZ], [Z[zo]])
                        cur = nxt
                        if mode == "d%d3" % lvl and h == 0:
                            sA.close()
                            return early_stop()
                    if mode == "p7" and h == 0:
                        sA.close()
                        return early_stop()
                    ZF = Z[1]
                    TKKT, MV = P[0], Q[0]
                    pp = psf.next()
                    for c in range(8):
                        cs = slice(c * HS, (c + 1) * HS)
                        k.pe(lambda q, pp=pp, cs=cs, c=c: q.matmul(pp[0:HS, cs], lhsT=kktm[:, c, :], rhs=ZF[:, c, :], start=True, stop=True), [kktm, ZF], [pp])
                    k.v(lambda q, pp=pp: q.tensor_copy(out=TKKT[:, :, :], in_=pp[0:HS, :].rearrange("p (a b) -> p a b", a=8)), [pp], [TKKT])
                    pp = psf.next()
                    for c in range(8):
                        cs = slice(c * HS, (c + 1) * HS)
                        k.pe(lambda q, pp=pp, cs=cs, c=c: q.matmul(pp[0:HS, cs], lhsT=MT[:, c, :], rhs=vtm[:, c, :], start=True, stop=True), [MT, vtm], [pp])
                    k.v(lambda q, pp=pp: q.tensor_copy(out=MV[:, :, :], in_=pp[0:HS, :].rearrange("p (a b) -> p a b", a=8)), [pp], [MV])
                    py = pyb
                    for c in range(8):
                        cs = slice(c * HS, (c + 1) * HS)
                        pu_ = psf.next()
                        k.pe(lambda q, pu_=pu_, c=c, h=h: q.matmul(pu_[0:HS, 0:HS], lhsT=TKKT[:, c, :], rhs=S[:, h, :], start=True, stop=False), [TKKT, S], [pu_])
                        k.pe(lambda q, pu_=pu_, c=c: q.matmul(pu_[0:HS, 0:HS], lhsT=ZF[:, c, :], rhs=MV[:, c, :], start=False, stop=True), [ZF, MV], [pu_])
                        k.a(lambda q, pu_=pu_: q.activation(out=NU[:, :], in_=pu_[0:HS, 0:HS], func=AF.Copy, scale=-1.0), [pu_], [NU])
                        if out_grp:
                            k.pe(lambda q, cs=cs, h=h: q.matmul(py[0:HS, cs], lhsT=S[:, h, :], rhs=Rp[:, cs], start=True, stop=False), [S, Rp], [py])
                            k.pe(lambda q, cs=cs, c=c: q.matmul(py[0:HS, cs], lhsT=vtm[:, c, :], rhs=GT[:, c, :], start=False, stop=False), [vtm, GT], [py])
                            k.pe(lambda q, cs=cs, c=c: q.matmul(py[0:HS, cs], lhsT=NU[:, :], rhs=HT[:, c, :], start=False, stop=True), [NU, HT], [py])
                        pS = psf.next()
                        k.pe(lambda q, pS=pS, c=c: q.matmul(pS[0:HS, 0:HS], lhsT=ketm[:, c, :], rhs=vtm[:, c, :], start=True, stop=False), [ketm, vtm], [pS])
                        k.pe(lambda q, pS=pS, c=c: q.matmul(pS[0:HS, 0:HS], lhsT=betm[:, c, :], rhs=NU[:, :], start=False, stop=True), [betm, NU], [pS])
                        k.v(lambda q, pS=pS, c=c, h=h: q.scalar_tensor_tensor(out=S[:, h, :], in0=S[:, h, :], scalar=eL[:, c * HS + HS - 1:c * HS + HS], in1=pS[0:HS, 0:HS], op0=ALU.mult, op1=ALU.add), [S, eL, pS], [S])
                    if not out_grp:
                        continue
                    k.a(lambda q: q.activation(out=ysb[:, :], in_=py[0:HS, :], func=AF.Copy), [py], [ysb])
                    if mode == "p8" and h == 0:
                        sA.close()
                        return early_stop()
                    pp = psf.next()
                    k.pe(lambda q, pp=pp: q.matmul(pp[0:HS, :], lhsT=ones64, rhs=ysb[:, :], start=True, stop=True), [cst, ysb], [pp])
                    k.v(lambda q, pp=pp: q.scalar_tensor_tensor(out=ysb[:, :], in0=pp[0:HS, :], scalar=-1.0 / HS, in1=ysb[:, :], op0=ALU.mult, op1=ALU.add), [pp, ysb], [ysb])
                    k.v(lambda q: q.tensor_tensor(out=t0[:, :], in0=ysb[:, :], in1=ysb[:, :], op=ALU.mult), [ysb], [t0])
                    pp = psf.next()
                    k.pe(lambda q, pp=pp: q.matmul(pp[0:HS, :], lhsT=ones64, rhs=t0[:, :], start=True, stop=True), [cst, t0], [pp])
                    k.v(lambda q, pp=pp: q.tensor_scalar(out=t1[:, :], in0=pp[0:HS, :], scalar1=1.0 / HS, scalar2=GN_EPS, op0=ALU.mult, op1=ALU.add), [pp], [t1])
                    k.a(lambda q: q.activation(out=t1[:, :], in_=t1[:, :], func=AF.Sqrt), [t1], [t1])
                    k.v(lambda q: q.reciprocal(out=t1[:, :], in_=t1[:, :]), [t1], [t1])
                    k.v(lambda q: q.tensor_tensor(out=ysb[:, :], in0=ysb[:, :], in1=t1[:, :], op=ALU.mult), [ysb, t1], [ysb])
                    k.v(lambda q, h=h: q.tensor_scalar(out=ysb[:, :], in0=ysb[:, :], scalar1=rw_cp[:, h, 8:9], scalar2=rw_cp[:, h, 9:10], op0=ALU.mult, op1=ALU.add), [ysb, rw_cp], [ysb])
                    k.v(lambda q, h=h: q.scalar_tensor_tensor(out=t0[:, :], in0=r_[:, :], scalar=rw_cp[:, h, 7:8], in1=k_[:, :], op0=ALU.mult, op1=ALU.mult), [r_, rw_cp, k_], [t0])
                    pp = psf.next()
                    k.pe(lambda q, pp=pp: q.matmul(pp[0:HS, :], lhsT=ones64, rhs=t0[:, :], start=True, stop=True), [cst, t0], [pp])
                    k.v(lambda q, pp=pp: q.tensor_tensor(out=t1[:, :], in0=pp[0:HS, :], in1=v_[:, :], op=ALU.mult), [pp, v_], [t1])
                    k.v(lambda q: q.tensor_tensor(out=ysb[:, :], in0=ysb[:, :], in1=t1[:, :], op=ALU.add), [ysb, t1], [ysb])
                    k.v(lambda q, h=h: q.tensor_tensor(out=yr[:, h, :], in0=ysb[:, :], in1=g_[:, :], op=ALU.mult), [ysb, g_], [yr])
            k.barrier()
            if mode == "sA":
                return early_stop()
            if g < NG // 2:
                continue
            if mode == "mixer" and DEBUG and g == NG // 2:
                dbb = Buf(dbg_yl)
                k.dma("sp", lambda q: q.dma_start(out=dbg_yl, in_=yl[:, :, :]), [yl], [dbb])
                k.dma("sp", lambda q: q.dma_start(out=dbg_yr, in_=yr[:, :, :]), [yr], [dbb])
                k.dma("sp", lambda q: q.dma_start(out=dbg_hT, in_=hT[:, :, :]), [hT], [dbb])
            with ExitStack() as sB:
                k.es = sB
                lnin = k.sb("lninB", [128, 2, D])
                k.dma("sp", lambda q: q.dma_start(out=lnin[:, :, :], in_=lnrep[0:2].rearrange("a p d -> p a d")), [], [lnin])
                ln1 = k.sb("ln1", [128, 2, D])
                k.dma("sp", lambda q: q.dma_start(out=ln1[:, :, :], in_=lnrep[2:4].rearrange("a p d -> p a d")), [], [ln1])
                lntmp = k.sb("lntmpB", [128, D])
                zb = [k.sb("zb%d" % i, [128, D]) for i in range(4)]
                wol = k.sb("wol", [128, 8, 512], BF16)
                wor = k.sb("wor", [HS, NH, 512], BF16)
                h1T = k.sb("h1T", [128, FC, 128])
                for ti in range(4):
                    r0 = t0g + ti * 128
                    k.dma("sp", lambda q, ti=ti, r0=r0: q.dma_start(out=zb[ti][:, :], in_=xa[r0:r0 + 128, :]), [], [zb[ti]])
                    layer_norm(zb[ti][:, :], zb[ti], zb[ti][:, :], zb[ti], lnin, lnin, 0, ALPHA, lntmp, st, mv)
                for n in range(4):
                    k.dma("pool", lambda q, n=n: q.dma_start(out=wol[:, :, :], in_=wout_l[n]), [], [wol])
                    k.dma("pool", lambda q, n=n: q.dma_start(out=wor[:, :, :], in_=wout_r[n]), [], [wor])
                    for ti in range(4):
                        ts_ = slice(ti * 128, (ti + 1) * 128)
                        pp = psf.next()
                        for j in range(8):
                            k.pe(lambda q, pp=pp, j=j, ts_=ts_: q.matmul(pp[:, :], lhsT=yl[:, j, ts_], rhs=wol[:, j, :], start=(j == 0), stop=False), [yl, wol], [pp])
                        for j in range(NH):
                            k.pe(lambda q, pp=pp, j=j, ts_=ts_: q.matmul(pp[:, :], lhsT=yr[:, j, ts_], rhs=wor[:, j, :], start=False, stop=(j == NH - 1)), [yr, wor], [pp])
                        k.v(lambda q, pp=pp, ti=ti, n=n: q.tensor_tensor(out=zb[ti][:, n * 512:(n + 1) * 512], in0=zb[ti][:, n * 512:(n + 1) * 512], in1=pp[:, :], op=ALU.add), [zb[ti], pp], [zb[ti]])
                for ti in range(4):
                    tg = (g - NG // 2) * 4 + ti
                    h1 = zb[ti]
                    layer_norm(h1[:, :], h1, h1[:, :], h1, ln1, ln1, 0, 1.0, lntmp, st, mv)
                    k.dma("sp", lambda q, tg=tg, h1=h1: q.dma_start(out=H1[tg * 128:(tg + 1) * 128, :], in_=h1[:, :]), [h1], [H1b])
                    for c4 in range(4):
                        pp = psf.next()
                        for j in range(4):
                            kc = c4 * 4 + j
                            k.pe(lambda q, pp=pp, kc=kc, j=j, h1=h1: q.transpose(out=pp[:, j * 128:(j + 1) * 128], in_=h1[:, kc * 128:(kc + 1) * 128], identity=ident), [h1, cst], [pp])
                        k.a(lambda q, pp=pp, c4=c4: q.activation(out=h1T[:, c4 * 4:(c4 + 1) * 4, :], in_=pp[:, :].rearrange("p (a b) -> p a b", a=4), func=AF.Copy), [pp], [h1T])
                    pp = psf.next()
                    for kc in range(FC):
                        k.pe(lambda q, pp=pp, kc=kc: q.matmul(pp[:, 0:NE], lhsT=h1T[:, kc, :], rhs=wr[:, kc, :], start=(kc == 0), stop=(kc == FC - 1)), [h1T, wr], [pp])
                    k.v(lambda q, pp=pp: q.tensor_tensor(out=lg[:, :], in0=pp[:, 0:NE], in1=brt[:, :], op=ALU.add), [pp, brt], [lg])
                    k.v(lambda q: q.max(out=m8[:, :], in_=lg[:, :]), [lg], [m8])
                    k.v(lambda q: q.max_index(out=i8[:, :], in_max=m8[:, :], in_values=lg[:, :]), [m8, lg], [i8])
                    k.v(lambda q, tg=tg: q.tensor_copy(out=eidf[:, tg, :], in_=i8[:, :]), [i8], [eidf])
                    k.v(lambda q: q.tensor_scalar(out=sm[:, 0:1], in0=m8[:, 0:1], scalar1=-1.0, scalar2=None, op0=ALU.mult), [m8], [sm])
                    k.a(lambda q: q.activation(out=sm[:, 4:8], in_=m8[:, 0:4], func=AF.Exp, bias=sm[:, 0:1]), [m8, sm], [sm])
                    k.v(lambda q: q.reduce_sum(out=sm[:, 1:2], in_=sm[:, 4:8], axis=AX.X), [sm], [sm])
                    k.v(lambda q: q.reciprocal(out=sm[:, 2:3], in_=sm[:, 1:2]), [sm], [sm])
                    k.v(lambda q, tg=tg: q.tensor_scalar(out=gates[:, tg, :], in0=sm[:, 4:8], scalar1=sm[:, 2:3], scalar2=None, op0=ALU.mult), [sm], [gates])
                    for kk in range(TOPK):
                        k.v(lambda q, tg=tg, kk=kk: q.tensor_scalar(out=ohall[:, tg * TOPK + kk, :], in0=iota_e, scalar1=eidf[:, tg, kk:kk + 1], scalar2=None, op0=ALU.is_equal), [cst, eidf], [ohall])
                    k.v(lambda q, tg=tg: q.tensor_tensor(out=mall[:, tg, :], in0=ohall[:, tg * TOPK, :], in1=ohall[:, tg * TOPK + 1, :], op=ALU.add), [ohall], [mall])
                    for kk in range(2, TOPK):
                        k.v(lambda q, tg=tg, kk=kk: q.tensor_tensor(out=mall[:, tg, :], in0=mall[:, tg, :], in1=ohall[:, tg * TOPK + kk, :], op=ALU.add), [ohall, mall], [mall])
            k.barrier()
        with ExitStack() as sC:
            k.es = sC
            cntb = k.sb("cntb", [128, 4, NE])
            pp = psf.next()
            for tg in range(NTILE):
                k.pe(lambda q, pp=pp, tg=tg: q.matmul(pp[:, 0:NE], lhsT=ones, rhs=mall[:, tg, :], start=(tg == 0), stop=(tg == NTILE - 1)), [cst, mall], [pp])
            k.v(lambda q, pp=pp: q.tensor_copy(out=cntb[:, 3, :], in_=pp[:, 0:NE]), [pp], [cntb])
            k.v(lambda q: q.tensor_scalar(out=cntb[:, 0, :], in0=cntb[:, 3, :], scalar1=0.0, scalar2=None, op0=ALU.is_gt), [cntb], [cntb])
            for j_ in range(1, NTILE):
                k.v(lambda q, j_=j_: q.scalar_tensor_tensor(out=cntb[:, 0, :], in0=cntb[:, 3, :], scalar=float(128 * j_), in1=cntb[:, 0, :], op0=ALU.is_gt, op1=ALU.add), [cntb], [cntb])
            k.v(lambda q: q.tensor_scalar(out=cntb[:, 0, :], in0=cntb[:, 0, :], scalar1=128.0, scalar2=None, op0=ALU.mult), [cntb], [cntb])
            k.v(lambda q: q.tensor_tensor_scan(out=cntb[:, 1, :], data0=ones[:, 0:NE], data1=cntb[:, 0, :], initial=0.0, op0=ALU.mult, op1=ALU.add), [cst, cntb], [cntb])
            k.v(lambda q: q.tensor_tensor(out=cntb[:, 2, :], in0=cntb[:, 1, :], in1=cntb[:, 0, :], op=ALU.subtract), [cntb], [cntb])
            blkf = k.sb("blkf", [128, 2])
            blki = k.sb("blki", [128, 1], I32)
            k.v(lambda q: q.tensor_scalar(out=cntb[:, 3, :], in0=cntb[:, 1, :], scalar1=iota_p, scalar2=None, op0=ALU.is_le), [cntb, cst], [cntb])
            k.v(lambda q: q.reduce_sum(out=blkf[:, 0:1], in_=cntb[:, 3, :], axis=AX.X), [cntb], [blkf])
            k.v(lambda q: q.tensor_scalar(out=blkf[:, 1:2], in0=blkf[:, 0:1], scalar1=float(NE - 1), scalar2=None, op0=ALU.min), [blkf], [blkf])
            dg = k.sb("dg", [128, 128])
            ef = k.sb("ef", [128, 3, 128])
            tf = k.sb("tf", [128, 8, 128])
            pcol = k.sb("pcol", [128, 1])
            k.v(lambda q: q.tensor_scalar(out=pcol[:, :], in0=iota_p, scalar1=1.0 / 128.0, scalar2=None, op0=ALU.mult), [cst], [pcol])
            k.v(lambda q: q.tensor_scalar(out=dg[:, :], in0=ident, scalar1=blkf[:, 1:2], scalar2=None, op0=ALU.mult), [cst, blkf], [dg])
            ppe = psf.next()
            k.pe(lambda q: q.matmul(ppe[:, 0:128], lhsT=ones, rhs=dg[:, :], start=True, stop=True), [cst, dg], [ppe])
            k.v(lambda q: q.tensor_copy(out=ef[:, 0, :], in_=ppe[:, 0:128]), [ppe], [ef])
            k.v(lambda q: q.tensor_scalar(out=ef[:, 1, :], in0=ef[:, 0, :], scalar1=1024.0, scalar2=pcol[:, 0:1], op0=ALU.mult, op1=ALU.add), [ef, pcol], [ef])
            k.v(lambda q: q.tensor_scalar(out=ef[:, 2, :], in0=ef[:, 0, :], scalar1=512.0, scalar2=pcol[:, 0:1], op0=ALU.mult, op1=ALU.add), [ef, pcol], [ef])
            k.v(lambda q: q.tensor_copy(out=eidx_i[:, :], in_=ef[:, 0, :]), [ef], [eidx_i])
            for n_ in range(8):
                k.v(lambda q, n_=n_: q.tensor_scalar(out=tf[:, n_, :], in0=ef[:, 1, :], scalar1=float(n_ * 128), scalar2=None, op0=ALU.add), [ef], [tf])
            k.v(lambda q: q.tensor_copy(out=idx1_i[:, :, :], in_=tf[:, :, :]), [tf], [idx1_i])
            for n_ in range(4):
                k.v(lambda q, n_=n_: q.tensor_scalar(out=tf[:, n_, :], in0=ef[:, 2, :], scalar1=float(n_ * 128), scalar2=None, op0=ALU.add), [ef], [tf])
            k.v(lambda q: q.tensor_copy(out=idx2_i[:, :, :], in_=tf[:, 0:4, :]), [tf], [idx2_i])
            slotf = k.sb("slotf", [128, NTILE * TOPK])
            basef = k.sb("basef", [128, NE])
            prodf = k.sb("prodf", [128, NE])
            for tg in range(NTILE):
                pp = psf.next()
                for t2 in range(tg):
                    k.pe(lambda q, pp=pp, t2=t2: q.matmul(pp[:, 0:NE], lhsT=ones, rhs=mall[:, t2, :], start=(t2 == 0), stop=False), [cst, mall], [pp])
                k.pe(lambda q, pp=pp, tg=tg: q.matmul(pp[:, 0:NE], lhsT=tri, rhs=mall[:, tg, :], start=(tg == 0), stop=True), [cst, mall], [pp])
                k.v(lambda q, pp=pp: q.tensor_tensor(out=basef[:, :], in0=pp[:, 0:NE], in1=cntb[:, 2, :], op=ALU.add), [pp, cntb], [basef])
                for kk in range(TOPK):
                    j = tg * TOPK + kk
                    k.v(lambda q, j=j: q.tensor_tensor(out=prodf[:, :], in0=basef[:, :], in1=ohall[:, j, :], op=ALU.mult), [basef, ohall], [prodf])
                    k.v(lambda q, j=j: q.reduce_sum(out=slotf[:, j:j + 1], in_=prodf[:, :], axis=AX.X), [prodf], [slotf])
            k.v(lambda q: q.tensor_copy(out=slot_i[:, :, 0], in_=slotf[:, :]), [slotf], [slot_i])
            dbg("slotf", slotf[:, :], [128, NTILE * TOPK], slotf)
            dbg("cntb", cntb[:, :, :], [128, 4, NE], cntb)
            dbg("eidf", eidf[:, :, :], [128, NTILE, 8], eidf)
            dbg("gates", gates[:, :, :], [128, NTILE, TOPK], gates)
            dbg("sloti", slot_i[:, :, :], [128, NTILE * TOPK, 1], slot_i, I32)
            dbg("ef", ef[:, :, :], [128, 3, 128], ef)
            dbg("mall", mall[:, :, :], [128, NTILE, NE], mall)
        k.barrier()
        esM.close()
        if mode == "mixer":
            with ExitStack() as esX:
                k.es = esX
                tb = k.sb("tb", [128, D])
                for tg in range(NTILE):
                    k.dma("sp", lambda q, tg=tg: q.dma_start(out=tb[:, :], in_=H1[tg * 128:(tg + 1) * 128, :]), [H1b], [tb])
                    k.dma("sp", lambda q, tg=tg: q.dma_start(out=out_d[tg * 128:(tg + 1) * 128, :], in_=tb[:, :]), [tb], [OUTB])
                k.wait_all("sp", [OUTB])
            k.es = es
            print("instructions:", k.n_ins)
            return nc
        with ExitStack() as es3:
            k.es = es3
            hb = k.ring("hb", [128, D], BF16, 2)
            zt_ = hb.next()
            k.v(lambda q: q.memset(zt_[:, :], 0.0), [], [zt_])
            for s in range(NBLK):
                k.dma("sp", lambda q, s=s: q.dma_start(out=XS[s * 128:(s + 1) * 128, :], in_=zt_[:, :]), [zt_], [XSb])
            for tg in range(NTILE):
                t = hb.next()
                k.dma("pool", lambda q, t=t, tg=tg: q.dma_start(out=t[:, :], in_=H1[tg * 128:(tg + 1) * 128, :]), [H1b], [t])
                for kk in range(TOPK):
                    j = tg * TOPK + kk
                    k.dma("pool", lambda q, t=t, j=j: q.indirect_dma_start(out=XS, out_offset=bass.IndirectOffsetOnAxis(ap=slot_i[:, j, :], axis=0), in_=t[:, :], in_offset=None), [t, slot_i], [XSb])
            wbuf = k.ring("wbuf", [128, FC * 512], BF16, 3)
            b1f = k.sb("b1f", [128, 2 * DE])
            b2f = k.sb("b2f", [128, D])
            xb = k.ring("xb", [128, D], BF16, 2)
            xT = k.sb("xT", [128, FC, 128], BF16)
            act = k.sb("act", [128, DE], BF16)
            actT = k.sb("actT", [128, FC, 128], BF16)
            ob = k.ring("ob", [128, D], F32, 2)
            b1b = k.ring("b1b", [1, 2 * DE], BF16, 1)
            b2b = k.ring("b2b", [1, D], BF16, 1)
            gsb = k.ring("gsb", [128, 4, 512], F32, 2)
            s_g, s_u, s_s, s_p = 0, 1, 2, 3
            for s in range(NBLK):
                x_ = xb.next()
                k.dma("sp", lambda q, x_=x_, s=s: q.dma_start(out=x_[:, :], in_=XS[s * 128:(s + 1) * 128, :]), [XSb], [x_])
                b1 = b1b.next()
                b2 = b2b.next()
                k.dma("pool", lambda q, s=s: q.indirect_dma_start(out=b1f[:, :], out_offset=None, in_=b1_d, in_offset=bass.IndirectOffsetOnAxis(ap=eidx_i[:, s:s + 1], axis=0)), [eidx_i], [b1f])
                k.dma("pool", lambda q, s=s: q.indirect_dma_start(out=b2f[:, :], out_offset=None, in_=b2_d, in_offset=bass.IndirectOffsetOnAxis(ap=eidx_i[:, s:s + 1], axis=0)), [eidx_i], [b2f])
                k.a(lambda q, b1=b1: q.activation(out=b1[:, :], in_=b1f[0:1, :], func=AF.Copy), [b1f], [b1])
                k.a(lambda q, b2=b2: q.activation(out=b2[:, :], in_=b2f[0:1, :], func=AF.Copy), [b2f], [b2])
                for c4 in range(4):
                    hf = psb_half[0]
                    psb_half[0] ^= 1
                    for j in range(4):
                        kc = c4 * 4 + j
                        k.pe(lambda q, kc=kc, hf=hf, j=j, x_=x_: q.transpose(out=psb[:, hf * 512 + j * 128: hf * 512 + (j + 1) * 128], in_=x_[:, kc * 128:(kc + 1) * 128], identity=identb[:, :]), [x_, identb], [psb])
                    k.a(lambda q, c4=c4, hf=hf: q.activation(out=xT[:, c4 * 4:(c4 + 1) * 4, :], in_=psb[:, hf * 512:(hf + 1) * 512].rearrange("p (a b) -> p a b", a=4), func=AF.Copy), [psb], [xT])
                for n in range(4):
                    gs = gsb.next()
                    pgu = []
                    for half in range(2):
                        col0 = half * DE + n * 512
                        w = wbuf.next()
                        n8 = half * 4 + n
                        k.dma("pool", lambda q, w=w, n8=n8, s=s: q.indirect_dma_start(out=w[:, :], out_offset=None, in_=w1_d, in_offset=bass.IndirectOffsetOnAxis(ap=idx1_i[:, n8, s:s + 1], axis=0)), [idx1_i], [w])
                        pp = psf.next()
                        for kc in range(FC):
                            k.pe(lambda q, pp=pp, kc=kc, w=w: q.matmul(pp[:, :], lhsT=xT[:, kc, :], rhs=w[:, kc * 512:(kc + 1) * 512], start=(kc == 0), stop=False), [xT, w], [pp])
                        k.pe(lambda q, pp=pp, col0=col0, b1=b1: q.matmul(pp[:, :], lhsT=onesb[:, :], rhs=b1[:, col0:col0 + 512], start=False, stop=True), [onesb, b1], [pp])
                        pgu.append(pp)
                    k.v(lambda q, gs=gs, pp=pgu[0]: q.tensor_scalar(out=gs[:, s_g, :], in0=pp[:, :], scalar1=7.0, scalar2=None, op0=ALU.min), [pgu[0]], [gs])
                    k.a(lambda q, gs=gs: q.activation(out=gs[:, s_s, :], in_=gs[:, s_g, :], func=AF.Sigmoid, scale=1.702), [gs], [gs])
                    k.v(lambda q, gs=gs, pp=pgu[1]: q.tensor_scalar(out=gs[:, s_u, :], in0=pp[:, :], scalar1=7.0, scalar2=-7.0, op0=ALU.min, op1=ALU.max), [pgu[1]], [gs])
                    k.v(lambda q, gs=gs: q.tensor_tensor(out=gs[:, s_p, :], in0=gs[:, s_g, :], in1=gs[:, s_s, :], op=ALU.mult), [gs], [gs])
                    k.v(lambda q, gs=gs, n=n: q.scalar_tensor_tensor(out=act[:, n * 512:(n + 1) * 512], in0=gs[:, s_u, :], scalar=1.0, in1=gs[:, s_p, :], op0=ALU.add, op1=ALU.mult), [gs], [act])
                for c4 in range(4):
                    hf = psb_half[0]
                    psb_half[0] ^= 1
                    for j in range(4):
                        kc = c4 * 4 + j
                        k.pe(lambda q, kc=kc, hf=hf, j=j: q.transpose(out=psb[:, hf * 512 + j * 128: hf * 512 + (j + 1) * 128], in_=act[:, kc * 128:(kc + 1) * 128], identity=identb[:, :]), [act, identb], [psb])
                    k.a(lambda q, c4=c4, hf=hf: q.activation(out=actT[:, c4 * 4:(c4 + 1) * 4, :], in_=psb[:, hf * 512:(hf + 1) * 512].rearrange("p (a b) -> p a b", a=4), func=AF.Copy), [psb], [actT])
                o_ = ob.next()
                for n in range(4):
                    w = wbuf.next()
                    k.dma("pool", lambda q, w=w, n=n, s=s: q.indirect_dma_start(out=w[:, :], out_offset=None, in_=w2_d, in_offset=bass.IndirectOffsetOnAxis(ap=idx2_i[:, n, s:s + 1], axis=0)), [idx2_i], [w])
                    pp = psf.next()
                    for kc in range(FC):
                        k.pe(lambda q, pp=pp, kc=kc, w=w: q.matmul(pp[:, :], lhsT=actT[:, kc, :], rhs=w[:, kc * 512:(kc + 1) * 512], start=(kc == 0), stop=False), [actT, w], [pp])
                    k.pe(lambda q, pp=pp, n=n, b2=b2: q.matmul(pp[:, :], lhsT=onesb[:, :], rhs=b2[:, n * 512:(n + 1) * 512], start=False, stop=True), [onesb, b2], [pp])
                    k.a(lambda q, pp=pp, n=n, o_=o_: q.activation(out=o_[:, n * 512:(n + 1) * 512], in_=pp[:, :], func=AF.Copy), [pp], [o_])
                k.dma("sp", lambda q, o_=o_, s=s: q.dma_start(out=OS[s * 128:(s + 1) * 128, :], in_=o_[:, :]), [o_], [OSb])
        k.barrier()
        with ExitStack() as es4:
            k.es = es4
            lnp = k.sb("ln2", [128, 2, D])
            k.dma("sp", lambda q: q.dma_start(out=lnp[:, :, :], in_=lnrep[4:6].rearrange("a p d -> p a d")), [], [lnp])
            gth = k.ring("gth", [128, D], F32, 3)
            h1r = k.ring("h1r", [128, D], F32, 2)
            zt = k.sb("zt", [128, D])
            lntmp = k.sb("lntmp2", [128, D])
            st = k.sb("st2", [128, 24])
            mv = k.sb("mv2", [128, 4])
            outr = k.ring("outr", [128, D], F32, 2)
            for tg in range(NTILE):
                hh = h1r.next()
                k.dma("sp", lambda q, hh=hh, tg=tg: q.dma_start(out=hh[:, :], in_=H1[tg * 128:(tg + 1) * 128, :]), [H1b], [hh])
                k.v(lambda q, hh=hh: q.tensor_scalar(out=zt[:, :], in0=hh[:, :], scalar1=float(ALPHA), scalar2=None, op0=ALU.mult), [hh], [zt])
                for kk in range(TOPK):
                    j = tg * TOPK + kk
                    gt = gth.next()
                    k.dma("pool", lambda q, gt=gt, j=j: q.indirect_dma_start(out=gt[:, :], out_offset=None, in_=OS, in_offset=bass.IndirectOffsetOnAxis(ap=slot_i[:, j, :], axis=0)), [OSb, slot_i], [gt])
                    k.v(lambda q, gt=gt, tg=tg, kk=kk: q.scalar_tensor_tensor(out=zt[:, :], in0=gt[:, :], scalar=gates[:, tg, kk:kk + 1], in1=zt[:, :], op0=ALU.mult, op1=ALU.add), [gt, gates, zt], [zt])
                ot = outr.next()
                layer_norm(zt[:, :], zt, ot[:, :], ot, lnp, lnp, 0, 1.0, lntmp, st, mv)
                k.dma("sp", lambda q, ot=ot, tg=tg: q.dma_start(out=out_d[tg * 128:(tg + 1) * 128, :], in_=ot[:, :]), [ot], [OUTB])
            k.wait_all("sp", [OUTB])
        k.es = es
        print("instructions:", k.n_ins)
    return nc


def _consts(NE):
    c = np.zeros((128, 128 * 3 + 64 * 4 + NE + 1 + 4 * 512), np.float32)
    c[:, 0:128] = np.eye(128)
    c[:, 128:256] = 1.0
    c[:, 256:384] = np.triu(np.ones((128, 128)), 1)
    o = 384
    t = np.arange(64)
    c[0:64, o:o + 64] = (t[:, None] > t[None, :])
    c[0:64, o + 64:o + 128] = (t[None, :] > t[:, None])
    c[0:64, o + 128:o + 192] = (t[None, :] >= t[:, None])
    c[0:64, o + 192:o + 256] = np.eye(64)
    c[:, o + 256:o + 256 + NE] = np.arange(NE)[None, :]
    c[:, o + 256 + NE] = np.arange(128) * 128.0
    o8 = o + 257 + NE
    for i_ in range(4):
        c[0:64, o8 + i_ * 512:o8 + (i_ + 1) * 512] = np.tile(c[0:64, o + i_ * 64:o + (i_ + 1) * 64], (1, 8))
    return c


def prepare_shared(p, NE):
    f = np.float32
    sh = {}
    ln = np.stack([p["ln_in_g"], p["ln_in_b"], p["ln1_g"][0], p["ln1_b"][0], p["ln2_g"][0], p["ln2_b"][0]])
    sh["lnrep"] = np.ascontiguousarray(np.broadcast_to(ln[:, None, :], (6, 128, D))).astype(f)
    w_in = p["w_in"][0]
    wl = w_in[:, 0:2048].reshape(FC, 128, 16, 128)
    sh["win_lru"] = np.ascontiguousarray(wl.transpose(2, 1, 0, 3))
    rkv = w_in[:, 2048:2048 + 3072].reshape(FC, 128, 3, NH, HS)
    sh["win_rkv"] = np.ascontiguousarray(rkv.transpose(3, 1, 0, 2, 4).reshape(NH, 128, FC, 3 * HS))
    lo = w_in[:, 2048 + 3072:].reshape(FC, 128, 448)
    sh["win_lora"] = np.ascontiguousarray(lo.transpose(1, 0, 2))
    cp = np.stack([p["conv_w"][0][0], p["conv_w"][0][1], p["conv_w"][0][2], p["conv_w"][0][3], p["conv_b"][0], p["b_rgate"][0], p["b_igate"][0], p["lru_lambda"][0]], -1)
    sh["lru_cp"] = np.ascontiguousarray(cp.reshape(8, 128, 8).transpose(1, 0, 2))

    def bd(w):
        o = np.zeros((8, 128, 128), f)
        for n in range(16):
            t, q = n // 2, (n % 2) * 64
            o[t, q:q + 64, q:q + 64] = w[n]
        return o
    sh["wrg_bd"] = bd(p["w_rgate"][0])
    sh["wig_bd"] = bd(p["w_igate"][0])
    mu = p["shift_mu"][0]
    hv = lambda v: v.reshape(NH, HS).T
    cols = [hv(mu[0:1024]), hv(mu[1024:2048]), hv(mu[2048:3072]), hv(p["w0"][0]), hv(p["a0"][0]), hv(p["k_k"][0]), hv(p["k_a"][0]), p["r_k"][0].T, hv(p["gn_g"][0]), hv(p["gn_b"][0])]
    sh["rw_cp"] = np.ascontiguousarray(np.stack(cols, -1)).astype(f)
    ml = np.zeros((128, 4), f)
    ml[0:96, 0] = mu[3072:3168]
    ml[0:96, 1] = mu[3168:3264]
    ml[:, 2] = mu[3264:3392]
    ml[:, 3] = mu[3392:3520]
    sh["mu_lora"] = ml
    sh["dec_up"] = np.ascontiguousarray(p["rw_decay_up"][0].reshape(96, NH, HS).transpose(1, 0, 2))
    sh["aaa_up"] = np.ascontiguousarray(p["rw_aaa_up"][0].reshape(96, NH, HS).transpose(1, 0, 2))
    sh["gate_up"] = np.ascontiguousarray(p["rw_gate_up"][0].reshape(2, 128, NH, HS).transpose(2, 1, 0, 3))
    wo = p["w_out"][0]
    sh["wout_l"] = np.ascontiguousarray(wo[0:1024].reshape(8, 128, 4, 512).transpose(2, 1, 0, 3))
    sh["wout_r"] = np.ascontiguousarray(wo[1024:2048].reshape(NH, HS, 4, 512).transpose(2, 1, 0, 3))
    sh["w_router"] = np.ascontiguousarray(p["w_router"][0].reshape(FC, 128, NE).transpose(1, 0, 2))
    sh["b_router"] = np.ascontiguousarray(np.broadcast_to(p["b_router"][0][None, :], (128, NE))).astype(f)
    sh["w_exp1"] = np.ascontiguousarray(p["w_exp1"][0].reshape(NE, FC, 128, 8, 512).transpose(0, 3, 2, 1, 4)).reshape(NE * 8 * 128, FC * 512)
    sh["b_exp1"] = np.ascontiguousarray(p["b_exp1"][0])
    sh["w_exp2"] = np.ascontiguousarray(p["w_exp2"][0].reshape(NE, FC, 128, 4, 512).transpose(0, 3, 2, 1, 4)).reshape(NE * 4 * 128, FC * 512)
    sh["b_exp2"] = np.ascontiguousarray(p["b_exp2"][0])
    sh["cst"] = _consts(NE)
    return sh


def run(inputs, TH, NE, n_cores, mode="full"):
    x = np.asarray(inputs["x"], np.float32)
    B, T, _ = x.shape
    assert T == 2 * TH and B * 2 == n_cores
    p = {kk: np.asarray(v, np.float32) for kk, v in inputs.items() if kk != "x"}
    sh = prepare_shared(p, NE)
    in_maps = []
    for c in range(n_cores):
        b, j = c // 2, c % 2
        first = x[b, 0:TH]
        second = x[b, j * TH:(j + 1) * TH]
        m = dict(sh)
        m["xa"] = np.ascontiguousarray(np.concatenate([first, second], 0))
        m["flag"] = np.full((128, 1), float(j), np.float32)
        in_maps.append(m)
    if mode != "full":
        for m in in_maps:
            m["w_exp1"] = np.zeros((128, FC * 512), np.float32)
            m["w_exp2"] = np.zeros((128, FC * 512), np.float32)
    nc = build(TH, NE, mode)
    res = run_bass_kernel_spmd(nc, in_maps, core_ids=list(range(n_cores)))
    out = np.zeros((B, T, D), np.float32)
    for c in range(n_cores):
        b, j = c // 2, c % 2
        out[b, j * TH:(j + 1) * TH] = res.results[c]["out"]
    return out


def kernel(**inputs):
    return run(inputs, 2048, 32, 8)
```
